# Optimizing a Trainium2 kernel written in Bass

```python
import jax
import jax.numpy as jnp
from jax import lax
import numpy as np

D_MODEL = 1024
BATCH = 8
SEQ = 4096
DEPTH = 2

GRID_W = 64
CTX_LEN = 256
EPS = 1e-6
N_MOD = 6

A_HEADS = 8
A_KV_HEADS = 2
A_GROUP = A_HEADS // A_KV_HEADS
A_HEAD_DIM = 64
A_WIDTH = A_HEADS * A_HEAD_DIM
A_KV_WIDTH = A_KV_HEADS * A_HEAD_DIM
A_SCALE = A_HEAD_DIM ** -0.5
ROPE_THETA = 10000.0
ROPE_FREQS = A_HEAD_DIM // 4
Q_BLOCK = 128

B_GROUPS = 4
B_WIDTH = D_MODEL // 2
B_GROUP_DIM = B_WIDTH // B_GROUPS
POOL_WINDOWS = (2, 4, 8, 16)

EVEN_IN = A_WIDTH + 2 * A_KV_WIDTH + B_WIDTH
EVEN_OUT = A_WIDTH + B_WIDTH

C_HEADS = 16
C_HEAD_DIM = D_MODEL // C_HEADS
C_WIDTH = C_HEADS * C_HEAD_DIM
C_SCALE = C_HEAD_DIM ** -0.5
NA_ROWS_MAX = 8
NA_COLS = 16

N_GROUPS = 4
E_PER_GROUP = 8
N_EXPERTS = N_GROUPS * E_PER_GROUP
TOP_K = 2
D_EXPERT = D_MODEL // 2
MOE_BLOCK = 128

kernel_name = 'hybrid_gqa_pool_natten_hmoe_diffusion'


def rmsnorm(x, g):
    xf = x.astype(jnp.float32)
    y = xf * lax.rsqrt(jnp.mean(xf * xf, axis=-1, keepdims=True) + EPS)
    return (y * g.astype(jnp.float32)).astype(x.dtype)


def modulate(h, shift, scale):
    return h * (1 + scale) + shift


def axial_rope_tables(n_tok):
    t = jnp.arange(n_tok, dtype=jnp.int32)
    pos = jnp.stack([t // GRID_W, t % GRID_W], axis=-1).astype(jnp.float32)
    inv = ROPE_THETA ** (-jnp.arange(ROPE_FREQS, dtype=jnp.float32) / ROPE_FREQS)
    ang = pos[:, :, None] * inv
    return jnp.cos(ang), jnp.sin(ang)


def apply_axial_rope(x, cos, sin):
    sh = x.shape
    xr = x.reshape(sh[:-1] + (2, 2, ROPE_FREQS)).astype(jnp.float32)
    x1, x2 = xr[..., 0, :], xr[..., 1, :]
    out = jnp.stack([x1 * cos - x2 * sin, x1 * sin + x2 * cos], axis=-2)
    return out.reshape(sh).astype(x.dtype)


def gqa_queries(q, gain):
    B, L, _ = q.shape
    q = rmsnorm(q.reshape(B, L, A_KV_HEADS, A_GROUP, A_HEAD_DIM), gain)
    return q.transpose(0, 2, 3, 1, 4)


def gqa_keys(k, gain):
    B, L, _ = k.shape
    return rmsnorm(k.reshape(B, L, A_KV_HEADS, A_HEAD_DIM), gain).transpose(0, 2, 1, 3)


def gqa_values(v):
    B, L, _ = v.shape
    return v.reshape(B, L, A_KV_HEADS, A_HEAD_DIM).transpose(0, 2, 1, 3)


def pool_mixer(u, pool_w, pool_scale):
    B, L, _ = u.shape
    ug = u.reshape(B, L, B_GROUPS, B_GROUP_DIM).astype(jnp.float32)
    cs = jnp.concatenate([jnp.zeros((B, 1, B_GROUPS, B_GROUP_DIM), jnp.float32), jnp.cumsum(ug, axis=1)], axis=1)
    t = jnp.arange(L, dtype=jnp.int32)
    means = []
    for g, w in enumerate(POOL_WINDOWS):
        lo = jnp.clip(t - w // 2, 0, L)
        hi = jnp.clip(t - w // 2 + w, 0, L)
        cnt = (hi - lo).astype(jnp.float32)
        cs_g = cs[:, :, g]
        means.append((cs_g[:, hi] - cs_g[:, lo]) / cnt[None, :, None])
    pooled = (jnp.stack(means, axis=2) - ug).astype(u.dtype)
    mixed = jnp.einsum('blgc,gcd->blgd', pooled, pool_w)
    return mixed.reshape(B, L, B_WIDTH) * pool_scale


def even_mixer(h, hc, w_in, w_out, q_gain, k_gain, pool_w, pool_scale, cos, sin, ctx_out):
    B, S, _ = h.shape
    cuts = (A_WIDTH, A_WIDTH + A_KV_WIDTH, A_WIDTH + 2 * A_KV_WIDTH)
    q, k, v, u = jnp.split(h @ w_in, cuts, axis=-1)
    if ctx_out:
        qc, kc, vc, uc = jnp.split(hc @ w_in, cuts, axis=-1)
    else:
        kc, vc = jnp.split(hc @ w_in[:, A_WIDTH:A_WIDTH + 2 * A_KV_WIDTH], 2, axis=-1)
    kc_h = gqa_keys(kc, k_gain)
    vc_h = gqa_values(vc)
    q_h = apply_axial_rope(gqa_queries(q, q_gain), cos, sin)
    k_h = apply_axial_rope(gqa_keys(k, k_gain), cos, sin)
    k_all = jnp.concatenate([kc_h, k_h], axis=2)
    v_all = jnp.concatenate([vc_h, gqa_values(v)], axis=2)
    nb = S // Q_BLOCK
    qb = q_h.reshape(B, A_KV_HEADS, A_GROUP, nb, Q_BLOCK, A_HEAD_DIM).transpose(3, 0, 1, 2, 4, 5)

    def attend_block(qblk):
        s = jnp.einsum('bhgqd,bhkd->bhgqk', qblk, k_all).astype(jnp.float32) * A_SCALE
        p = jax.nn.softmax(s, axis=-1).astype(v_all.dtype)
        return jnp.einsum('bhgqk,bhkd->bhgqd', p, v_all)

    o = lax.map(attend_block, qb)
    a_lat = o.transpose(1, 0, 4, 2, 3, 5).reshape(B, S, A_WIDTH)
    b_lat = pool_mixer(u, pool_w, pool_scale)
    y = jnp.concatenate([a_lat, b_lat], axis=-1) @ w_out
    if not ctx_out:
        return y, None
    Lc = hc.shape[1]
    qc_h = gqa_queries(qc, q_gain)
    s = jnp.einsum('bhgqd,bhkd->bhgqk', qc_h, kc_h).astype(jnp.float32) * A_SCALE
    p = jax.nn.softmax(s, axis=-1).astype(vc_h.dtype)
    a_ctx = jnp.einsum('bhgqk,bhkd->bhgqd', p, vc_h).transpose(0, 3, 1, 2, 4).reshape(B, Lc, A_WIDTH)
    b_ctx = pool_mixer(uc, pool_w, pool_scale)
    yc = jnp.concatenate([a_ctx, b_ctx], axis=-1) @ w_out
    return y, yc


def c_heads(t):
    B, L, _ = t.shape
    return t.reshape(B, L, C_HEADS, C_HEAD_DIM).transpose(0, 2, 1, 3)


def odd_mixer(h, hc, w_in, w_out, rel_bias, ctx_out):
    B, S, _ = h.shape
    rows = S // GRID_W
    kh = min(NA_ROWS_MAX, rows)
    q, k, v = jnp.split(h @ w_in, 3, axis=-1)
    if ctx_out:
        qc, kc, vc = jnp.split(hc @ w_in, 3, axis=-1)
    else:
        kc, vc = jnp.split(hc @ w_in[:, C_WIDTH:], 2, axis=-1)
    kc_h, vc_h = c_heads(kc), c_heads(vc)

    def grid(t):
        return t.reshape(B, rows, GRID_W, C_HEADS, C_HEAD_DIM).transpose(0, 3, 1, 2, 4)

    qg, kg, vg = grid(q), grid(k), grid(v)
    cols = jnp.arange(GRID_W, dtype=jnp.int32)
    col_start = jnp.clip(cols - NA_COLS // 2, 0, GRID_W - NA_COLS)
    col_idx = col_start[:, None] + jnp.arange(NA_COLS, dtype=jnp.int32)[None, :]
    col_off = col_idx - cols[:, None] + (NA_COLS - 1)
    n_loc = kh * NA_COLS

    def row_block(r):
        rs = jnp.clip(r - kh // 2, 0, rows - kh)
        qr = lax.dynamic_index_in_dim(qg, r, axis=2, keepdims=False)
        kr = lax.dynamic_slice_in_dim(kg, rs, kh, axis=2)
        vr = lax.dynamic_slice_in_dim(vg, rs, kh, axis=2)
        kw = kr[:, :, :, col_idx]
        vw = vr[:, :, :, col_idx]
        row_off = rs + jnp.arange(kh, dtype=jnp.int32) - r + (NA_ROWS_MAX - 1)
        bias = rel_bias[:, row_off][:, :, col_off].transpose(0, 2, 1, 3)
        s_loc = jnp.einsum('bhqd,bhiqjd->bhqij', qr, kw).astype(jnp.float32) * C_SCALE + bias.astype(jnp.float32)
        s_ctx = jnp.einsum('bhqd,bhkd->bhqk', qr, kc_h).astype(jnp.float32) * C_SCALE
        s = jnp.concatenate([s_loc.reshape(B, C_HEADS, GRID_W, n_loc), s_ctx], axis=-1)
        p = jax.nn.softmax(s, axis=-1).astype(vw.dtype)
        p_loc = p[..., :n_loc].reshape(B, C_HEADS, GRID_W, kh, NA_COLS)
        return (jnp.einsum('bhqij,bhiqjd->bhqd', p_loc, vw)
                + jnp.einsum('bhqk,bhkd->bhqd', p[..., n_loc:], vc_h))

    o = lax.map(row_block, jnp.arange(rows, dtype=jnp.int32))
    y = o.transpose(1, 0, 3, 2, 4).reshape(B, S, C_WIDTH) @ w_out
    if not ctx_out:
        return y, None
    Lc = hc.shape[1]
    qc_h = c_heads(qc)
    s = jnp.einsum('bhqd,bhkd->bhqk', qc_h, kc_h).astype(jnp.float32) * C_SCALE
    p = jax.nn.softmax(s, axis=-1).astype(vc_h.dtype)
    yc = jnp.einsum('bhqk,bhkd->bhqd', p, vc_h).transpose(0, 2, 1, 3).reshape(B, Lc, C_WIDTH) @ w_out
    return y, yc


def hier_moe(xt, wg, bg, we, be, w1, w3, w2):
    T, D = xt.shape
    xf = xt.astype(jnp.float32)
    g_logits = xf @ wg.astype(jnp.float32) + bg.astype(jnp.float32)
    g_prob = jax.nn.softmax(g_logits, axis=-1)
    g_idx = jnp.argmax(g_logits, axis=-1)
    g_w = jnp.take_along_axis(g_prob, g_idx[:, None], axis=1)[:, 0]
    e_logits = (xf @ we.astype(jnp.float32) + be.astype(jnp.float32)).reshape(T, N_GROUPS, E_PER_GROUP)
    e_logits = jnp.take_along_axis(e_logits, g_idx[:, None, None], axis=1)[:, 0]
    top_p, top_e = lax.top_k(jax.nn.softmax(e_logits, axis=-1), TOP_K)
    top_p = top_p / jnp.sum(top_p, axis=-1, keepdims=True)
    weights = (g_w[:, None] * top_p).reshape(-1)
    flat_e = (g_idx[:, None] * E_PER_GROUP + top_e).reshape(-1)
    n_assign = T * TOP_K
    order = jnp.argsort(flat_e)
    sorted_e = flat_e[order]
    tok = order // TOP_K
    counts = jnp.bincount(flat_e, length=N_EXPERTS)
    start = jnp.cumsum(counts) - counts
    padded = (counts + MOE_BLOCK - 1) // MOE_BLOCK * MOE_BLOCK
    pend = jnp.cumsum(padded)
    pstart = pend - padded
    dest = pstart[sorted_e] + jnp.arange(n_assign, dtype=jnp.int32) - start[sorted_e]
    n_blocks = -(-n_assign // MOE_BLOCK) + N_EXPERTS
    xpad = jnp.zeros((n_blocks * MOE_BLOCK, D), xt.dtype).at[dest].set(xt[tok])
    block_e = jnp.minimum(jnp.searchsorted(pend, jnp.arange(n_blocks, dtype=jnp.int32) * MOE_BLOCK, side='right'), N_EXPERTS - 1)

    def expert_block(args):
        xb, e = args
        hb = jax.nn.silu(xb @ w1[e]) * (xb @ w3[e])
        return hb @ w2[e]

    ypad = lax.map(expert_block, (xpad.reshape(n_blocks, MOE_BLOCK, D), block_e))
    y = ypad.reshape(-1, D)[dest] * weights[order][:, None].astype(xt.dtype)
    return jnp.zeros_like(xt).at[tok].add(y)


def setup_inputs(seed: int = 0) -> dict:
    key = jax.random.key(seed)
    ks = iter(jax.random.split(key, 32))
    n_even = (DEPTH + 1) // 2
    n_odd = DEPTH // 2
    D = D_MODEL

    def nrm(shape, scale):
        return jax.random.normal(next(ks), shape, jnp.float32) * scale

    return {
        'x': nrm((BATCH, SEQ, D), 1.0),
        'c': nrm((BATCH, D), 1.0),
        'ctx': nrm((BATCH, CTX_LEN, D), 1.0),
        'c_ctx': nrm((D,), 1.0),
        'ada_w': nrm((DEPTH, D, N_MOD * D), 0.5 * D ** -0.5),
        'ada_b': nrm((DEPTH, N_MOD * D), 0.02),
        'norm_mix_g': 1.0 + nrm((DEPTH, D), 0.1),
        'norm_ffn_g': 1.0 + nrm((DEPTH, D), 0.1),
        'even_w_in': nrm((n_even, D, EVEN_IN), D ** -0.5),
        'even_w_out': nrm((n_even, EVEN_OUT, D), EVEN_OUT ** -0.5),
        'a_q_gain': 1.0 + nrm((n_even, A_HEAD_DIM), 0.1),
        'a_k_gain': 1.0 + nrm((n_even, A_HEAD_DIM), 0.1),
        'pool_w': nrm((n_even, B_GROUPS, B_GROUP_DIM, B_GROUP_DIM), B_GROUP_DIM ** -0.5),
        'pool_scale': 1.0 + nrm((n_even, B_WIDTH), 0.1),
        'odd_w_in': nrm((n_odd, D, 3 * C_WIDTH), D ** -0.5),
        'odd_w_out': nrm((n_odd, C_WIDTH, D), C_WIDTH ** -0.5),
        'na_rel_bias': nrm((n_odd, C_HEADS, 2 * NA_ROWS_MAX - 1, 2 * NA_COLS - 1), 0.5),
        'moe_w_group': nrm((DEPTH, D, N_GROUPS), D ** -0.5),
        'moe_b_group': nrm((DEPTH, N_GROUPS), 0.01),
        'moe_w_expert': nrm((DEPTH, D, N_EXPERTS), D ** -0.5),
        'moe_b_expert': nrm((DEPTH, N_EXPERTS), 0.01),
        'moe_w1': nrm((DEPTH, N_EXPERTS, D, D_EXPERT), D ** -0.5),
        'moe_w3': nrm((DEPTH, N_EXPERTS, D, D_EXPERT), D ** -0.5),
        'moe_w2': nrm((DEPTH, N_EXPERTS, D_EXPERT, D), D_EXPERT ** -0.5),
        'final_g': 1.0 + nrm((D,), 0.1),
    }


def reference(x, c, ctx, c_ctx, ada_w, ada_b, norm_mix_g, norm_ffn_g, even_w_in, even_w_out, a_q_gain, a_k_gain,
              pool_w, pool_scale, odd_w_in, odd_w_out, na_rel_bias, moe_w_group, moe_b_group, moe_w_expert,
              moe_b_expert, moe_w1, moe_w3, moe_w2, final_g):
    B, S, D = x.shape
    Lc = ctx.shape[1]
    cos, sin = axial_rope_tables(S)
    for l in range(DEPTH):
        last = l == DEPTH - 1
        mod = jax.nn.silu(c) @ ada_w[l] + ada_b[l]
        mod_c = jax.nn.silu(c_ctx) @ ada_w[l] + ada_b[l]
        sh1, sc1, g1, sh2, sc2, g2 = jnp.split(mod[:, None, :], N_MOD, axis=-1)
        csh1, csc1, cg1, csh2, csc2, cg2 = jnp.split(mod_c, N_MOD, axis=-1)
        h = modulate(rmsnorm(x, norm_mix_g[l]), sh1, sc1)
        hc = modulate(rmsnorm(ctx, norm_mix_g[l]), csh1, csc1)
        if l % 2 == 0:
            i = l // 2
            y, yc = even_mixer(h, hc, even_w_in[i], even_w_out[i], a_q_gain[i], a_k_gain[i], pool_w[i],
                               pool_scale[i], cos, sin, not last)
        else:
            i = l // 2
            y, yc = odd_mixer(h, hc, odd_w_in[i], odd_w_out[i], na_rel_bias[i], not last)
        x = x + g1 * y
        h2 = modulate(rmsnorm(x, norm_ffn_g[l]), sh2, sc2)
        moe_args = (moe_w_group[l], moe_b_group[l], moe_w_expert[l], moe_b_expert[l], moe_w1[l], moe_w3[l], moe_w2[l])
        if not last:
            ctx = ctx + cg1 * yc
            hc2 = modulate(rmsnorm(ctx, norm_ffn_g[l]), csh2, csc2)
            f = hier_moe(jnp.concatenate([h2.reshape(B * S, D), hc2.reshape(B * Lc, D)], axis=0), *moe_args)
            x = x + g2 * f[:B * S].reshape(B, S, D)
            ctx = ctx + cg2 * f[B * S:].reshape(B, Lc, D)
        else:
            f = hier_moe(h2.reshape(B * S, D), *moe_args)
            x = x + g2 * f.reshape(B, S, D)
    return rmsnorm(x, final_g)
```

```python
import numpy as np
import concourse.bass as bass
import concourse.mybir as mybir
from concourse.bass_utils import run_bass_kernel_spmd
from contextlib import ExitStack

F32 = mybir.dt.float32
BF16 = mybir.dt.bfloat16
AF = mybir.ActivationFunctionType
ALU = mybir.AluOpType
AX = mybir.AxisListType

COMPUTE = ("tensor", "vector", "scalar", "gpsimd")
ENGS = COMPUTE + ("sync",)
NDMASEM = 8

D = 1024
KC = 8
S = 4096
LC = 256
T = S + LC
EPS = 1e-6
NEXP = 32
DE = 512
GRID = 64
NEG = -30000.0


class Op:
    __slots__ = ("eng", "fn", "waits", "signal", "idx", "dma", "dsem", "dval")

    def __init__(self, eng, fn, dma):
        self.eng = eng
        self.fn = fn
        self.waits = []
        self.signal = False
        self.idx = None
        self.dma = dma
        self.dsem = None
        self.dval = None


class Prog:
    def __init__(self, nc):
        self.nc = nc
        self.es = ExitStack()
        self.queues = {e: [] for e in ENGS}
        self.lastw = {}
        self.readers = {}
        self.dma_ops = {e: [] for e in ENGS}
        self.n_tensors = 0
        self.fence = []
        self.fence_pending = {e: False for e in ENGS}

    def sb(self, shape, dt, name=None):
        self.n_tensors += 1
        return self.es.enter_context(self.nc.sbuf_tensor(f"sb{self.n_tensors}_{name or ''}", list(shape), dt))

    def ps(self, shape, dt=F32, name=None):
        self.n_tensors += 1
        return self.es.enter_context(self.nc.psum_tensor(f"ps{self.n_tensors}_{name or ''}", list(shape), dt))

    def scope(self):
        return Scope(self)

    def op(self, eng, fn, reads=(), writes=(), dma=False):
        o = Op(eng, fn, dma)
        deps = []
        for k in reads:
            w = self.lastw.get(k)
            if w is not None:
                deps.append(w)
        for k in writes:
            w = self.lastw.get(k)
            if w is not None:
                deps.append(w)
            deps.extend(self.readers.get(k, ()))
        seen = set()
        for d in deps:
            if id(d) in seen or d is o:
                continue
            seen.add(id(d))
            if (not d.dma) and (not dma) and d.eng == eng and eng == "tensor":
                continue
            o.waits.append(d)
            d.signal = True
        if self.fence_pending[eng]:
            for d in self.fence:
                if d is not o and d not in o.waits:
                    o.waits.append(d)
                    d.signal = True
            self.fence_pending[eng] = False
        if dma:
            lst = self.dma_ops[eng]
            if len(lst) >= NDMASEM:
                prev = lst[len(lst) - NDMASEM]
                if prev not in o.waits:
                    o.waits.append(prev)
            lst.append(o)
            o.signal = True
        for k in reads:
            self.readers.setdefault(k, []).append(o)
        for k in writes:
            self.lastw[k] = o
            self.readers[k] = []
        self.queues[eng].append(o)
        return o

    def barrier(self):
        fence = []
        for e in ENGS:
            for o in reversed(self.queues[e]):
                if not o.dma:
                    fence.append(o)
                    break
            fence.extend(self.dma_ops[e][-NDMASEM:])
        self.fence = fence
        self.fence_pending = {e: True for e in ENGS}

    def emit(self):
        nc = self.nc
        es = self.es
        csem = {e: es.enter_context(nc.semaphore(f"c_{e}")) for e in ENGS}
        dsem = {e: [es.enter_context(nc.semaphore(f"d_{e}{i}")) for i in range(NDMASEM)]
                for e in ENGS if self.dma_ops[e]}
        for e in ENGS:
            cnt = 0
            dcnt = [0] * NDMASEM
            n = 0
            for o in self.queues[e]:
                if o.dma:
                    slot = n % NDMASEM
                    n += 1
                    dcnt[slot] += 16
                    o.dsem = dsem[e][slot]
                    o.dval = dcnt[slot]
                elif o.signal:
                    cnt += 1
                    o.idx = cnt
        block = es.enter_context(nc.Block())
        queues = self.queues
        dma_ops = self.dma_ops

        def run(engname):
            def body(eng):
                seen = {}
                for o in queues[engname]:
                    for d in o.waits:
                        if d.dma:
                            sem, val = d.dsem, d.dval
                        else:
                            sem, val = csem[d.eng], d.idx
                        key = id(sem)
                        if seen.get(key, -1) >= val:
                            continue
                        seen[key] = val
                        eng.wait_ge(sem, val)
                    ins = o.fn(eng)
                    if o.dma:
                        ins.then_inc(o.dsem, 16)
                    elif o.signal:
                        ins.then_inc(csem[engname], 1)
                for d in dma_ops[engname][-NDMASEM:]:
                    if seen.get(id(d.dsem), -1) < d.dval:
                        seen[id(d.dsem)] = d.dval
                        eng.wait_ge(d.dsem, d.dval)
            return body

        block.tensor(run("tensor"))
        block.vector(run("vector"))
        block.scalar(run("scalar"))
        block.gpsimd(run("gpsimd"))
        block.sync(run("sync"))

    def close(self):
        self.es.close()

    def mm(self, out, lhsT, rhs, start, stop, reads, writes):
        return self.op("tensor", lambda e: e.matmul(out, lhsT, rhs, start=start, stop=stop), reads, writes)

    def act(self, out, in_, func, reads, writes, bias=None, scale=None, accum_out=None):
        kw = {}
        if bias is not None:
            kw["bias"] = bias
        if scale is not None:
            kw["scale"] = scale
        if accum_out is not None:
            kw["accum_out"] = accum_out
        return self.op("scalar", lambda e: e.activation(out=out, in_=in_, func=func, **kw), reads, writes)

    def tt(self, eng, out, in0, in1, op, reads, writes):
        return self.op(eng, lambda e: e.tensor_tensor(out, in0, in1, op), reads, writes)

    def stt(self, eng, out, in0, scalar, in1, op0, op1, reads, writes):
        return self.op(eng, lambda e: e.scalar_tensor_tensor(out=out, in0=in0, scalar=scalar, in1=in1,
                                                             op0=op0, op1=op1), reads, writes)

    def ts(self, eng, out, in0, s1, s2, op0, op1, reads, writes):
        if op1 is None:
            return self.op(eng, lambda e: e.tensor_scalar(out, in0, s1, None, op0), reads, writes)
        return self.op(eng, lambda e: e.tensor_scalar(out, in0, s1, s2, op0, op1), reads, writes)

    def cp(self, eng, out, in_, reads, writes):
        if eng == "scalar":
            return self.op(eng, lambda e: e.copy(out, in_), reads, writes)
        return self.op(eng, lambda e: e.tensor_copy(out, in_), reads, writes)

    def recip(self, out, in_, reads, writes):
        return self.op("vector", lambda e: e.reciprocal(out, in_), reads, writes)

    def memset(self, eng, ap, val, writes):
        return self.op(eng, lambda e: e.memset(ap, val), (), writes)

    def rmax(self, out, in_, reads, writes):
        return self.op("vector", lambda e: e.reduce_max(out, in_, AX.X), reads, writes)

    def dma(self, out, in_, reads=(), writes=(), eng="sync"):
        return self.op(eng, lambda e: e.dma_start(out=out, in_=in_), reads, writes, dma=True)


class Scope:
    def __init__(self, P):
        self.P = P
        self.es = ExitStack()

    def __enter__(self):
        self.es.__enter__()
        return self

    def __exit__(self, *a):
        r = self.es.__exit__(*a)
        self.P.barrier()
        return r

    def sb(self, shape, dt, name=None):
        self.P.n_tensors += 1
        return self.es.enter_context(self.P.nc.sbuf_tensor(f"sb{self.P.n_tensors}_{name or ''}", list(shape), dt))

    def ps(self, shape, dt=F32, name=None):
        self.P.n_tensors += 1
        return self.es.enter_context(self.P.nc.psum_tensor(f"ps{self.P.n_tensors}_{name or ''}", list(shape), dt))

    def ring(self, n, shape, dt, tag, psum=False):
        tiles = [(self.ps(shape, dt) if psum else self.sb(shape, dt)) for _ in range(n)]
        return Ring(tiles, tag)


class Ring:
    def __init__(self, tiles, tag):
        self.tiles = tiles
        self.tag = tag
        self.i = 0

    def next(self):
        j = self.i % len(self.tiles)
        self.i += 1
        return self.tiles[j], (self.tag, j)


class Builder:
    def __init__(self, nc, layers=(0, 1), moe=True, dbg=()):
        self.nc = nc
        self.P = Prog(nc)
        self.layers = layers
        self.moe = moe
        self.dbg = dbg
        self.inputs = {}
        self.outputs = {}

    def din(self, name, shape, dt=F32):
        t = self.nc.dram_tensor(name, list(shape), dt, kind="ExternalInput").ap()
        self.inputs[name] = t
        return t

    def dout(self, name, shape, dt=F32):
        t = self.nc.dram_tensor(name, list(shape), dt, kind="ExternalOutput").ap()
        self.outputs[name] = t
        return t

    def dscratch(self, name, shape, dt=F32):
        return self.nc.dram_tensor(name, list(shape), dt).ap()

    def build(self):
        P = self.P
        self.xin = self.din("xin", [D, T])
        self.cc = self.din("cc", [128, KC, 2])
        self.ada_w = self.din("ada_w", [2, D, 6 * D])
        self.adab = self.din("adab", [128, 2, 48])
        self.gmix = self.din("gmix", [128, 2, KC])
        self.gffn = self.din("gffn", [128, 2, KC])
        self.gfin = self.din("gfin", [128, KC])
        self.ident_d = self.din("ident", [128, 128])
        if 0 in self.layers:
            self.e_win = self.din("e_win", [D, 1280])
            self.e_wout = self.din("e_wout", [D, D])
            self.gqk = self.din("gqk", [128, 2])
            self.perm_d = self.din("perm", [128, 128])
            self.ropeC = self.din("ropeC", [128, T])
            self.ropeS = self.din("ropeS", [128, T])
            self.pool_w = self.din("pool_w", [4, 128, 128])
            self.pool_s = self.din("pool_s", [128, 4])
            self.pool_ic = self.din("pool_ic", [128, 4, 2, 8])
        if 1 in self.layers:
            self.o_win = self.din("o_win", [D, 3 * D])
            self.o_wout = self.din("o_wout", [D, D])
            self.nab = self.din("nab", [16, 128, 14, 256])
        self.wr = self.din("wr", [2, D, 36])
        self.br = self.din("br", [128, 2, 36])
        if self.moe:
            self.sel_d = self.din("sel", [32, NEXP * 128])
            self.w1 = self.din("moe_w1", [2, NEXP, D, DE])
            self.w3 = self.din("moe_w3", [2, NEXP, D, DE])
            self.w2 = self.din("moe_w2", [2, NEXP, DE, D])
        self.outT = self.dout("outT", [D, S])
        self.xres = self.dscratch("xres", [D, T])
        self.h2s = self.dscratch("h2s", [D, T], BF16)
        self.aTs = self.dscratch("aTs", [D, T], BF16)
        self.us = self.dscratch("us", [512, T])
        self.wts = self.dscratch("wts", [32, T])

        self.ident = P.sb([128, 128], F32, "ident")
        P.dma(self.ident[:], self.ident_d, writes=["ident"])
        self.ones_all = P.sb([128, 128], BF16, "ones_all")
        P.memset("vector", self.ones_all[:], 1.0, ["ones_all"])
        self.ones_bd = P.sb([128, 128], BF16, "ones_bd")
        P.memset("vector", self.ones_bd[:], 0.0, ["ones_bd"])
        P.memset("vector", self.ones_bd[0:64, 0:64], 1.0, ["ones_bd"])
        P.memset("vector", self.ones_bd[64:128, 64:128], 1.0, ["ones_bd"])
        self.mod = P.sb([128, 2, 48, 2], F32, "mod")
        self.S1 = P.sb([128, 2, KC, 2], F32, "S1")
        self.S2 = P.sb([128, 2, KC, 2], F32, "S2")
        self.gfin_sb = P.sb([128, KC], F32, "gfin_sb")
        P.dma(self.gfin_sb[:], self.gfin, writes=["gfin"])

        self.eps_ap()
        self.phase_mod()
        if 0 not in self.layers:
            xri = self.din("xres_in", [D, T])
            with P.scope() as sc:
                r = sc.ring(2, [128, T], F32, "xri")
                for r0 in range(0, D, 128):
                    t, k = r.next()
                    P.dma(t[:], xri[r0:r0 + 128, :], writes=[k])
                    P.dma(self.xres[r0:r0 + 128, :], t[:], reads=[k], writes=["xres"])
        if 0 in self.layers:
            self.layer0_front()
            self.dump("a0", self.aTs, D, T, BF16, "aTs")
            self.phase_post(0)
            self.dump("xp0", self.xres, D, T, F32, "xres")
            self.dump("h20", self.h2s, D, T, BF16, "h2s")
            self.dump("wt0", self.wts, 32, T, F32, "wts")
            if self.moe:
                self.phase_moe(0)
                self.dump("x0", self.xres, D, T, F32, "xres")
        if 1 in self.layers:
            self.layer1_front()
            self.dump("a1", self.aTs, D, T, BF16, "aTs")
            self.phase_post(1)
            self.dump("xp1", self.xres, D, T, F32, "xres")
            self.dump("h21", self.h2s, D, T, BF16, "h2s")
            self.dump("wt1", self.wts, 32, T, F32, "wts")
            if self.moe:
                self.phase_moe(1)
        P.emit()

    def dump(self, tag, src, R, C, dt, key):
        if tag not in self.dbg:
            return
        P = self.P
        o = self.dout("dbg_" + tag, [R, C], dt)
        with P.scope() as sc:
            r = sc.ring(2, [128, C], dt, "dump_" + tag)
            for r0 in range(0, R, 128):
                n = min(128, R - r0)
                t, k = r.next()
                P.dma(t[0:n, :], src[r0:r0 + n, :], reads=[key], writes=[k])
                P.dma(o[r0:r0 + n, :], t[0:n, :], reads=[k])

    def phase_mod(self):
        P = self.P
        with P.scope() as sc:
            cc = sc.sb([128, KC, 2], F32)
            cs = sc.sb([128, KC, 2], F32)
            adab = sc.sb([128, 2, 48], F32)
            gm = sc.sb([128, 2, KC], F32)
            gf = sc.sb([128, 2, KC], F32)
            P.dma(cc[:], self.cc, writes=["cc"])
            P.dma(adab[:], self.adab, writes=["adab"])
            P.dma(gm[:], self.gmix, writes=["gm"])
            P.dma(gf[:], self.gffn, writes=["gf"])
            P.act(cs[:], cc[:], AF.Silu, ["cc"], ["cs"])
            wring = sc.ring(2, [128, KC, 768], F32, "adaw")
            pm = sc.ps([128, 48, 2], F32)
            for l in self.layers:
                awl = self.ada_w[l].rearrange("(k p) n -> p k n", p=128)
                for blk in range(8):
                    wt, wk = wring.next()
                    P.dma(wt[:], awl[:, :, blk * 768:(blk + 1) * 768], writes=[wk])
                    for j in range(6):
                        oc = blk * 6 + j
                        for k in range(KC):
                            P.mm(pm[:, oc, :], wt[:, k, j * 128:(j + 1) * 128], cs[:, k, :],
                                 k == 0, k == KC - 1, [wk, "cs"], ["pm"])
                P.tt("vector", self.mod[:, l], pm[:], adab[:, l].unsqueeze(2).to_broadcast([128, 48, 2]),
                     ALU.add, ["pm", "adab"], ["mod"])
                P.stt("vector", self.S1[:, l], self.mod[:, l, 8:16, :], 1.0,
                      gm[:, l].unsqueeze(2).to_broadcast([128, KC, 2]), ALU.add, ALU.mult,
                      ["mod", "gm"], ["S1"])
                P.stt("vector", self.S2[:, l], self.mod[:, l, 32:40, :], 1.0,
                      gf[:, l].unsqueeze(2).to_broadcast([128, KC, 2]), ALU.add, ALU.mult,
                      ["mod", "gf"], ["S2"])
            if "mod" in self.dbg:
                o = self.dout("dbg_mod", [128, 2 * 48 * 2])
                P.dma(o, self.mod[:].rearrange("p a b c -> p (a b c)"), reads=["mod"])

    def mvec(self, l, kind, k, s):
        return self.mod[:, l, kind * 8 + k, s:s + 1]

    def load_cast(self, sc_ring, dst, src, shape, dkeys, eng="gpsimd"):
        P = self.P
        st, sk = sc_ring.next()
        view = st
        idx = tuple(slice(0, s) for s in shape)
        P.dma(view[idx], src, writes=[sk])
        P.cp(eng, dst, view[idx], [sk], dkeys)

    def rmsnorm_mod(self, sc, xt, xk, N, Svec, shk_l_kind, s, l, out_bf, out_bf_key, out_f32=None, out_f32_key=None,
                    rings=None):
        P = self.P
        xsq, xsqk = rings["xsq"].next()
        P.act(xsq[:, :, :N], xt[:, :, :N], AF.Square, [xk], [xsqk])
        ssp, sspk = rings["ssp"].next()
        for k in range(KC):
            P.mm(ssp[:, :N], self.ones_all[:], xsq[:, k, :N], k == 0, k == KC - 1, [xsqk, "ones_all"], [sspk])
        sq, sqk = rings["f32a"].next()
        P.act(sq[:, :N], ssp[:, :N], AF.Sqrt, [sspk], [sqk], bias=self.eps_ap(), scale=1.0 / D)
        rstd, rk = rings["f32b"].next()
        P.recip(rstd[:, :N], sq[:, :N], [sqk], [rk])
        for k in range(KC):
            tmp, tk = rings["f32c"].next()
            P.stt("vector", tmp[:, :N], xt[:, k, :N], Svec[:, l, k, s:s + 1], rstd[:, :N], ALU.mult, ALU.mult,
                  [xk, rk, "S1", "S2"], [tk])
            sh = self.mvec(l, shk_l_kind, k, s)
            if out_f32 is not None:
                P.act(out_f32[:, k, :N], tmp[:, :N], AF.Identity, [tk, "mod"], [out_f32_key], bias=sh, scale=1.0)
                P.cp("gpsimd", out_bf[:, k, :N], out_f32[:, k, :N], [out_f32_key], [out_bf_key])
            else:
                P.act(out_bf[:, k, :N], tmp[:, :N], AF.Identity, [tk, "mod"], [out_bf_key], bias=sh, scale=1.0)

    def eps_ap(self):
        if not hasattr(self, "_eps"):
            self._eps = self.P.sb([128, 1], F32, "eps")
            self.P.memset("vector", self._eps[:], EPS, ["eps"])
        return self._eps[:]

    def layer0_front(self):
        P = self.P
        l = 0
        TN = 256
        ntile = T // TN
        xin_v = self.xin.rearrange("(k p) t -> p k t", p=128)
        with P.scope() as sq_scope:
            QT = sq_scope.sb([128, 4, T], BF16, "QT")
            K2T = sq_scope.sb([128, 2, T], BF16, "K2T")
            Vaug = sq_scope.sb([128, 2, T // 128, 192], BF16, "Vaug")
            P.memset("gpsimd", Vaug[:, :, :, 64:128], 1.0, ["Vaug"])
            with P.scope() as sc:
                stg = sc.ring(2, [128, KC, 256], F32, "stg")
                Win = sc.sb([128, KC, 1280], BF16, "Win")
                Wk2 = sc.sb([128, KC, 256], BF16, "Wk2")
                ewv = self.e_win.rearrange("(k p) n -> p k n", p=128)
                for j in range(5):
                    self.load_cast(stg, Win[:, :, j * 256:(j + 1) * 256], ewv[:, :, j * 256:(j + 1) * 256],
                                   [128, KC, 256], ["Win"])
                for g in range(2):
                    for h in range(2):
                        P.cp("gpsimd", Wk2[:, :, g * 128 + h * 64: g * 128 + h * 64 + 64],
                             Win[:, :, 512 + g * 64: 512 + g * 64 + 64], ["Win"], ["Wk2"])
                gqk = sc.sb([128, 2], F32, "gqk")
                P.dma(gqk[:], self.gqk, writes=["gqk"])
                perm = sc.sb([128, 128], F32, "perm")
                P.dma(perm[:], self.perm_d, writes=["perm"])
                rings = {
                    "xsq": sc.ring(1, [128, KC, TN], BF16, "xsq"),
                    "ssp": sc.ring(1, [128, TN], F32, "ssp", psum=True),
                    "f32a": sc.ring(2, [128, TN], F32, "f32a"),
                    "f32b": sc.ring(2, [128, TN], F32, "f32b"),
                    "f32c": sc.ring(3, [128, TN], F32, "f32c"),
                }
                xring = sc.ring(2, [128, KC, TN], F32, "xt")
                hring = sc.ring(2, [128, KC, TN], BF16, "hT")
                cring = sc.ring(2, [128, TN], F32, "ropeC")
                sring = sc.ring(2, [128, TN], F32, "ropeS")
                pp = sc.ring(3, [128, TN], F32, "pp", psum=True)
                pq = sc.ring(1, [128, TN], F32, "pq", psum=True)
                pr = sc.ring(1, [128, TN], F32, "pr", psum=True)
                pv = sc.ring(1, [128, 128], F32, "pv", psum=True)
                qsqr = sc.ring(2, [128, TN], BF16, "qsq")
                qnr = sc.ring(2, [128, TN], F32, "qn")
                t1r = sc.ring(2, [128, TN], F32, "t1")
                t2r = sc.ring(2, [128, TN], F32, "t2")
                ur = sc.ring(2, [128, TN], F32, "ust")
                def load0(ti):
                    c0 = ti * TN
                    xt, xk = xring.next()
                    P.dma(xt[:], xin_v[:, :, c0:c0 + TN], writes=[xk])
                    ct, ck = cring.next()
                    st_, sk_ = sring.next()
                    P.dma(ct[:], self.ropeC[:, c0:c0 + TN], writes=[ck])
                    P.dma(st_[:], self.ropeS[:, c0:c0 + TN], writes=[sk_])
                    return xt, xk, ct, ck, st_, sk_
                nxt = load0(0)
                for ti in range(ntile):
                    c0 = ti * TN
                    s = 1 if ti == 0 else 0
                    xt, xk, ct, ck, st_, sk_ = nxt
                    if ti + 1 < ntile:
                        nxt = load0(ti + 1)
                    hT, hk = hring.next()
                    self.rmsnorm_mod(sc, xt, xk, TN, self.S1, 0, s, l, hT, hk, rings=rings)
                    for oc in range(6):
                        ps_, pk_ = pp.next()
                        for k in range(KC):
                            if oc < 4:
                                w = Win[:, k, oc * 128:(oc + 1) * 128]
                                wkey = "Win"
                            else:
                                w = Wk2[:, k, (oc - 4) * 128:(oc - 3) * 128]
                                wkey = "Wk2"
                            P.mm(ps_[:], w, hT[:, k, :], k == 0, k == KC - 1, [wkey, hk], [pk_])
                        gain = gqk[:, 0:1] if oc < 4 else gqk[:, 1:2]
                        qsq, qsqk = qsqr.next()
                        P.act(qsq[:], ps_[:], AF.Square, [pk_], [qsqk])
                        ssq, ssqk = pq.next()
                        P.mm(ssq[:], self.ones_bd[:], qsq[:], True, True, [qsqk, "ones_bd"], [ssqk])
                        sq, sqk = rings["f32a"].next()
                        P.act(sq[:], ssq[:], AF.Sqrt, [ssqk], [sqk], bias=self.eps_ap(), scale=1.0 / 64)
                        rs, rsk = rings["f32b"].next()
                        P.recip(rs[:], sq[:], [sqk], [rsk])
                        qn, qnk = qnr.next()
                        P.stt("vector", qn[:], ps_[:], gain, rs[:], ALU.mult, ALU.mult, [pk_, rsk, "gqk"], [qnk])
                        prp, prk = pr.next()
                        P.mm(prp[:], perm[:], qn[:], True, True, [qnk, "perm"], [prk])
                        t1, t1k = t1r.next()
                        P.tt("gpsimd", t1[:], qn[:], ct[:], ALU.mult, [qnk, ck], [t1k])
                        t2, t2k = t2r.next()
                        P.tt("vector", t2[:], prp[:], st_[:], ALU.mult, [prk, sk_], [t2k])
                        if oc < 4:
                            dst, dk = QT[:, oc, c0:c0 + TN], "QT"
                        else:
                            dst, dk = K2T[:, oc - 4, c0:c0 + TN], "K2T"
                        P.tt("gpsimd", dst, t1[:], t2[:], ALU.add, [t1k, t2k], [dk])
                    for j in range(TN // 128):
                        vp, vk = pv.next()
                        for k in range(KC):
                            P.mm(vp[:], hT[:, k, j * 128:(j + 1) * 128], Win[:, k, 640:768], k == 0, k == KC - 1,
                                 [hk, "Win"], [vk])
                        tix = c0 // 128 + j
                        for g in range(2):
                            P.cp("scalar", Vaug[:, g, tix, 0:64], vp[:, g * 64:(g + 1) * 64], [vk], ["Vaug"])
                            P.cp("vector", Vaug[:, g, tix, 128:192], vp[:, g * 64:(g + 1) * 64], [vk], ["Vaug"])
                    for g in range(4):
                        ps_, pk_ = pp.next()
                        for k in range(KC):
                            P.mm(ps_[:], Win[:, k, 768 + g * 128: 768 + (g + 1) * 128], hT[:, k, :], k == 0,
                                 k == KC - 1, ["Win", hk], [pk_])
                        ut, uk = ur.next()
                        P.cp("scalar", ut[:], ps_[:], [pk_], [uk])
                        P.dma(self.us[g * 128:(g + 1) * 128, c0:c0 + TN], ut[:], reads=[uk], writes=["us"])
                if "qkv" in self.dbg:
                    o = self.dout("dbg_QT", [128, 4 * T], BF16)
                    P.dma(o, QT[:].rearrange("p a t -> p (a t)"), reads=["QT"])
                    o = self.dout("dbg_K2T", [128, 2 * T], BF16)
                    P.dma(o, K2T[:].rearrange("p a t -> p (a t)"), reads=["K2T"])
                    o = self.dout("dbg_V", [128, 2 * (T // 128) * 192], BF16)
                    P.dma(o, Vaug[:].rearrange("p a t d -> p (a t d)"), reads=["Vaug"])
            with P.scope() as sc:
                PADL = 16
                WT = PADL + LC + PADL + S + PADL
                OFFC = PADL
                OFFX = PADL + LC + PADL
                pooled = sc.sb([128, 4, T], BF16, "pooled")
                pw = sc.sb([128, 4, 128], BF16, "pw")
                pws = sc.sb([128, 4, 128], F32, "pws")
                P.dma(pws[:], self.pool_w.rearrange("g c d -> c g d"), writes=["pws"])
                P.cp("gpsimd", pw[:], pws[:], ["pws"], ["pw"])
                psc = sc.sb([128, 4], F32, "psc")
                P.dma(psc[:], self.pool_s, writes=["psc"])
                pic = sc.sb([128, 4, 2, 8], F32, "pic")
                P.dma(pic[:], self.pool_ic, writes=["pic"])
                with P.scope() as sc2:
                    U = sc2.sb([128, WT], F32, "U")
                    A = sc2.sb([128, WT], F32, "A")
                    B = sc2.sb([128, WT], F32, "B")
                    for g in range(4):
                        w = (2, 4, 8, 16)[g]
                        P.memset("vector", U[:], 0.0, ["U"])
                        P.dma(U[:, OFFC:OFFC + LC], self.us[g * 128:(g + 1) * 128, 0:LC], reads=["us"], writes=["U"])
                        P.dma(U[:, OFFX:OFFX + S], self.us[g * 128:(g + 1) * 128, LC:T], reads=["us"], writes=["U"])
                        lo, hi = 8, WT - 8
                        P.memset("gpsimd", A[:], 0.0, ["A"])
                        P.memset("gpsimd", B[:], 0.0, ["B"])
                        P.tt("vector", A[:, lo:hi], U[:, lo - 1:hi - 1], U[:, lo:hi], ALU.add, ["U"], ["A"])
                        cur, curk, oth, othk = A, "A", B, "B"
                        sh = 1
                        ww = 2
                        while ww < w:
                            d = ww // 2
                            P.tt("vector", oth[:, lo:hi], cur[:, lo - d:hi - d], cur[:, lo + d:hi + d], ALU.add,
                                 [curk], [othk])
                            cur, curk, oth, othk = oth, othk, cur, curk
                            ww *= 2
                        pl, plk = oth, othk
                        P.stt("vector", pl[:, lo:hi], cur[:, lo:hi], 1.0 / w, U[:, lo:hi], ALU.mult, ALU.subtract,
                              [curk, "U"], [plk])
                        for (off, L) in ((OFFC, LC), (OFFX, S)):
                            nl = w // 2
                            tmpb = sc2.sb([128, 8], F32)
                            P.tt("vector", tmpb[:, 0:nl], cur[:, off:off + nl], pic[:, g, 0, 0:nl], ALU.mult,
                                 [curk, "pic"], ["tmpb"])
                            P.tt("vector", pl[:, off:off + nl], tmpb[:, 0:nl], U[:, off:off + nl], ALU.subtract,
                                 ["tmpb", "U"], [plk])
                            nr = w // 2 - 1
                            if nr > 0:
                                r0 = off + L - nr
                                tmpc = sc2.sb([128, 8], F32)
                                P.tt("vector", tmpc[:, 0:nr], cur[:, r0:r0 + nr], pic[:, g, 1, 0:nr], ALU.mult,
                                     [curk, "pic"], ["tmpc"])
                                P.tt("vector", pl[:, r0:r0 + nr], tmpc[:, 0:nr], U[:, r0:r0 + nr], ALU.subtract,
                                     ["tmpc", "U"], [plk])
                        P.cp("gpsimd", pooled[:, g, 0:LC], pl[:, OFFC:OFFC + LC], [plk], ["pooled"])
                        P.cp("gpsimd", pooled[:, g, LC:T], pl[:, OFFX:OFFX + S], [plk], ["pooled"])
                self.attention0(QT, K2T, Vaug)
                with P.scope() as sc2:
                    pmm = sc2.ring(2, [128, 512], F32, "pmm", psum=True)
                    ob = sc2.ring(2, [128, 512], BF16, "pob")
                    for g in range(4):
                        for c0 in range(0, T, 512):
                            n = min(512, T - c0)
                            ps_, pk_ = pmm.next()
                            P.mm(ps_[:, :n], pw[:, g, :], pooled[:, g, c0:c0 + n], True, True, ["pw", "pooled"], [pk_])
                            o_, ok_ = ob.next()
                            P.act(o_[:, :n], ps_[:, :n], AF.Identity, [pk_, "psc"], [ok_], scale=psc[:, g:g + 1])
                            P.dma(self.aTs[(4 + g) * 128:(5 + g) * 128, c0:c0 + n], o_[:, :n], reads=[ok_],
                                  writes=["aTs"])

    def attention0(self, QT, K2T, Vaug):
        P = self.P
        A_SCALE = 64 ** -0.5
        with P.scope() as sc:
            sps = sc.ring(4, [128, 512], F32, "sps", psum=True)
            oA = sc.ring(2, [128, 512], F32, "oA", psum=True)
            oB = sc.ring(2, [128, 512], F32, "oB", psum=True)
            pt = sc.ring(4, [128, 512], BF16, "pt")
            Rr = sc.ring(2, [128, 512], F32, "R")
            ao = sc.ring(2, [128, 512], BF16, "ao")
            qtiles = [(0, LC, 2)] + [(LC + i * 512, 512, T // 128) for i in range(S // 512)]
            for c in range(4):
                g = c // 2
                for (q0, N, nkc) in qtiles:
                    a_, ak = oA.next()
                    b_, bk = oB.next()
                    iters = [(kc, h) for kc in range(nkc) for h in range(2)]
                    LA = 3
                    pend = {}

                    def emitS(i, g=g, c=c, q0=q0, N=N, iters=iters, pend=pend):
                        kc, h = iters[i]
                        hp = slice(h * 64, h * 64 + 64)
                        sp_, spk = sps.next()
                        P.mm(sp_[:, :N], K2T[hp, g, kc * 128:(kc + 1) * 128], QT[hp, c, q0:q0 + N], True, True,
                             ["K2T", "QT"], [spk])
                        pend[i] = (sp_, spk)

                    for i in range(min(LA, len(iters))):
                        emitS(i)
                    for i in range(len(iters)):
                        if i + LA < len(iters):
                            emitS(i + LA)
                        kc, h = iters[i]
                        sp_, spk = pend.pop(i)
                        p_, pk_ = pt.next()
                        P.act(p_[:, :N], sp_[:, :N], AF.Exp, [spk], [pk_], scale=A_SCALE)
                        if h == 0:
                            P.mm(a_[:, :N], Vaug[:, g, kc, 0:128], p_[:, :N], kc == 0, kc == nkc - 1,
                                 ["Vaug", pk_], [ak])
                        else:
                            P.mm(b_[:, :N], Vaug[:, g, kc, 64:192], p_[:, :N], kc == 0, kc == nkc - 1,
                                 ["Vaug", pk_], [bk])
                    R, Rk = Rr.next()
                    P.recip(R[0:64, :N], a_[64:128, :N], [ak], [Rk])
                    P.recip(R[64:128, :N], b_[0:64, :N], [bk], [Rk])
                    o_, ok_ = ao.next()
                    P.tt("vector", o_[0:64, :N], a_[0:64, :N], R[0:64, :N], ALU.mult, [ak, Rk], [ok_])
                    P.tt("vector", o_[64:128, :N], b_[64:128, :N], R[64:128, :N], ALU.mult, [bk, Rk], [ok_])
                    P.dma(self.aTs[c * 128:(c + 1) * 128, q0:q0 + N], o_[:, :N], reads=[ok_], writes=["aTs"])

    def layer1_front(self):
        P = self.P
        l = 1
        TN = 256
        ntile = T // TN
        xv = self.xres.rearrange("(k p) t -> p k t", p=128)
        C_SCALE = 64 ** -0.5
        with P.scope() as sc0:
            hT = sc0.sb([128, KC, T], BF16, "hT1")
            with P.scope() as sc:
                rings = {
                    "xsq": sc.ring(1, [128, KC, TN], BF16, "xsq"),
                    "ssp": sc.ring(1, [128, TN], F32, "ssp", psum=True),
                    "f32a": sc.ring(2, [128, TN], F32, "f32a"),
                    "f32b": sc.ring(2, [128, TN], F32, "f32b"),
                    "f32c": sc.ring(3, [128, TN], F32, "f32c"),
                }
                xring = sc.ring(2, [128, KC, TN], F32, "xt")
                hring = sc.ring(2, [128, KC, TN], BF16, "hTt")
                def load1(ti):
                    c0 = ti * TN
                    xt, xk = xring.next()
                    P.dma(xt[:], xv[:, :, c0:c0 + TN], reads=["xres"], writes=[xk])
                    return xt, xk
                nxt = load1(0)
                for ti in range(ntile):
                    c0 = ti * TN
                    s = 1 if ti == 0 else 0
                    xt, xk = nxt
                    if ti + 1 < ntile:
                        nxt = load1(ti + 1)
                    ht, hk = hring.next()
                    self.rmsnorm_mod(sc, xt, xk, TN, self.S1, 0, s, l, ht, hk, rings=rings)
                    P.cp("gpsimd", hT[:, :, c0:c0 + TN], ht[:], [hk], ["hT1"])
            if "h1" in self.dbg:
                o = self.dout("dbg_h1", [128, KC * T], BF16)
                P.dma(o, hT[:].rearrange("p a t -> p (a t)"), reads=["hT1"])
            with P.scope() as sc:
                owv = self.o_win.rearrange("(k p) n -> p k n", p=128)
                stg = sc.ring(2, [128, KC, 128], F32, "stg1")
                wq = sc.ring(2, [128, KC, 128], BF16, "wq")
                wk = sc.ring(2, [128, KC, 128], BF16, "wk")
                wv = sc.ring(2, [128, KC, 128], BF16, "wv")
                qT = sc.sb([128, S], BF16, "qT1")
                kT = sc.sb([128, T], BF16, "kT1")
                Vaug = sc.sb([128, T // 128, 192], BF16, "Vaug1")
                P.memset("gpsimd", Vaug[:, :, 64:128], 1.0, ["Vaug1"])
                bias = sc.sb([128, 2, 14, 256], F32, "nab")
                pp = sc.ring(2, [128, 512], F32, "pp1", psum=True)
                pv = sc.ring(1, [128, 128], F32, "pv1", psum=True)
                sps = sc.ring(3, [128, 256], F32, "sps1", psum=True)
                oA = sc.ring(1, [128, 256], F32, "oA1", psum=True)
                oB = sc.ring(1, [128, 256], F32, "oB1", psum=True)
                ssb = sc.ring(3, [128, 256], F32, "ssb1")
                pt = sc.ring(4, [128, 256], BF16, "pt1")
                Rr = sc.ring(2, [128, 256], F32, "R1")
                ao = sc.ring(2, [128, 256], BF16, "ao1")
                for c in range(8):
                    wq_, wqk = wq.next()
                    wk_, wkk = wk.next()
                    wv_, wvk = wv.next()
                    self.load_cast(stg, wq_[:], owv[:, :, c * 128:(c + 1) * 128], [128, KC, 128], [wqk])
                    self.load_cast(stg, wk_[:], owv[:, :, D + c * 128:D + (c + 1) * 128], [128, KC, 128], [wkk])
                    self.load_cast(stg, wv_[:], owv[:, :, 2 * D + c * 128:2 * D + (c + 1) * 128], [128, KC, 128], [wvk])
                    for hh in range(2):
                        P.dma(bias[:, hh], self.nab[2 * c + hh], writes=["nab"])
                    for c0 in range(0, S, 512):
                        ps_, pk_ = pp.next()
                        for k in range(KC):
                            P.mm(ps_[:], wq_[:, k, :], hT[:, k, LC + c0:LC + c0 + 512], k == 0, k == KC - 1,
                                 [wqk, "hT1"], [pk_])
                        P.act(qT[:, c0:c0 + 512], ps_[:], AF.Identity, [pk_], ["qT1"], scale=C_SCALE)
                    for c0 in range(0, T, 512):
                        n = min(512, T - c0)
                        ps_, pk_ = pp.next()
                        for k in range(KC):
                            P.mm(ps_[:, :n], wk_[:, k, :], hT[:, k, c0:c0 + n], k == 0, k == KC - 1,
                                 [wkk, "hT1"], [pk_])
                        P.cp("vector", kT[:, c0:c0 + n], ps_[:, :n], [pk_], ["kT1"])
                    for tix in range(T // 128):
                        vp, vk = pv.next()
                        for k in range(KC):
                            P.mm(vp[:], hT[:, k, tix * 128:(tix + 1) * 128], wv_[:, k, :], k == 0, k == KC - 1,
                                 ["hT1", wvk], [vk])
                        P.cp("scalar", Vaug[:, tix, 0:64], vp[:, 0:64], [vk], ["Vaug1"])
                        P.cp("vector", Vaug[:, tix, 128:192], vp[:, 64:128], [vk], ["Vaug1"])
                    for qb in range(16):
                        if qb == 0:
                            kr0, nloc, tb = 0, 4, 6
                        elif qb == 15:
                            kr0, nloc, tb = 56, 4, 10
                        else:
                            kr0, nloc, tb = 4 * qb - 4, 6, 0
                        chunks = [(LC + 64 * kr0 + 128 * j, tb + j) for j in range(nloc)] + [(0, None), (128, None)]
                        a_, ak = oA.next()
                        b_, bk = oB.next()
                        q0 = qb * 256
                        nch = len(chunks)
                        iters = [(ci, h) for ci in range(nch) for h in range(2)]
                        LA = 2
                        pend = {}

                        def emitS1(i, iters=iters, chunks=chunks, q0=q0, pend=pend):
                            ci, h = iters[i]
                            k0 = chunks[ci][0]
                            hp = slice(h * 64, h * 64 + 64)
                            sp_, spk = sps.next()
                            P.mm(sp_[:], kT[hp, k0:k0 + 128], qT[hp, q0:q0 + 256], True, True,
                                 ["kT1", "qT1"], [spk])
                            pend[i] = (sp_, spk)

                        for i in range(LA):
                            emitS1(i)
                        for i in range(len(iters)):
                            if i + LA < len(iters):
                                emitS1(i + LA)
                            ci, h = iters[i]
                            k0, bt = chunks[ci]
                            sp_, spk = pend.pop(i)
                            p_, pk_ = pt.next()
                            if bt is not None:
                                sb_, sbk = ssb.next()
                                P.tt("vector", sb_[:], sp_[:], bias[:, h, bt, :], ALU.add, [spk, "nab"], [sbk])
                                P.act(p_[:], sb_[:], AF.Exp, [sbk], [pk_])
                            else:
                                P.act(p_[:], sp_[:], AF.Exp, [spk], [pk_])
                            tix = k0 // 128
                            if h == 0:
                                P.mm(a_[:], Vaug[:, tix, 0:128], p_[:], ci == 0, ci == nch - 1, ["Vaug1", pk_], [ak])
                            else:
                                P.mm(b_[:], Vaug[:, tix, 64:192], p_[:], ci == 0, ci == nch - 1, ["Vaug1", pk_], [bk])
                        R, Rk = Rr.next()
                        P.recip(R[0:64, :], a_[64:128, :], [ak], [Rk])
                        P.recip(R[64:128, :], b_[0:64, :], [bk], [Rk])
                        o_, ok_ = ao.next()
                        P.tt("vector", o_[0:64, :], a_[0:64, :], R[0:64, :], ALU.mult, [ak, Rk], [ok_])
                        P.tt("vector", o_[64:128, :], b_[64:128, :], R[64:128, :], ALU.mult, [bk, Rk], [ok_])
                        P.dma(self.aTs[c * 128:(c + 1) * 128, LC + q0:LC + q0 + 256], o_[:], reads=[ok_],
                              writes=["aTs"])

    def phase_post(self, l):
        P = self.P
        TN = 256
        t0 = 0 if l == 0 else LC
        src = self.xin if l == 0 else self.xres
        srck = "xin" if l == 0 else "xres"
        xv = src.rearrange("(k p) t -> p k t", p=128)
        xo = self.xres.rearrange("(k p) t -> p k t", p=128)
        h2v = self.h2s.rearrange("(k p) t -> p k t", p=128)
        aTv = self.aTs.rearrange("(k p) t -> p k t", p=128)
        wout_d = (self.e_wout if l == 0 else self.o_wout).rearrange("(k p) n -> p k n", p=128)
        with P.scope() as sc:
            stg = sc.ring(2, [128, KC, 256], F32, "stgp")
            Wout = sc.sb([128, KC, D], BF16, "Wout")
            for j in range(4):
                self.load_cast(stg, Wout[:, :, j * 256:(j + 1) * 256], wout_d[:, :, j * 256:(j + 1) * 256],
                               [128, KC, 256], ["Wout"])
            wr = sc.sb([128, KC, 36], F32, "wr")
            P.dma(wr[:], self.wr[l].rearrange("(k p) n -> p k n", p=128), writes=["wr"])
            br = sc.sb([128, 36], F32, "br")
            P.dma(br[:], self.br[:, l, :], writes=["br"])
            rings = {
                "xsq": sc.ring(1, [128, KC, TN], BF16, "xsq"),
                "ssp": sc.ring(1, [128, TN], F32, "ssp", psum=True),
                "f32a": sc.ring(2, [128, TN], F32, "f32a"),
                "f32b": sc.ring(2, [128, TN], F32, "f32b"),
                "f32c": sc.ring(3, [128, TN], F32, "f32c"),
            }
            xring = sc.ring(2, [128, KC, TN], F32, "xtp")
            aring = sc.ring(2, [128, KC, TN], BF16, "aTt")
            xnring = sc.ring(2, [128, KC, TN], F32, "xn")
            hfring = sc.ring(1, [128, KC, TN], F32, "h2f")
            hbring = sc.ring(2, [128, KC, TN], BF16, "h2b")
            yp = sc.ring(3, [128, TN], F32, "yp", psum=True)
            lp = sc.ring(1, [128, 36], F32, "lp", psum=True)
            wtp = sc.ring(1, [32, 128], F32, "wtp", psum=True)
            wtr = sc.ring(2, [32, TN], F32, "wtr")
            sm = sc.ring(2, [128, 160], F32, "sm")
            def loadp(c0):
                xt, xk = xring.next()
                P.dma(xt[:], xv[:, :, c0:c0 + TN], reads=[srck], writes=[xk])
                at, atk = aring.next()
                P.dma(at[:], aTv[:, :, c0:c0 + TN], reads=["aTs"], writes=[atk])
                return xt, xk, at, atk
            nxt = loadp(t0)
            for c0 in range(t0, T, TN):
                s = 1 if c0 < LC else 0
                xt, xk, at, atk = nxt
                if c0 + TN < T:
                    nxt = loadp(c0 + TN)
                xn, xnk = xnring.next()
                for oc in range(KC):
                    ps_, pk_ = yp.next()
                    for k in range(KC):
                        P.mm(ps_[:], Wout[:, k, oc * 128:(oc + 1) * 128], at[:, k, :], k == 0, k == KC - 1,
                             ["Wout", atk], [pk_])
                    P.stt("vector", xn[:, oc, :], ps_[:], self.mvec(l, 2, oc, s), xt[:, oc, :], ALU.mult, ALU.add,
                          [pk_, xk, "mod"], [xnk])
                P.dma(xo[:, :, c0:c0 + TN], xn[:], reads=[xnk], writes=["xres"])
                hf, hfk = hfring.next()
                hb, hbk = hbring.next()
                self.rmsnorm_mod(sc, xn, xnk, TN, self.S2, 3, s, l, hb, hbk, out_f32=hf, out_f32_key=hfk, rings=rings)
                P.dma(h2v[:, :, c0:c0 + TN], hb[:], reads=[hbk], writes=["h2s"])
                wt_, wtk = wtr.next()
                for j in range(TN // 128):
                    lg, lgk = lp.next()
                    for k in range(KC):
                        P.mm(lg[:], hf[:, k, j * 128:(j + 1) * 128], wr[:, k, :], k == 0, k == KC - 1, [hfk, "wr"], [lgk])
                    m, mk = sm.next()
                    L = m[:, 0:36]
                    gmax = m[:, 36:37]
                    ngmax = m[:, 37:38]
                    gsum = m[:, 38:39]
                    gw = m[:, 39:40]
                    gmk = m[:, 40:44]
                    pen = m[:, 44:48]
                    Lm = m[:, 48:80]
                    mk1 = m[:, 80:112]
                    Lm2 = m[:, 112:144]
                    m1 = m[:, 144:145]
                    m2 = m[:, 145:146]
                    dd = m[:, 146:147]
                    ee = m[:, 147:148]
                    w1 = m[:, 148:149]
                    w2 = m[:, 149:150]
                    gex = m[:, 150:154]
                    rk = [mk]
                    P.tt("vector", L, lg[:], br[:], ALU.add, [lgk, "br"], rk)
                    P.rmax(gmax, m[:, 0:4], rk, rk)
                    P.ts("vector", ngmax, gmax, -1.0, None, ALU.mult, None, rk, rk)
                    P.ts("vector", gmk, m[:, 0:4], gmax, None, ALU.is_ge, None, rk, rk)
                    P.act(gex, m[:, 0:4], AF.Exp, rk, rk, bias=ngmax, scale=1.0, accum_out=gsum)
                    P.recip(gw, gsum, rk, rk)
                    P.ts("vector", pen, gmk, 1e30, -1e30, ALU.mult, ALU.add, rk, rk)
                    P.tt("vector", Lm.rearrange("p (g e) -> p g e", g=4), m[:, 4:36].rearrange("p (g e) -> p g e", g=4),
                         pen.unsqueeze(2).to_broadcast([128, 4, 8]), ALU.add, rk, rk)
                    P.rmax(m1, Lm, rk, rk)
                    P.ts("vector", mk1, Lm, m1, None, ALU.is_ge, None, rk, rk)
                    P.stt("vector", Lm2, mk1, -1e30, Lm, ALU.mult, ALU.add, rk, rk)
                    P.rmax(m2, Lm2, rk, rk)
                    mk2 = m[:, 48:80]
                    P.ts("vector", mk2, Lm2, m2, None, ALU.is_ge, None, rk, rk)
                    P.tt("vector", dd, m2, m1, ALU.subtract, rk, rk)
                    P.act(ee, dd, AF.Exp, rk, rk)
                    P.ts("vector", ee, ee, 1.0, None, ALU.add, None, rk, rk)
                    P.recip(ee, ee, rk, rk)
                    P.tt("vector", w1, gw, ee, ALU.mult, rk, rk)
                    P.tt("vector", w2, gw, w1, ALU.subtract, rk, rk)
                    wm = m[:, 112:144]
                    P.ts("vector", mk1, mk1, w1, None, ALU.mult, None, rk, rk)
                    P.stt("vector", wm, mk2, w2, mk1, ALU.mult, ALU.add, rk, rk)
                    tp, tpk = wtp.next()
                    P.mm(tp[:], wm, self.ident[:], True, True, rk + ["ident"], [tpk])
                    P.cp("scalar", wt_[:, j * 128:(j + 1) * 128], tp[:], [tpk], [wtk])
                P.dma(self.wts[:, c0:c0 + TN], wt_[:], reads=[wtk], writes=["wts"])
            if f"post{l}" in self.dbg:
                pass

    def phase_moe(self, l):
        P = self.P
        last = (l == 1)
        t0 = 0 if l == 0 else LC
        if l == 0:
            stiles = [[(0, 384), (384, 384), (768, 384)], [(0, 384), (384, 384), (768, 384)],
                      [(0, 512), (512, 512)], [(0, 512), (512, 512)]]
        else:
            stiles = [[(0, 512), (512, 512)]] * 4
        st_sizes = [sum(n for _, n in st) for st in stiles]
        STN = max(st_sizes)
        NT = 512
        xv = self.xres.rearrange("(k p) t -> p k t", p=128)
        h2v = self.h2s.rearrange("(k p) t -> p k t", p=128)
        outv = self.outT.rearrange("(k p) t -> p k t", p=128)
        w1v = self.w1[l].rearrange("e (k p) n -> e p k n", p=128)
        w3v = self.w3[l].rearrange("e (k p) n -> e p k n", p=128)
        w2v = self.w2[l].rearrange("e (k p) n -> e p k n", p=128)
        with P.scope() as sc:
            sel = sc.sb([32, NEXP * 128], F32, "sel")
            P.dma(sel[:], self.sel_d, writes=["sel"])
            yacc = sc.sb([128, KC, STN], F32, "yacc")
            h2 = sc.sb([128, KC, STN], BF16, "h2")
            wts = sc.sb([32, STN], F32, "wts_sb")
            W1b = sc.ring(2, [128, KC, DE], BF16, "W1b")
            W3b = sc.ring(2, [128, KC, DE], BF16, "W3b")
            W2b = sc.ring(2, [128, 4, D], BF16, "W2b")
            stg = sc.ring(2, [128, 4, DE], F32, "stgm")
            wbp = sc.ring(1, [128, NT], F32, "wbp", psum=True)
            ap_ = sc.ring(2, [128, NT], F32, "aps", psum=True)
            bp_ = sc.ring(2, [128, NT], F32, "bps", psum=True)
            ypr = sc.ring(3, [128, NT], F32, "ypm", psum=True)
            wbs = sc.ring(2, [128, NT], F32, "wbs")
            sar = sc.ring(2, [128, NT], F32, "sa")
            tr = sc.ring(2, [128, NT], F32, "tmoe")
            hbr = sc.ring(2, [128, 4, NT], BF16, "hb")
            xfr = sc.ring(1, [128, KC, NT], F32, "xf")
            xor_ = sc.ring(1, [128, KC, NT], F32, "xo")
            if last:
                rings = {
                    "xsq": sc.ring(1, [128, KC, NT], BF16, "xsq"),
                    "f32a": sc.ring(2, [128, NT], F32, "f32a"),
                    "f32b": sc.ring(2, [128, NT], F32, "f32b"),
                }
            cvt = [0]

            def load_expert(e):
                w1b, w1k = W1b.next()
                w3b, w3k = W3b.next()
                w2b, w2k = W2b.next()
                for half in range(2):
                    self.load_cast(stg, w1b[:, half * 4:(half + 1) * 4, :], w1v[e][:, half * 4:(half + 1) * 4, :],
                                   [128, 4, DE], [w1k], eng=("gpsimd" if cvt[0] % 2 == 0 else "scalar"))
                    cvt[0] += 1
                for half in range(2):
                    self.load_cast(stg, w3b[:, half * 4:(half + 1) * 4, :], w3v[e][:, half * 4:(half + 1) * 4, :],
                                   [128, 4, DE], [w3k], eng=("gpsimd" if cvt[0] % 2 == 0 else "scalar"))
                    cvt[0] += 1
                for half in range(2):
                    self.load_cast(stg, w2b[:, :, half * DE:(half + 1) * DE], w2v[e][:, :, half * DE:(half + 1) * DE],
                                   [128, 4, DE], [w2k], eng=("gpsimd" if cvt[0] % 2 == 0 else "scalar"))
                    cvt[0] += 1
                return w1b, w1k, w3b, w3k, w2b, w2k

            seq = [(st_, e_) for st_ in range(4) for e_ in range(NEXP)]
            loaded = {}

            def ensure_loaded(j):
                if j < len(seq) and j not in loaded:
                    loaded[j] = load_expert(seq[j][1])

            def emit_ab(W, e, c0, n):
                w1b, w1k, w3b, w3k, w2b, w2k = W
                wb, wbk = wbp.next()
                P.mm(wb[:, :n], sel[:, e * 128:(e + 1) * 128], wts[:, c0:c0 + n], True, True,
                     ["sel", "wts_sb"], [wbk])
                wb_s, wbsk = wbs.next()
                P.cp("scalar", wb_s[:, :n], wb[:, :n], [wbk], [wbsk])
                hb, hbk = hbr.next()
                for hc in range(4):
                    a_, ak = ap_.next()
                    b_, bk = bp_.next()
                    for k in range(KC):
                        P.mm(a_[:, :n], w1b[:, k, hc * 128:(hc + 1) * 128], h2[:, k, c0:c0 + n], k == 0,
                             k == KC - 1, [w1k, "h2"], [ak])
                    for k in range(KC):
                        P.mm(b_[:, :n], w3b[:, k, hc * 128:(hc + 1) * 128], h2[:, k, c0:c0 + n], k == 0,
                             k == KC - 1, [w3k, "h2"], [bk])
                    sa, sak = sar.next()
                    P.act(sa[:, :n], a_[:, :n], AF.Silu, [ak], [sak])
                    t_, tk = tr.next()
                    P.tt("vector", t_[:, :n], b_[:, :n], wb_s[:, :n], ALU.mult, [bk, wbsk], [tk])
                    P.tt("gpsimd", hb[:, hc, :n], t_[:, :n], sa[:, :n], ALU.mult, [tk, sak], [hbk])
                return hb, hbk

            def emit_w2(W, hb, hbk, ti, c0, n):
                w1b, w1k, w3b, w3k, w2b, w2k = W
                for dc in range(KC):
                    y_, yk = ypr.next()
                    for hc in range(4):
                        P.mm(y_[:, :n], w2b[:, hc, dc * 128:(dc + 1) * 128], hb[:, hc, :n], hc == 0, hc == 3,
                             [w2k, hbk], [yk])
                    P.tt("vector", yacc[:, dc, c0:c0 + n], y_[:, :n], yacc[:, dc, c0:c0 + n], ALU.add,
                         [yk, ("yacc", dc, ti)], [("yacc", dc, ti)])

            ensure_loaded(0)
            ensure_loaded(1)
            s0 = t0
            for st in range(4):
                tiles = stiles[st]
                stn = st_sizes[st]
                nti = len(tiles)
                P.dma(h2[:, :, 0:stn], h2v[:, :, s0:s0 + stn], reads=["h2s"], writes=["h2"])
                P.dma(wts[:, 0:stn], self.wts[:, s0:s0 + stn], reads=["wts"], writes=["wts_sb"])
                P.memset("gpsimd", yacc[:], 0.0, [("yacc", dc_, ti_) for dc_ in range(KC) for ti_ in range(4)])
                prev = None

                def flush(prev):
                    W, hb, hbk, ti, c0, n, lastt, j = prev
                    emit_w2(W, hb, hbk, ti, c0, n)
                    if lastt:
                        del loaded[j]
                        ensure_loaded(j + 2)

                for e in range(NEXP):
                    j = st * NEXP + e
                    W = loaded[j]
                    for ti, (c0, n) in enumerate(tiles):
                        hb, hbk = emit_ab(W, e, c0, n)
                        if prev is not None:
                            flush(prev)
                        prev = (W, hb, hbk, ti, c0, n, ti == nti - 1, j)
                flush(prev)
                for ti, (c0, n) in enumerate(tiles):
                    g0 = s0 + c0
                    xf, xfk = xfr.next()
                    P.dma(xf[:, :, :n], xv[:, :, g0:g0 + n], reads=["xres"], writes=[xfk])
                    xo, xok = xor_.next()
                    segs = []
                    if g0 < LC:
                        nb = min(LC, g0 + n) - g0
                        segs.append((0, nb, 1))
                        if nb < n:
                            segs.append((nb, n, 0))
                    else:
                        segs.append((0, n, 0))
                    for (a, b, s) in segs:
                        for k in range(KC):
                            P.stt("vector", xo[:, k, a:b], yacc[:, k, c0 + a:c0 + b], self.mvec(l, 5, k, s),
                                  xf[:, k, a:b], ALU.mult, ALU.add, [("yacc", k, ti), xfk, "mod"], [xok])
                    if not last:
                        P.dma(xv[:, :, g0:g0 + n], xo[:, :, :n], reads=[xok], writes=["xres"])
                    else:
                        xsq, xsqk = rings["xsq"].next()
                        P.act(xsq[:, :, :n], xo[:, :, :n], AF.Square, [xok], [xsqk])
                        ssp, sspk = wbp.next()
                        for k in range(KC):
                            P.mm(ssp[:, :n], self.ones_all[:], xsq[:, k, :n], k == 0, k == KC - 1,
                                 [xsqk, "ones_all"], [sspk])
                        sq, sqk = rings["f32a"].next()
                        P.act(sq[:, :n], ssp[:, :n], AF.Sqrt, [sspk], [sqk], bias=self.eps_ap(), scale=1.0 / D)
                        rstd, rk = rings["f32b"].next()
                        P.recip(rstd[:, :n], sq[:, :n], [sqk], [rk])
                        for k in range(KC):
                            P.stt("vector", xf[:, k, :n], xo[:, k, :n], self.gfin_sb[:, k:k + 1], rstd[:, :n], ALU.mult,
                                  ALU.mult, [xok, rk, "gfin"], [xfk])
                        P.dma(outv[:, :, g0 - LC:g0 - LC + n], xf[:, :, :n], reads=[xfk], writes=["outT"])
                s0 += stn


def _feat(v):
    return np.ascontiguousarray(np.asarray(v, np.float32).reshape(KC, 128).T)


def host_constants():
    cst = {}
    cst["ident"] = np.eye(128, dtype=np.float32)
    perm = np.zeros((128, 128), np.float32)
    for p in range(128):
        partner = p + 16 if (p % 32) < 16 else p - 16
        perm[partner, p] = 1.0
    cst["perm"] = perm
    t = np.arange(S)
    pos = np.stack([t // GRID, t % GRID], -1).astype(np.float32)
    inv = (10000.0 ** (-np.arange(16, dtype=np.float32) / 16)).astype(np.float32)
    C = np.ones((128, T), np.float32)
    Sn = np.zeros((128, T), np.float32)
    for p in range(128):
        pp = p % 64
        axis = pp // 32
        half = (pp % 32) // 16
        f = pp % 16
        ang = (pos[:, axis] * inv[f]).astype(np.float32)
        C[p, LC:] = np.cos(ang)
        Sn[p, LC:] = np.sin(ang) * (-1.0 if half == 0 else 1.0)
    cst["ropeC"] = C
    cst["ropeS"] = Sn
    ic = np.ones((128, 4, 2, 8), np.float32)
    for g, w in enumerate((2, 4, 8, 16)):
        for i in range(w // 2):
            ic[:, g, 0, i] = 1.0 / (i + w // 2)
        nr = w // 2 - 1
        for i in range(nr):
            ic[:, g, 1, i] = 1.0 / (nr - i + w // 2)
    cst["pool_ic"] = ic
    sel = np.zeros((32, NEXP, 128), np.float32)
    for e in range(NEXP):
        sel[e, e, :] = 1.0
    cst["sel"] = sel.reshape(32, NEXP * 128)
    return cst


def na_bias_tiles(rel_bias):
    H = rel_bias.shape[0]
    out = np.full((H, 14, 128, 256), NEG, np.float32)
    cols = np.arange(GRID)
    cstart = np.clip(cols - 8, 0, GRID - 16)

    def fill(tile, kr_abs, qr_abs_list, kh_slot):
        for qi, qr in enumerate(qr_abs_list):
            rs = min(max(qr - 4, 0), GRID - 8)
            if not (rs <= kr_abs < rs + 8):
                continue
            ro = kr_abs - qr + 7
            for qc in range(GRID):
                kcs = np.arange(cstart[qc], cstart[qc] + 16)
                out[:, tile, kh_slot * 64 + kcs, qi * 64 + qc] = rel_bias[:, ro, kcs - qc + 15]

    for j in range(6):
        for hslot in range(2):
            fill(j, 12 + 2 * j + hslot, [16, 17, 18, 19], hslot)
    for j in range(4):
        for hslot in range(2):
            fill(6 + j, 0 + 2 * j + hslot, [0, 1, 2, 3], hslot)
            fill(10 + j, 56 + 2 * j + hslot, [60, 61, 62, 63], hslot)
    return np.ascontiguousarray(out.transpose(0, 2, 1, 3))


def make_in_maps(inputs, ncores=8, layers=(0, 1), moe=True):
    f32 = lambda a: np.ascontiguousarray(np.asarray(a, np.float32))
    cst = host_constants()
    x = np.asarray(inputs["x"], np.float32)
    ctx = np.asarray(inputs["ctx"], np.float32)
    c = np.asarray(inputs["c"], np.float32)
    c_ctx = np.asarray(inputs["c_ctx"], np.float32)
    shared = {}
    shared["ada_w"] = f32(inputs["ada_w"])
    ada_b = np.asarray(inputs["ada_b"], np.float32)
    shared["adab"] = np.ascontiguousarray(ada_b.reshape(2, 48, 128).transpose(2, 0, 1))
    shared["gmix"] = np.ascontiguousarray(np.stack([_feat(inputs["norm_mix_g"][l]) for l in range(2)], 1))
    shared["gffn"] = np.ascontiguousarray(np.stack([_feat(inputs["norm_ffn_g"][l]) for l in range(2)], 1))
    shared["gfin"] = _feat(inputs["final_g"])
    shared["ident"] = cst["ident"]
    if 0 in layers:
        shared["e_win"] = f32(inputs["even_w_in"][0])
        shared["e_wout"] = f32(inputs["even_w_out"][0])
        gq = np.asarray(inputs["a_q_gain"][0], np.float32)
        gk = np.asarray(inputs["a_k_gain"][0], np.float32)
        shared["gqk"] = np.ascontiguousarray(np.stack([np.tile(gq, 2), np.tile(gk, 2)], 1))
        shared["perm"] = cst["perm"]
        shared["ropeC"] = cst["ropeC"]
        shared["ropeS"] = cst["ropeS"]
        shared["pool_w"] = f32(inputs["pool_w"][0])
        shared["pool_s"] = np.ascontiguousarray(np.asarray(inputs["pool_scale"][0], np.float32).reshape(4, 128).T)
        shared["pool_ic"] = cst["pool_ic"]
    if 1 in layers:
        shared["o_win"] = f32(inputs["odd_w_in"][0])
        shared["o_wout"] = f32(inputs["odd_w_out"][0])
        shared["nab"] = na_bias_tiles(np.asarray(inputs["na_rel_bias"][0], np.float32))
    shared["wr"] = np.ascontiguousarray(np.concatenate(
        [np.asarray(inputs["moe_w_group"], np.float32), np.asarray(inputs["moe_w_expert"], np.float32)], -1))
    brow = np.concatenate([np.asarray(inputs["moe_b_group"], np.float32),
                           np.asarray(inputs["moe_b_expert"], np.float32)], -1)
    shared["br"] = np.ascontiguousarray(np.broadcast_to(brow[None], (128, 2, 36)))
    if moe:
        shared["sel"] = cst["sel"]
        shared["moe_w1"] = f32(inputs["moe_w1"])
        shared["moe_w3"] = f32(inputs["moe_w3"])
        shared["moe_w2"] = f32(inputs["moe_w2"])
    maps = []
    for b in range(ncores):
        m = dict(shared)
        m["xin"] = np.ascontiguousarray(np.concatenate([ctx[b].T, x[b].T], 1))
        m["cc"] = np.ascontiguousarray(np.stack([_feat(c[b]), _feat(c_ctx)], -1))
        maps.append(m)
    return maps


def kernel(**inputs):
    nc = bass.Bass("TRN2", target_bir_lowering=False)
    bld = Builder(nc)
    bld.build()
    maps = make_in_maps(inputs, 8)
    maps = [{k: v for k, v in m.items() if k in bld.inputs} for m in maps]
    res = run_bass_kernel_spmd(nc, maps, core_ids=list(range(8)))
    bld.P.close()
    out = np.stack([np.ascontiguousarray(np.asarray(r["outT"]).T) for r in res.results], 0)
    return out.astype(np.float32)
```

```python
import numpy as np
import concourse.bass as bass
import concourse.mybir as mybir
from concourse.bass_utils import run_bass_kernel_spmd
from contextlib import ExitStack

F32 = mybir.dt.float32
BF16 = mybir.dt.bfloat16
AF = mybir.ActivationFunctionType
ALU = mybir.AluOpType
AX = mybir.AxisListType

COMPUTE = ("tensor", "vector", "scalar", "gpsimd")
ENGS = COMPUTE + ("sync",)
NDMASEM = 8

D = 1024
KC = 8
S = 4096
LC = 256
T = S + LC
EPS = 1e-6
NEXP = 32
DE = 512
GRID = 64
NEG = -30000.0


class Op:
    __slots__ = ("eng", "fn", "waits", "signal", "idx", "dma", "dsem", "dval")

    def __init__(self, eng, fn, dma):
        self.eng = eng
        self.fn = fn
        self.waits = []
        self.signal = False
        self.idx = None
        self.dma = dma
        self.dsem = None
        self.dval = None


class Prog:
    def __init__(self, nc):
        self.nc = nc
        self.es = ExitStack()
        self.queues = {e: [] for e in ENGS}
        self.lastw = {}
        self.readers = {}
        self.dma_ops = {e: [] for e in ENGS}
        self.n_tensors = 0
        self.fence = []
        self.fence_pending = {e: False for e in ENGS}

    def sb(self, shape, dt, name=None):
        self.n_tensors += 1
        return self.es.enter_context(self.nc.sbuf_tensor(f"sb{self.n_tensors}_{name or ''}", list(shape), dt))

    def ps(self, shape, dt=F32, name=None):
        self.n_tensors += 1
        return self.es.enter_context(self.nc.psum_tensor(f"ps{self.n_tensors}_{name or ''}", list(shape), dt))

    def scope(self):
        return Scope(self)

    def op(self, eng, fn, reads=(), writes=(), dma=False):
        o = Op(eng, fn, dma)
        deps = []
        for k in reads:
            w = self.lastw.get(k)
            if w is not None:
                deps.append(w)
        for k in writes:
            w = self.lastw.get(k)
            if w is not None:
                deps.append(w)
            deps.extend(self.readers.get(k, ()))
        seen = set()
        for d in deps:
            if id(d) in seen or d is o:
                continue
            seen.add(id(d))
            if (not d.dma) and (not dma) and d.eng == eng and eng == "tensor":
                continue
            o.waits.append(d)
            d.signal = True
        if self.fence_pending[eng]:
            for d in self.fence:
                if d is not o and d not in o.waits:
                    o.waits.append(d)
                    d.signal = True
            self.fence_pending[eng] = False
        if dma:
            lst = self.dma_ops[eng]
            if len(lst) >= NDMASEM:
                prev = lst[len(lst) - NDMASEM]
                if prev not in o.waits:
                    o.waits.append(prev)
            lst.append(o)
            o.signal = True
        for k in reads:
            self.readers.setdefault(k, []).append(o)
        for k in writes:
            self.lastw[k] = o
            self.readers[k] = []
        self.queues[eng].append(o)
        return o

    def barrier(self):
        fence = []
        for e in ENGS:
            for o in reversed(self.queues[e]):
                if not o.dma:
                    fence.append(o)
                    break
            fence.extend(self.dma_ops[e][-NDMASEM:])
        self.fence = fence
        self.fence_pending = {e: True for e in ENGS}

    def emit(self):
        nc = self.nc
        es = self.es
        csem = {e: es.enter_context(nc.semaphore(f"c_{e}")) for e in ENGS}
        dsem = {e: [es.enter_context(nc.semaphore(f"d_{e}{i}")) for i in range(NDMASEM)]
                for e in ENGS if self.dma_ops[e]}
        for e in ENGS:
            cnt = 0
            dcnt = [0] * NDMASEM
            n = 0
            for o in self.queues[e]:
                if o.dma:
                    slot = n % NDMASEM
                    n += 1
                    dcnt[slot] += 16
                    o.dsem = dsem[e][slot]
                    o.dval = dcnt[slot]
                elif o.signal:
                    cnt += 1
                    o.idx = cnt
        block = es.enter_context(nc.Block())
        queues = self.queues
        dma_ops = self.dma_ops

        def run(engname):
            def body(eng):
                seen = {}
                for o in queues[engname]:
                    for d in o.waits:
                        if d.dma:
                            sem, val = d.dsem, d.dval
                        else:
                            sem, val = csem[d.eng], d.idx
                        key = id(sem)
                        if seen.get(key, -1) >= val:
                            continue
                        seen[key] = val
                        eng.wait_ge(sem, val)
                    ins = o.fn(eng)
                    if o.dma:
                        ins.then_inc(o.dsem, 16)
                    elif o.signal:
                        ins.then_inc(csem[engname], 1)
                for d in dma_ops[engname][-NDMASEM:]:
                    if seen.get(id(d.dsem), -1) < d.dval:
                        seen[id(d.dsem)] = d.dval
                        eng.wait_ge(d.dsem, d.dval)
            return body

        block.tensor(run("tensor"))
        block.vector(run("vector"))
        block.scalar(run("scalar"))
        block.gpsimd(run("gpsimd"))
        block.sync(run("sync"))

    def close(self):
        self.es.close()

    def mm(self, out, lhsT, rhs, start, stop, reads, writes):
        return self.op("tensor", lambda e: e.matmul(out, lhsT, rhs, start=start, stop=stop), reads, writes)

    def act(self, out, in_, func, reads, writes, bias=None, scale=None, accum_out=None):
        kw = {}
        if bias is not None:
            kw["bias"] = bias
        if scale is not None:
            kw["scale"] = scale
        if accum_out is not None:
            kw["accum_out"] = accum_out
        return self.op("scalar", lambda e: e.activation(out=out, in_=in_, func=func, **kw), reads, writes)

    def tt(self, eng, out, in0, in1, op, reads, writes):
        return self.op(eng, lambda e: e.tensor_tensor(out, in0, in1, op), reads, writes)

    def stt(self, eng, out, in0, scalar, in1, op0, op1, reads, writes):
        return self.op(eng, lambda e: e.scalar_tensor_tensor(out=out, in0=in0, scalar=scalar, in1=in1,
                                                             op0=op0, op1=op1), reads, writes)

    def ts(self, eng, out, in0, s1, s2, op0, op1, reads, writes):
        if op1 is None:
            return self.op(eng, lambda e: e.tensor_scalar(out, in0, s1, None, op0), reads, writes)
        return self.op(eng, lambda e: e.tensor_scalar(out, in0, s1, s2, op0, op1), reads, writes)

    def cp(self, eng, out, in_, reads, writes):
        if eng == "scalar":
            return self.op(eng, lambda e: e.copy(out, in_), reads, writes)
        return self.op(eng, lambda e: e.tensor_copy(out, in_), reads, writes)

    def recip(self, out, in_, reads, writes):
        return self.op("vector", lambda e: e.reciprocal(out, in_), reads, writes)

    def memset(self, eng, ap, val, writes):
        return self.op(eng, lambda e: e.memset(ap, val), (), writes)

    def rmax(self, out, in_, reads, writes):
        return self.op("vector", lambda e: e.reduce_max(out, in_, AX.X), reads, writes)

    def dma(self, out, in_, reads=(), writes=(), eng="sync"):
        return self.op(eng, lambda e: e.dma_start(out=out, in_=in_), reads, writes, dma=True)


class Scope:
    def __init__(self, P):
        self.P = P
        self.es = ExitStack()

    def __enter__(self):
        self.es.__enter__()
        return self

    def __exit__(self, *a):
        r = self.es.__exit__(*a)
        self.P.barrier()
        return r

    def sb(self, shape, dt, name=None):
        self.P.n_tensors += 1
        return self.es.enter_context(self.P.nc.sbuf_tensor(f"sb{self.P.n_tensors}_{name or ''}", list(shape), dt))

    def ps(self, shape, dt=F32, name=None):
        self.P.n_tensors += 1
        return self.es.enter_context(self.P.nc.psum_tensor(f"ps{self.P.n_tensors}_{name or ''}", list(shape), dt))

    def ring(self, n, shape, dt, tag, psum=False):
        tiles = [(self.ps(shape, dt) if psum else self.sb(shape, dt)) for _ in range(n)]
        return Ring(tiles, tag)


class Ring:
    def __init__(self, tiles, tag):
        self.tiles = tiles
        self.tag = tag
        self.i = 0

    def next(self):
        j = self.i % len(self.tiles)
        self.i += 1
        return self.tiles[j], (self.tag, j)


class Builder:
    def __init__(self, nc, layers=(0, 1), moe=True, dbg=()):
        self.nc = nc
        self.P = Prog(nc)
        self.layers = layers
        self.moe = moe
        self.dbg = dbg
        self.inputs = {}
        self.outputs = {}

    def din(self, name, shape, dt=F32):
        t = self.nc.dram_tensor(name, list(shape), dt, kind="ExternalInput").ap()
        self.inputs[name] = t
        return t

    def dout(self, name, shape, dt=F32):
        t = self.nc.dram_tensor(name, list(shape), dt, kind="ExternalOutput").ap()
        self.outputs[name] = t
        return t

    def dscratch(self, name, shape, dt=F32):
        return self.nc.dram_tensor(name, list(shape), dt).ap()

    def build(self):
        P = self.P
        self.xin = self.din("xin", [D, T])
        self.cc = self.din("cc", [128, KC, 2])
        self.ada_w = self.din("ada_w", [2, D, 6 * D])
        self.adab = self.din("adab", [128, 2, 48])
        self.gmix = self.din("gmix", [128, 2, KC])
        self.gffn = self.din("gffn", [128, 2, KC])
        self.gfin = self.din("gfin", [128, KC])
        self.ident_d = self.din("ident", [128, 128])
        if 0 in self.layers:
            self.e_win = self.din("e_win", [D, 1280])
            self.e_wout = self.din("e_wout", [D, D])
            self.gqk = self.din("gqk", [128, 2])
            self.perm_d = self.din("perm", [128, 128])
            self.ropeC = self.din("ropeC", [128, T])
            self.ropeS = self.din("ropeS", [128, T])
            self.pool_w = self.din("pool_w", [4, 128, 128])
            self.pool_s = self.din("pool_s", [128, 4])
            self.pool_ic = self.din("pool_ic", [128, 4, 2, 8])
        if 1 in self.layers:
            self.o_win = self.din("o_win", [D, 3 * D])
            self.o_wout = self.din("o_wout", [D, D])
            self.nab = self.din("nab", [16, 128, 14, 256])
        self.wr = self.din("wr", [2, D, 36])
        self.br = self.din("br", [128, 2, 36])
        if self.moe:
            self.sel_d = self.din("sel", [32, NEXP * 128])
            self.w1 = self.din("moe_w1", [2, NEXP, D, DE])
            self.w3 = self.din("moe_w3", [2, NEXP, D, DE])
            self.w2 = self.din("moe_w2", [2, NEXP, DE, D])
        self.outT = self.dout("outT", [D, S])
        self.xres = self.dscratch("xres", [D, T])
        self.h2s = self.dscratch("h2s", [D, T], BF16)
        self.aTs = self.dscratch("aTs", [D, T], BF16)
        self.us = self.dscratch("us", [512, T])
        self.wts = self.dscratch("wts", [32, T])

        self.ident = P.sb([128, 128], F32, "ident")
        P.dma(self.ident[:], self.ident_d, writes=["ident"])
        self.ones_all = P.sb([128, 128], BF16, "ones_all")
        P.memset("vector", self.ones_all[:], 1.0, ["ones_all"])
        self.ones_bd = P.sb([128, 128], BF16, "ones_bd")
        P.memset("vector", self.ones_bd[:], 0.0, ["ones_bd"])
        P.memset("vector", self.ones_bd[0:64, 0:64], 1.0, ["ones_bd"])
        P.memset("vector", self.ones_bd[64:128, 64:128], 1.0, ["ones_bd"])
        self.mod = P.sb([128, 2, 48, 2], F32, "mod")
        self.S1 = P.sb([128, 2, KC, 2], F32, "S1")
        self.S2 = P.sb([128, 2, KC, 2], F32, "S2")
        self.gfin_sb = P.sb([128, KC], F32, "gfin_sb")
        P.dma(self.gfin_sb[:], self.gfin, writes=["gfin"])

        self.eps_ap()
        self.phase_mod()
        if 0 not in self.layers:
            xri = self.din("xres_in", [D, T])
            with P.scope() as sc:
                r = sc.ring(2, [128, T], F32, "xri")
                for r0 in range(0, D, 128):
                    t, k = r.next()
                    P.dma(t[:], xri[r0:r0 + 128, :], writes=[k])
                    P.dma(self.xres[r0:r0 + 128, :], t[:], reads=[k], writes=["xres"])
        if 0 in self.layers:
            self.layer0_front()
            self.dump("a0", self.aTs, D, T, BF16, "aTs")
            self.phase_post(0)
            self.dump("xp0", self.xres, D, T, F32, "xres")
            self.dump("h20", self.h2s, D, T, BF16, "h2s")
            self.dump("wt0", self.wts, 32, T, F32, "wts")
            if self.moe:
                self.phase_moe(0)
                self.dump("x0", self.xres, D, T, F32, "xres")
        if 1 in self.layers:
            self.layer1_front()
            self.dump("a1", self.aTs, D, T, BF16, "aTs")
            self.phase_post(1)
            self.dump("xp1", self.xres, D, T, F32, "xres")
            self.dump("h21", self.h2s, D, T, BF16, "h2s")
            self.dump("wt1", self.wts, 32, T, F32, "wts")
            if self.moe:
                self.phase_moe(1)
        P.emit()

    def dump(self, tag, src, R, C, dt, key):
        if tag not in self.dbg:
            return
        P = self.P
        o = self.dout("dbg_" + tag, [R, C], dt)
        with P.scope() as sc:
            r = sc.ring(2, [128, C], dt, "dump_" + tag)
            for r0 in range(0, R, 128):
                n = min(128, R - r0)
                t, k = r.next()
                P.dma(t[0:n, :], src[r0:r0 + n, :], reads=[key], writes=[k])
                P.dma(o[r0:r0 + n, :], t[0:n, :], reads=[k])

    def phase_mod(self):
        P = self.P
        with P.scope() as sc:
            cc = sc.sb([128, KC, 2], F32)
            cs = sc.sb([128, KC, 2], F32)
            adab = sc.sb([128, 2, 48], F32)
            gm = sc.sb([128, 2, KC], F32)
            gf = sc.sb([128, 2, KC], F32)
            P.dma(cc[:], self.cc, writes=["cc"])
            P.dma(adab[:], self.adab, writes=["adab"])
            P.dma(gm[:], self.gmix, writes=["gm"])
            P.dma(gf[:], self.gffn, writes=["gf"])
            P.act(cs[:], cc[:], AF.Silu, ["cc"], ["cs"])
            wring = sc.ring(2, [128, KC, 768], F32, "adaw")
            pm = sc.ps([128, 48, 2], F32)
            for l in self.layers:
                awl = self.ada_w[l].rearrange("(k p) n -> p k n", p=128)
                for blk in range(8):
                    wt, wk = wring.next()
                    P.dma(wt[:], awl[:, :, blk * 768:(blk + 1) * 768], writes=[wk])
                    for j in range(6):
                        oc = blk * 6 + j
                        for k in range(KC):
                            P.mm(pm[:, oc, :], wt[:, k, j * 128:(j + 1) * 128], cs[:, k, :],
                                 k == 0, k == KC - 1, [wk, "cs"], ["pm"])
                P.tt("vector", self.mod[:, l], pm[:], adab[:, l].unsqueeze(2).to_broadcast([128, 48, 2]),
                     ALU.add, ["pm", "adab"], ["mod"])
                P.stt("vector", self.S1[:, l], self.mod[:, l, 8:16, :], 1.0,
                      gm[:, l].unsqueeze(2).to_broadcast([128, KC, 2]), ALU.add, ALU.mult,
                      ["mod", "gm"], ["S1"])
                P.stt("vector", self.S2[:, l], self.mod[:, l, 32:40, :], 1.0,
                      gf[:, l].unsqueeze(2).to_broadcast([128, KC, 2]), ALU.add, ALU.mult,
                      ["mod", "gf"], ["S2"])
            if "mod" in self.dbg:
                o = self.dout("dbg_mod", [128, 2 * 48 * 2])
                P.dma(o, self.mod[:].rearrange("p a b c -> p (a b c)"), reads=["mod"])

    def mvec(self, l, kind, k, s):
        return self.mod[:, l, kind * 8 + k, s:s + 1]

    def load_cast(self, sc_ring, dst, src, shape, dkeys, eng="gpsimd"):
        P = self.P
        st, sk = sc_ring.next()
        view = st
        idx = tuple(slice(0, s) for s in shape)
        P.dma(view[idx], src, writes=[sk])
        P.cp(eng, dst, view[idx], [sk], dkeys)

    def rmsnorm_mod(self, sc, xt, xk, N, Svec, shk_l_kind, s, l, out_bf, out_bf_key, out_f32=None, out_f32_key=None,
                    rings=None):
        P = self.P
        xsq, xsqk = rings["xsq"].next()
        P.act(xsq[:, :, :N], xt[:, :, :N], AF.Square, [xk], [xsqk])
        ssp, sspk = rings["ssp"].next()
        for k in range(KC):
            P.mm(ssp[:, :N], self.ones_all[:], xsq[:, k, :N], k == 0, k == KC - 1, [xsqk, "ones_all"], [sspk])
        sq, sqk = rings["f32a"].next()
        P.act(sq[:, :N], ssp[:, :N], AF.Sqrt, [sspk], [sqk], bias=self.eps_ap(), scale=1.0 / D)
        rstd, rk = rings["f32b"].next()
        P.recip(rstd[:, :N], sq[:, :N], [sqk], [rk])
        for k in range(KC):
            tmp, tk = rings["f32c"].next()
            P.stt("vector", tmp[:, :N], xt[:, k, :N], Svec[:, l, k, s:s + 1], rstd[:, :N], ALU.mult, ALU.mult,
                  [xk, rk, "S1", "S2"], [tk])
            sh = self.mvec(l, shk_l_kind, k, s)
            if out_f32 is not None:
                P.act(out_f32[:, k, :N], tmp[:, :N], AF.Identity, [tk, "mod"], [out_f32_key], bias=sh, scale=1.0)
                P.cp("gpsimd", out_bf[:, k, :N], out_f32[:, k, :N], [out_f32_key], [out_bf_key])
            else:
                P.act(out_bf[:, k, :N], tmp[:, :N], AF.Identity, [tk, "mod"], [out_bf_key], bias=sh, scale=1.0)

    def eps_ap(self):
        if not hasattr(self, "_eps"):
            self._eps = self.P.sb([128, 1], F32, "eps")
            self.P.memset("vector", self._eps[:], EPS, ["eps"])
        return self._eps[:]

    def layer0_front(self):
        P = self.P
        l = 0
        TN = 256
        ntile = T // TN
        xin_v = self.xin.rearrange("(k p) t -> p k t", p=128)
        with P.scope() as sq_scope:
            QT = sq_scope.sb([128, 4, T], BF16, "QT")
            K2T = sq_scope.sb([128, 2, T], BF16, "K2T")
            Vaug = sq_scope.sb([128, 2, T // 128, 192], BF16, "Vaug")
            P.memset("gpsimd", Vaug[:, :, :, 64:128], 1.0, ["Vaug"])
            with P.scope() as sc:
                stg = sc.ring(2, [128, KC, 256], F32, "stg")
                Win = sc.sb([128, KC, 1280], BF16, "Win")
                Wk2 = sc.sb([128, KC, 256], BF16, "Wk2")
                ewv = self.e_win.rearrange("(k p) n -> p k n", p=128)
                for j in range(5):
                    self.load_cast(stg, Win[:, :, j * 256:(j + 1) * 256], ewv[:, :, j * 256:(j + 1) * 256],
                                   [128, KC, 256], ["Win"])
                for g in range(2):
                    for h in range(2):
                        P.cp("gpsimd", Wk2[:, :, g * 128 + h * 64: g * 128 + h * 64 + 64],
                             Win[:, :, 512 + g * 64: 512 + g * 64 + 64], ["Win"], ["Wk2"])
                gqk = sc.sb([128, 2], F32, "gqk")
                P.dma(gqk[:], self.gqk, writes=["gqk"])
                perm = sc.sb([128, 128], F32, "perm")
                P.dma(perm[:], self.perm_d, writes=["perm"])
                rings = {
                    "xsq": sc.ring(1, [128, KC, TN], BF16, "xsq"),
                    "ssp": sc.ring(1, [128, TN], F32, "ssp", psum=True),
                    "f32a": sc.ring(2, [128, TN], F32, "f32a"),
                    "f32b": sc.ring(2, [128, TN], F32, "f32b"),
                    "f32c": sc.ring(3, [128, TN], F32, "f32c"),
                }
                xring = sc.ring(2, [128, KC, TN], F32, "xt")
                hring = sc.ring(2, [128, KC, TN], BF16, "hT")
                cring = sc.ring(2, [128, TN], F32, "ropeC")
                sring = sc.ring(2, [128, TN], F32, "ropeS")
                pp = sc.ring(3, [128, TN], F32, "pp", psum=True)
                pq = sc.ring(1, [128, TN], F32, "pq", psum=True)
                pr = sc.ring(1, [128, TN], F32, "pr", psum=True)
                pv = sc.ring(1, [128, 128], F32, "pv", psum=True)
                qsqr = sc.ring(2, [128, TN], BF16, "qsq")
                qnr = sc.ring(2, [128, TN], F32, "qn")
                t1r = sc.ring(2, [128, TN], F32, "t1")
                t2r = sc.ring(2, [128, TN], F32, "t2")
                ur = sc.ring(2, [128, TN], F32, "ust")
                def load0(ti):
                    c0 = ti * TN
                    xt, xk = xring.next()
                    P.dma(xt[:], xin_v[:, :, c0:c0 + TN], writes=[xk])
                    ct, ck = cring.next()
                    st_, sk_ = sring.next()
                    P.dma(ct[:], self.ropeC[:, c0:c0 + TN], writes=[ck])
                    P.dma(st_[:], self.ropeS[:, c0:c0 + TN], writes=[sk_])
                    return xt, xk, ct, ck, st_, sk_
                nxt = load0(0)
                for ti in range(ntile):
                    c0 = ti * TN
                    s = 1 if ti == 0 else 0
                    xt, xk, ct, ck, st_, sk_ = nxt
                    if ti + 1 < ntile:
                        nxt = load0(ti + 1)
                    hT, hk = hring.next()
                    self.rmsnorm_mod(sc, xt, xk, TN, self.S1, 0, s, l, hT, hk, rings=rings)
                    for oc in range(6):
                        ps_, pk_ = pp.next()
                        for k in range(KC):
                            if oc < 4:
                                w = Win[:, k, oc * 128:(oc + 1) * 128]
                                wkey = "Win"
                            else:
                                w = Wk2[:, k, (oc - 4) * 128:(oc - 3) * 128]
                                wkey = "Wk2"
                            P.mm(ps_[:], w, hT[:, k, :], k == 0, k == KC - 1, [wkey, hk], [pk_])
                        gain = gqk[:, 0:1] if oc < 4 else gqk[:, 1:2]
                        qsq, qsqk = qsqr.next()
                        P.act(qsq[:], ps_[:], AF.Square, [pk_], [qsqk])
                        ssq, ssqk = pq.next()
                        P.mm(ssq[:], self.ones_bd[:], qsq[:], True, True, [qsqk, "ones_bd"], [ssqk])
                        sq, sqk = rings["f32a"].next()
                        P.act(sq[:], ssq[:], AF.Sqrt, [ssqk], [sqk], bias=self.eps_ap(), scale=1.0 / 64)
                        rs, rsk = rings["f32b"].next()
                        P.recip(rs[:], sq[:], [sqk], [rsk])
                        qn, qnk = qnr.next()
                        P.stt("vector", qn[:], ps_[:], gain, rs[:], ALU.mult, ALU.mult, [pk_, rsk, "gqk"], [qnk])
                        prp, prk = pr.next()
                        P.mm(prp[:], perm[:], qn[:], True, True, [qnk, "perm"], [prk])
                        t1, t1k = t1r.next()
                        P.tt("gpsimd", t1[:], qn[:], ct[:], ALU.mult, [qnk, ck], [t1k])
                        t2, t2k = t2r.next()
                        P.tt("vector", t2[:], prp[:], st_[:], ALU.mult, [prk, sk_], [t2k])
                        if oc < 4:
                            dst, dk = QT[:, oc, c0:c0 + TN], "QT"
                        else:
                            dst, dk = K2T[:, oc - 4, c0:c0 + TN], "K2T"
                        P.tt("gpsimd", dst, t1[:], t2[:], ALU.add, [t1k, t2k], [dk])
                    for j in range(TN // 128):
                        vp, vk = pv.next()
                        for k in range(KC):
                            P.mm(vp[:], hT[:, k, j * 128:(j + 1) * 128], Win[:, k, 640:768], k == 0, k == KC - 1,
                                 [hk, "Win"], [vk])
                        tix = c0 // 128 + j
                        for g in range(2):
                            P.cp("scalar", Vaug[:, g, tix, 0:64], vp[:, g * 64:(g + 1) * 64], [vk], ["Vaug"])
                            P.cp("vector", Vaug[:, g, tix, 128:192], vp[:, g * 64:(g + 1) * 64], [vk], ["Vaug"])
                    for g in range(4):
                        ps_, pk_ = pp.next()
                        for k in range(KC):
                            P.mm(ps_[:], Win[:, k, 768 + g * 128: 768 + (g + 1) * 128], hT[:, k, :], k == 0,
                                 k == KC - 1, ["Win", hk], [pk_])
                        ut, uk = ur.next()
                        P.cp("scalar", ut[:], ps_[:], [pk_], [uk])
                        P.dma(self.us[g * 128:(g + 1) * 128, c0:c0 + TN], ut[:], reads=[uk], writes=["us"])
                if "qkv" in self.dbg:
                    o = self.dout("dbg_QT", [128, 4 * T], BF16)
                    P.dma(o, QT[:].rearrange("p a t -> p (a t)"), reads=["QT"])
                    o = self.dout("dbg_K2T", [128, 2 * T], BF16)
                    P.dma(o, K2T[:].rearrange("p a t -> p (a t)"), reads=["K2T"])
                    o = self.dout("dbg_V", [128, 2 * (T // 128) * 192], BF16)
                    P.dma(o, Vaug[:].rearrange("p a t d -> p (a t d)"), reads=["Vaug"])
            with P.scope() as sc:
                PADL = 16
                WT = PADL + LC + PADL + S + PADL
                OFFC = PADL
                OFFX = PADL + LC + PADL
                pooled = sc.sb([128, 4, T], BF16, "pooled")
                pw = sc.sb([128, 4, 128], BF16, "pw")
                pws = sc.sb([128, 4, 128], F32, "pws")
                P.dma(pws[:], self.pool_w.rearrange("g c d -> c g d"), writes=["pws"])
                P.cp("gpsimd", pw[:], pws[:], ["pws"], ["pw"])
                psc = sc.sb([128, 4], F32, "psc")
                P.dma(psc[:], self.pool_s, writes=["psc"])
                pic = sc.sb([128, 4, 2, 8], F32, "pic")
                P.dma(pic[:], self.pool_ic, writes=["pic"])
                with P.scope() as sc2:
                    U = sc2.sb([128, WT], F32, "U")
                    A = sc2.sb([128, WT], F32, "A")
                    B = sc2.sb([128, WT], F32, "B")
                    for g in range(4):
                        w = (2, 4, 8, 16)[g]
                        P.memset("vector", U[:], 0.0, ["U"])
                        P.dma(U[:, OFFC:OFFC + LC], self.us[g * 128:(g + 1) * 128, 0:LC], reads=["us"], writes=["U"])
                        P.dma(U[:, OFFX:OFFX + S], self.us[g * 128:(g + 1) * 128, LC:T], reads=["us"], writes=["U"])
                        lo, hi = 8, WT - 8
                        P.memset("gpsimd", A[:], 0.0, ["A"])
                        P.memset("gpsimd", B[:], 0.0, ["B"])
                        P.tt("vector", A[:, lo:hi], U[:, lo - 1:hi - 1], U[:, lo:hi], ALU.add, ["U"], ["A"])
                        cur, curk, oth, othk = A, "A", B, "B"
                        sh = 1
                        ww = 2
                        while ww < w:
                            d = ww // 2
                            P.tt("vector", oth[:, lo:hi], cur[:, lo - d:hi - d], cur[:, lo + d:hi + d], ALU.add,
                                 [curk], [othk])
                            cur, curk, oth, othk = oth, othk, cur, curk
                            ww *= 2
                        pl, plk = oth, othk
                        P.stt("vector", pl[:, lo:hi], cur[:, lo:hi], 1.0 / w, U[:, lo:hi], ALU.mult, ALU.subtract,
                              [curk, "U"], [plk])
                        for (off, L) in ((OFFC, LC), (OFFX, S)):
                            nl = w // 2
                            tmpb = sc2.sb([128, 8], F32)
                            P.tt("vector", tmpb[:, 0:nl], cur[:, off:off + nl], pic[:, g, 0, 0:nl], ALU.mult,
                                 [curk, "pic"], ["tmpb"])
                            P.tt("vector", pl[:, off:off + nl], tmpb[:, 0:nl], U[:, off:off + nl], ALU.subtract,
                                 ["tmpb", "U"], [plk])
                            nr = w // 2 - 1
                            if nr > 0:
                                r0 = off + L - nr
                                tmpc = sc2.sb([128, 8], F32)
                                P.tt("vector", tmpc[:, 0:nr], cur[:, r0:r0 + nr], pic[:, g, 1, 0:nr], ALU.mult,
                                     [curk, "pic"], ["tmpc"])
                                P.tt("vector", pl[:, r0:r0 + nr], tmpc[:, 0:nr], U[:, r0:r0 + nr], ALU.subtract,
                                     ["tmpc", "U"], [plk])
                        P.cp("gpsimd", pooled[:, g, 0:LC], pl[:, OFFC:OFFC + LC], [plk], ["pooled"])
                        P.cp("gpsimd", pooled[:, g, LC:T], pl[:, OFFX:OFFX + S], [plk], ["pooled"])
                self.attention0(QT, K2T, Vaug)
                with P.scope() as sc2:
                    pmm = sc2.ring(2, [128, 512], F32, "pmm", psum=True)
                    ob = sc2.ring(2, [128, 512], BF16, "pob")
                    for g in range(4):
                        for c0 in range(0, T, 512):
                            n = min(512, T - c0)
                            ps_, pk_ = pmm.next()
                            P.mm(ps_[:, :n], pw[:, g, :], pooled[:, g, c0:c0 + n], True, True, ["pw", "pooled"], [pk_])
                            o_, ok_ = ob.next()
                            P.act(o_[:, :n], ps_[:, :n], AF.Identity, [pk_, "psc"], [ok_], scale=psc[:, g:g + 1])
                            P.dma(self.aTs[(4 + g) * 128:(5 + g) * 128, c0:c0 + n], o_[:, :n], reads=[ok_],
                                  writes=["aTs"])

    def attention0(self, QT, K2T, Vaug):
        P = self.P
        A_SCALE = 64 ** -0.5
        with P.scope() as sc:
            sps = sc.ring(4, [128, 512], F32, "sps", psum=True)
            oA = sc.ring(2, [128, 512], F32, "oA", psum=True)
            oB = sc.ring(2, [128, 512], F32, "oB", psum=True)
            pt = sc.ring(4, [128, 512], BF16, "pt")
            Rr = sc.ring(2, [128, 512], F32, "R")
            ao = sc.ring(2, [128, 512], BF16, "ao")
            qtiles = [(0, LC, 2)] + [(LC + i * 512, 512, T // 128) for i in range(S // 512)]
            for c in range(4):
                g = c // 2
                for (q0, N, nkc) in qtiles:
                    a_, ak = oA.next()
                    b_, bk = oB.next()
                    iters = [(kc, h) for kc in range(nkc) for h in range(2)]
                    LA = 3
                    pend = {}

                    def emitS(i, g=g, c=c, q0=q0, N=N, iters=iters, pend=pend):
                        kc, h = iters[i]
                        hp = slice(h * 64, h * 64 + 64)
                        sp_, spk = sps.next()
                        P.mm(sp_[:, :N], K2T[hp, g, kc * 128:(kc + 1) * 128], QT[hp, c, q0:q0 + N], True, True,
                             ["K2T", "QT"], [spk])
                        pend[i] = (sp_, spk)

                    for i in range(min(LA, len(iters))):
                        emitS(i)
                    for i in range(len(iters)):
                        if i + LA < len(iters):
                            emitS(i + LA)
                        kc, h = iters[i]
                        sp_, spk = pend.pop(i)
                        p_, pk_ = pt.next()
                        P.act(p_[:, :N], sp_[:, :N], AF.Exp, [spk], [pk_], scale=A_SCALE)
                        if h == 0:
                            P.mm(a_[:, :N], Vaug[:, g, kc, 0:128], p_[:, :N], kc == 0, kc == nkc - 1,
                                 ["Vaug", pk_], [ak])
                        else:
                            P.mm(b_[:, :N], Vaug[:, g, kc, 64:192], p_[:, :N], kc == 0, kc == nkc - 1,
                                 ["Vaug", pk_], [bk])
                    R, Rk = Rr.next()
                    P.recip(R[0:64, :N], a_[64:128, :N], [ak], [Rk])
                    P.recip(R[64:128, :N], b_[0:64, :N], [bk], [Rk])
                    o_, ok_ = ao.next()
                    P.tt("vector", o_[0:64, :N], a_[0:64, :N], R[0:64, :N], ALU.mult, [ak, Rk], [ok_])
                    P.tt("vector", o_[64:128, :N], b_[64:128, :N], R[64:128, :N], ALU.mult, [bk, Rk], [ok_])
                    P.dma(self.aTs[c * 128:(c + 1) * 128, q0:q0 + N], o_[:, :N], reads=[ok_], writes=["aTs"])

    def layer1_front(self):
        P = self.P
        l = 1
        TN = 256
        ntile = T // TN
        xv = self.xres.rearrange("(k p) t -> p k t", p=128)
        C_SCALE = 64 ** -0.5
        with P.scope() as sc0:
            hT = sc0.sb([128, KC, T], BF16, "hT1")
            with P.scope() as sc:
                rings = {
                    "xsq": sc.ring(1, [128, KC, TN], BF16, "xsq"),
                    "ssp": sc.ring(1, [128, TN], F32, "ssp", psum=True),
                    "f32a": sc.ring(2, [128, TN], F32, "f32a"),
                    "f32b": sc.ring(2, [128, TN], F32, "f32b"),
                    "f32c": sc.ring(3, [128, TN], F32, "f32c"),
                }
                xring = sc.ring(2, [128, KC, TN], F32, "xt")
                hring = sc.ring(2, [128, KC, TN], BF16, "hTt")
                def load1(ti):
                    c0 = ti * TN
                    xt, xk = xring.next()
                    P.dma(xt[:], xv[:, :, c0:c0 + TN], reads=["xres"], writes=[xk])
                    return xt, xk
                nxt = load1(0)
                for ti in range(ntile):
                    c0 = ti * TN
                    s = 1 if ti == 0 else 0
                    xt, xk = nxt
                    if ti + 1 < ntile:
                        nxt = load1(ti + 1)
                    ht, hk = hring.next()
                    self.rmsnorm_mod(sc, xt, xk, TN, self.S1, 0, s, l, ht, hk, rings=rings)
                    P.cp("gpsimd", hT[:, :, c0:c0 + TN], ht[:], [hk], ["hT1"])
            if "h1" in self.dbg:
                o = self.dout("dbg_h1", [128, KC * T], BF16)
                P.dma(o, hT[:].rearrange("p a t -> p (a t)"), reads=["hT1"])
            with P.scope() as sc:
                owv = self.o_win.rearrange("(k p) n -> p k n", p=128)
                stg = sc.ring(2, [128, KC, 128], F32, "stg1")
                wq = sc.ring(2, [128, KC, 128], BF16, "wq")
                wk = sc.ring(2, [128, KC, 128], BF16, "wk")
                wv = sc.ring(2, [128, KC, 128], BF16, "wv")
                qT = sc.sb([128, S], BF16, "qT1")
                kT = sc.sb([128, T], BF16, "kT1")
                Vaug = sc.sb([128, T // 128, 192], BF16, "Vaug1")
                P.memset("gpsimd", Vaug[:, :, 64:128], 1.0, ["Vaug1"])
                bias = sc.sb([128, 2, 14, 256], F32, "nab")
                pp = sc.ring(2, [128, 512], F32, "pp1", psum=True)
                pv = sc.ring(1, [128, 128], F32, "pv1", psum=True)
                sps = sc.ring(3, [128, 256], F32, "sps1", psum=True)
                oA = sc.ring(1, [128, 256], F32, "oA1", psum=True)
                oB = sc.ring(1, [128, 256], F32, "oB1", psum=True)
                ssb = sc.ring(3, [128, 256], F32, "ssb1")
                pt = sc.ring(4, [128, 256], BF16, "pt1")
                Rr = sc.ring(2, [128, 256], F32, "R1")
                ao = sc.ring(2, [128, 256], BF16, "ao1")
                for c in range(8):
                    wq_, wqk = wq.next()
                    wk_, wkk = wk.next()
                    wv_, wvk = wv.next()
                    self.load_cast(stg, wq_[:], owv[:, :, c * 128:(c + 1) * 128], [128, KC, 128], [wqk])
                    self.load_cast(stg, wk_[:], owv[:, :, D + c * 128:D + (c + 1) * 128], [128, KC, 128], [wkk])
                    self.load_cast(stg, wv_[:], owv[:, :, 2 * D + c * 128:2 * D + (c + 1) * 128], [128, KC, 128], [wvk])
                    for hh in range(2):
                        P.dma(bias[:, hh], self.nab[2 * c + hh], writes=["nab"])
                    for c0 in range(0, S, 512):
                        ps_, pk_ = pp.next()
                        for k in range(KC):
                            P.mm(ps_[:], wq_[:, k, :], hT[:, k, LC + c0:LC + c0 + 512], k == 0, k == KC - 1,
                                 [wqk, "hT1"], [pk_])
                        P.act(qT[:, c0:c0 + 512], ps_[:], AF.Identity, [pk_], ["qT1"], scale=C_SCALE)
                    for c0 in range(0, T, 512):
                        n = min(512, T - c0)
                        ps_, pk_ = pp.next()
                        for k in range(KC):
                            P.mm(ps_[:, :n], wk_[:, k, :], hT[:, k, c0:c0 + n], k == 0, k == KC - 1,
                                 [wkk, "hT1"], [pk_])
                        P.cp("vector", kT[:, c0:c0 + n], ps_[:, :n], [pk_], ["kT1"])
                    for tix in range(T // 128):
                        vp, vk = pv.next()
                        for k in range(KC):
                            P.mm(vp[:], hT[:, k, tix * 128:(tix + 1) * 128], wv_[:, k, :], k == 0, k == KC - 1,
                                 ["hT1", wvk], [vk])
                        P.cp("scalar", Vaug[:, tix, 0:64], vp[:, 0:64], [vk], ["Vaug1"])
                        P.cp("vector", Vaug[:, tix, 128:192], vp[:, 64:128], [vk], ["Vaug1"])
                    for qb in range(16):
                        if qb == 0:
                            kr0, nloc, tb = 0, 4, 6
                        elif qb == 15:
                            kr0, nloc, tb = 56, 4, 10
                        else:
                            kr0, nloc, tb = 4 * qb - 4, 6, 0
                        chunks = [(LC + 64 * kr0 + 128 * j, tb + j) for j in range(nloc)] + [(0, None), (128, None)]
                        a_, ak = oA.next()
                        b_, bk = oB.next()
                        q0 = qb * 256
                        nch = len(chunks)
                        iters = [(ci, h) for ci in range(nch) for h in range(2)]
                        LA = 2
                        pend = {}

                        def emitS1(i, iters=iters, chunks=chunks, q0=q0, pend=pend):
                            ci, h = iters[i]
                            k0 = chunks[ci][0]
                            hp = slice(h * 64, h * 64 + 64)
                            sp_, spk = sps.next()
                            P.mm(sp_[:], kT[hp, k0:k0 + 128], qT[hp, q0:q0 + 256], True, True,
                                 ["kT1", "qT1"], [spk])
                            pend[i] = (sp_, spk)

                        for i in range(LA):
                            emitS1(i)
                        for i in range(len(iters)):
                            if i + LA < len(iters):
                                emitS1(i + LA)
                            ci, h = iters[i]
                            k0, bt = chunks[ci]
                            sp_, spk = pend.pop(i)
                            p_, pk_ = pt.next()
                            if bt is not None:
                                sb_, sbk = ssb.next()
                                P.tt("vector", sb_[:], sp_[:], bias[:, h, bt, :], ALU.add, [spk, "nab"], [sbk])
                                P.act(p_[:], sb_[:], AF.Exp, [sbk], [pk_])
                            else:
                                P.act(p_[:], sp_[:], AF.Exp, [spk], [pk_])
                            tix = k0 // 128
                            if h == 0:
                                P.mm(a_[:], Vaug[:, tix, 0:128], p_[:], ci == 0, ci == nch - 1, ["Vaug1", pk_], [ak])
                            else:
                                P.mm(b_[:], Vaug[:, tix, 64:192], p_[:], ci == 0, ci == nch - 1, ["Vaug1", pk_], [bk])
                        R, Rk = Rr.next()
                        P.recip(R[0:64, :], a_[64:128, :], [ak], [Rk])
                        P.recip(R[64:128, :], b_[0:64, :], [bk], [Rk])
                        o_, ok_ = ao.next()
                        P.tt("vector", o_[0:64, :], a_[0:64, :], R[0:64, :], ALU.mult, [ak, Rk], [ok_])
                        P.tt("vector", o_[64:128, :], b_[64:128, :], R[64:128, :], ALU.mult, [bk, Rk], [ok_])
                        P.dma(self.aTs[c * 128:(c + 1) * 128, LC + q0:LC + q0 + 256], o_[:], reads=[ok_],
                              writes=["aTs"])

    def phase_post(self, l):
        P = self.P
        TN = 256
        t0 = 0 if l == 0 else LC
        src = self.xin if l == 0 else self.xres
        srck = "xin" if l == 0 else "xres"
        xv = src.rearrange("(k p) t -> p k t", p=128)
        xo = self.xres.rearrange("(k p) t -> p k t", p=128)
        h2v = self.h2s.rearrange("(k p) t -> p k t", p=128)
        aTv = self.aTs.rearrange("(k p) t -> p k t", p=128)
        wout_d = (self.e_wout if l == 0 else self.o_wout).rearrange("(k p) n -> p k n", p=128)
        with P.scope() as sc:
            stg = sc.ring(2, [128, KC, 256], F32, "stgp")
            Wout = sc.sb([128, KC, D], BF16, "Wout")
            for j in range(4):
                self.load_cast(stg, Wout[:, :, j * 256:(j + 1) * 256], wout_d[:, :, j * 256:(j + 1) * 256],
                               [128, KC, 256], ["Wout"])
            wr = sc.sb([128, KC, 36], F32, "wr")
            P.dma(wr[:], self.wr[l].rearrange("(k p) n -> p k n", p=128), writes=["wr"])
            br = sc.sb([128, 36], F32, "br")
            P.dma(br[:], self.br[:, l, :], writes=["br"])
            rings = {
                "xsq": sc.ring(1, [128, KC, TN], BF16, "xsq"),
                "ssp": sc.ring(1, [128, TN], F32, "ssp", psum=True),
                "f32a": sc.ring(2, [128, TN], F32, "f32a"),
                "f32b": sc.ring(2, [128, TN], F32, "f32b"),
                "f32c": sc.ring(3, [128, TN], F32, "f32c"),
            }
            xring = sc.ring(2, [128, KC, TN], F32, "xtp")
            aring = sc.ring(2, [128, KC, TN], BF16, "aTt")
            xnring = sc.ring(2, [128, KC, TN], F32, "xn")
            hfring = sc.ring(1, [128, KC, TN], F32, "h2f")
            hbring = sc.ring(2, [128, KC, TN], BF16, "h2b")
            yp = sc.ring(3, [128, TN], F32, "yp", psum=True)
            lp = sc.ring(1, [128, 36], F32, "lp", psum=True)
            wtp = sc.ring(1, [32, 128], F32, "wtp", psum=True)
            wtr = sc.ring(2, [32, TN], F32, "wtr")
            sm = sc.ring(2, [128, 160], F32, "sm")
            def loadp(c0):
                xt, xk = xring.next()
                P.dma(xt[:], xv[:, :, c0:c0 + TN], reads=[srck], writes=[xk])
                at, atk = aring.next()
                P.dma(at[:], aTv[:, :, c0:c0 + TN], reads=["aTs"], writes=[atk])
                return xt, xk, at, atk
            nxt = loadp(t0)
            for c0 in range(t0, T, TN):
                s = 1 if c0 < LC else 0
                xt, xk, at, atk = nxt
                if c0 + TN < T:
                    nxt = loadp(c0 + TN)
                xn, xnk = xnring.next()
                for oc in range(KC):
                    ps_, pk_ = yp.next()
                    for k in range(KC):
                        P.mm(ps_[:], Wout[:, k, oc * 128:(oc + 1) * 128], at[:, k, :], k == 0, k == KC - 1,
                             ["Wout", atk], [pk_])
                    P.stt("vector", xn[:, oc, :], ps_[:], self.mvec(l, 2, oc, s), xt[:, oc, :], ALU.mult, ALU.add,
                          [pk_, xk, "mod"], [xnk])
                P.dma(xo[:, :, c0:c0 + TN], xn[:], reads=[xnk], writes=["xres"])
                hf, hfk = hfring.next()
                hb, hbk = hbring.next()
                self.rmsnorm_mod(sc, xn, xnk, TN, self.S2, 3, s, l, hb, hbk, out_f32=hf, out_f32_key=hfk, rings=rings)
                P.dma(h2v[:, :, c0:c0 + TN], hb[:], reads=[hbk], writes=["h2s"])
                wt_, wtk = wtr.next()
                for j in range(TN // 128):
                    lg, lgk = lp.next()
                    for k in range(KC):
                        P.mm(lg[:], hf[:, k, j * 128:(j + 1) * 128], wr[:, k, :], k == 0, k == KC - 1, [hfk, "wr"], [lgk])
                    m, mk = sm.next()
                    L = m[:, 0:36]
                    gmax = m[:, 36:37]
                    ngmax = m[:, 37:38]
                    gsum = m[:, 38:39]
                    gw = m[:, 39:40]
                    gmk = m[:, 40:44]
                    pen = m[:, 44:48]
                    Lm = m[:, 48:80]
                    mk1 = m[:, 80:112]
                    Lm2 = m[:, 112:144]
                    m1 = m[:, 144:145]
                    m2 = m[:, 145:146]
                    dd = m[:, 146:147]
                    ee = m[:, 147:148]
                    w1 = m[:, 148:149]
                    w2 = m[:, 149:150]
                    gex = m[:, 150:154]
                    rk = [mk]
                    P.tt("vector", L, lg[:], br[:], ALU.add, [lgk, "br"], rk)
                    P.rmax(gmax, m[:, 0:4], rk, rk)
                    P.ts("vector", ngmax, gmax, -1.0, None, ALU.mult, None, rk, rk)
                    P.ts("vector", gmk, m[:, 0:4], gmax, None, ALU.is_ge, None, rk, rk)
                    P.act(gex, m[:, 0:4], AF.Exp, rk, rk, bias=ngmax, scale=1.0, accum_out=gsum)
                    P.recip(gw, gsum, rk, rk)
                    P.ts("vector", pen, gmk, 1e30, -1e30, ALU.mult, ALU.add, rk, rk)
                    P.tt("vector", Lm.rearrange("p (g e) -> p g e", g=4), m[:, 4:36].rearrange("p (g e) -> p g e", g=4),
                         pen.unsqueeze(2).to_broadcast([128, 4, 8]), ALU.add, rk, rk)
                    P.rmax(m1, Lm, rk, rk)
                    P.ts("vector", mk1, Lm, m1, None, ALU.is_ge, None, rk, rk)
                    P.stt("vector", Lm2, mk1, -1e30, Lm, ALU.mult, ALU.add, rk, rk)
                    P.rmax(m2, Lm2, rk, rk)
                    mk2 = m[:, 48:80]
                    P.ts("vector", mk2, Lm2, m2, None, ALU.is_ge, None, rk, rk)
                    P.tt("vector", dd, m2, m1, ALU.subtract, rk, rk)
                    P.act(ee, dd, AF.Exp, rk, rk)
                    P.ts("vector", ee, ee, 1.0, None, ALU.add, None, rk, rk)
                    P.recip(ee, ee, rk, rk)
                    P.tt("vector", w1, gw, ee, ALU.mult, rk, rk)
                    P.tt("vector", w2, gw, w1, ALU.subtract, rk, rk)
                    wm = m[:, 112:144]
                    P.ts("vector", mk1, mk1, w1, None, ALU.mult, None, rk, rk)
                    P.stt("vector", wm, mk2, w2, mk1, ALU.mult, ALU.add, rk, rk)
                    tp, tpk = wtp.next()
                    P.mm(tp[:], wm, self.ident[:], True, True, rk + ["ident"], [tpk])
                    P.cp("scalar", wt_[:, j * 128:(j + 1) * 128], tp[:], [tpk], [wtk])
                P.dma(self.wts[:, c0:c0 + TN], wt_[:], reads=[wtk], writes=["wts"])
            if f"post{l}" in self.dbg:
                pass

    def phase_moe(self, l):
        P = self.P
        last = (l == 1)
        t0 = 0 if l == 0 else LC
        if l == 0:
            stiles = [[(0, 384), (384, 384), (768, 384)], [(0, 384), (384, 384), (768, 384)],
                      [(0, 512), (512, 512)], [(0, 512), (512, 512)]]
        else:
            stiles = [[(0, 512), (512, 512)]] * 4
        st_sizes = [sum(n for _, n in st) for st in stiles]
        STN = max(st_sizes)
        NT = 512
        xv = self.xres.rearrange("(k p) t -> p k t", p=128)
        h2v = self.h2s.rearrange("(k p) t -> p k t", p=128)
        outv = self.outT.rearrange("(k p) t -> p k t", p=128)
        w1v = self.w1[l].rearrange("e (k p) n -> e p k n", p=128)
        w3v = self.w3[l].rearrange("e (k p) n -> e p k n", p=128)
        w2v = self.w2[l].rearrange("e (k p) n -> e p k n", p=128)
        with P.scope() as sc:
            sel = sc.sb([32, NEXP * 128], F32, "sel")
            P.dma(sel[:], self.sel_d, writes=["sel"])
            yacc = sc.sb([128, KC, STN], F32, "yacc")
            h2 = sc.sb([128, KC, STN], BF16, "h2")
            wts = sc.sb([32, STN], F32, "wts_sb")
            W1b = sc.ring(2, [128, KC, DE], BF16, "W1b")
            W3b = sc.ring(2, [128, KC, DE], BF16, "W3b")
            W2b = sc.ring(2, [128, 4, D], BF16, "W2b")
            stg = sc.ring(6, [128, 4, DE], F32, "stgm")
            wbp = sc.ring(1, [128, NT], F32, "wbp", psum=True)
            ap_ = sc.ring(2, [128, NT], F32, "aps", psum=True)
            bp_ = sc.ring(2, [128, NT], F32, "bps", psum=True)
            ypr = sc.ring(3, [128, NT], F32, "ypm", psum=True)
            wbs = sc.ring(2, [128, NT], F32, "wbs")
            sar = sc.ring(2, [128, NT], F32, "sa")
            tr = sc.ring(2, [128, NT], F32, "tmoe")
            hbr = sc.ring(2, [128, 4, NT], BF16, "hb")
            FN = 256
            xfr = sc.ring(1, [128, KC, FN], F32, "xf")
            if last:
                rings = {
                    "xsq": sc.ring(1, [128, KC, FN], BF16, "xsq"),
                    "f32a": sc.ring(1, [128, FN], F32, "f32a"),
                    "f32b": sc.ring(1, [128, FN], F32, "f32b"),
                }
            seq = [(st_, e_) for st_ in range(4) for e_ in range(NEXP)]
            loaded = {}
            staged = {}

            def issue_dma(j):
                if j >= len(seq):
                    return
                e = seq[j][1]
                srcs = [w1v[e][:, 0:4, :], w1v[e][:, 4:8, :], w3v[e][:, 0:4, :], w3v[e][:, 4:8, :],
                        w2v[e][:, :, 0:DE], w2v[e][:, :, DE:2 * DE]]
                pieces = []
                for src in srcs:
                    st_, sk = stg.next()
                    P.dma(st_[:], src, writes=[sk])
                    pieces.append((st_, sk))
                staged[j] = pieces

            def issue_cast(j):
                if j >= len(seq):
                    return
                pieces = staged.pop(j)
                w1b, w1k = W1b.next()
                w3b, w3k = W3b.next()
                w2b, w2k = W2b.next()
                dsts = [w1b[:, 0:4, :], w1b[:, 4:8, :], w3b[:, 0:4, :], w3b[:, 4:8, :],
                        w2b[:, :, 0:DE], w2b[:, :, DE:2 * DE]]
                keys = [w1k, w1k, w3k, w3k, w2k, w2k]
                for (st_, sk), dst, kk in zip(pieces, dsts, keys):
                    P.cp("scalar", dst, st_[:], [sk], [kk])
                loaded[j] = (w1b, w1k, w3b, w3k, w2b, w2k)

            def emit_ab(W, e, c0, n):
                w1b, w1k, w3b, w3k, w2b, w2k = W
                wb, wbk = wbp.next()
                P.mm(wb[:, :n], sel[:, e * 128:(e + 1) * 128], wts[:, c0:c0 + n], True, True,
                     ["sel", "wts_sb"], [wbk])
                wb_s, wbsk = wbs.next()
                P.cp("scalar", wb_s[:, :n], wb[:, :n], [wbk], [wbsk])
                hb, hbk = hbr.next()
                for hc in range(4):
                    a_, ak = ap_.next()
                    b_, bk = bp_.next()
                    for k in range(KC):
                        P.mm(a_[:, :n], w1b[:, k, hc * 128:(hc + 1) * 128], h2[:, k, c0:c0 + n], k == 0,
                             k == KC - 1, [w1k, "h2"], [ak])
                    for k in range(KC):
                        P.mm(b_[:, :n], w3b[:, k, hc * 128:(hc + 1) * 128], h2[:, k, c0:c0 + n], k == 0,
                             k == KC - 1, [w3k, "h2"], [bk])
                    sa, sak = sar.next()
                    P.act(sa[:, :n], a_[:, :n], AF.Silu, [ak], [sak])
                    t_, tk = tr.next()
                    P.tt("vector", t_[:, :n], b_[:, :n], wb_s[:, :n], ALU.mult, [bk, wbsk], [tk])
                    P.tt("gpsimd", hb[:, hc, :n], t_[:, :n], sa[:, :n], ALU.mult, [tk, sak], [hbk])
                return hb, hbk

            def emit_w2(W, hb, hbk, ti, c0, n):
                w1b, w1k, w3b, w3k, w2b, w2k = W
                for dc in range(KC):
                    y_, yk = ypr.next()
                    for hc in range(4):
                        P.mm(y_[:, :n], w2b[:, hc, dc * 128:(dc + 1) * 128], hb[:, hc, :n], hc == 0, hc == 3,
                             [w2k, hbk], [yk])
                    P.tt("vector", yacc[:, dc, c0:c0 + n], y_[:, :n], yacc[:, dc, c0:c0 + n], ALU.add,
                         [yk, ("yacc", dc, ti)], [("yacc", dc, ti)])

            issue_dma(0)
            issue_cast(0)
            issue_dma(1)
            issue_cast(1)
            issue_dma(2)
            s0 = t0
            for st in range(4):
                tiles = stiles[st]
                stn = st_sizes[st]
                nti = len(tiles)
                P.dma(h2[:, :, 0:stn], h2v[:, :, s0:s0 + stn], reads=["h2s"], writes=["h2"])
                P.dma(wts[:, 0:stn], self.wts[:, s0:s0 + stn], reads=["wts"], writes=["wts_sb"])
                P.memset("gpsimd", yacc[:], 0.0, [("yacc", dc_, ti_) for dc_ in range(KC) for ti_ in range(4)])
                prev = None

                def flush(prev):
                    W, hb, hbk, ti, c0, n, lastt, j = prev
                    emit_w2(W, hb, hbk, ti, c0, n)
                    if lastt:
                        del loaded[j]
                        issue_cast(j + 2)
                        issue_dma(j + 3)

                for e in range(NEXP):
                    j = st * NEXP + e
                    W = loaded[j]
                    for ti, (c0, n) in enumerate(tiles):
                        hb, hbk = emit_ab(W, e, c0, n)
                        if prev is not None:
                            flush(prev)
                        prev = (W, hb, hbk, ti, c0, n, ti == nti - 1, j)
                flush(prev)
                for ti, (tc0, tn) in enumerate(tiles):
                    for cc in range(0, tn, FN):
                        n = min(FN, tn - cc)
                        c0 = tc0 + cc
                        g0 = s0 + c0
                        xf, xfk = xfr.next()
                        P.dma(xf[:, :, :n], xv[:, :, g0:g0 + n], reads=["xres"], writes=[xfk])
                        segs = []
                        if g0 < LC:
                            nb = min(LC, g0 + n) - g0
                            segs.append((0, nb, 1))
                            if nb < n:
                                segs.append((nb, n, 0))
                        else:
                            segs.append((0, n, 0))
                        for (a, b, s) in segs:
                            for k in range(KC):
                                P.stt("vector", xf[:, k, a:b], yacc[:, k, c0 + a:c0 + b], self.mvec(l, 5, k, s),
                                      xf[:, k, a:b], ALU.mult, ALU.add, [("yacc", k, ti), xfk, "mod"], [xfk])
                        if not last:
                            P.dma(xv[:, :, g0:g0 + n], xf[:, :, :n], reads=[xfk], writes=["xres"])
                        else:
                            xsq, xsqk = rings["xsq"].next()
                            P.act(xsq[:, :, :n], xf[:, :, :n], AF.Square, [xfk], [xsqk])
                            ssp, sspk = wbp.next()
                            for k in range(KC):
                                P.mm(ssp[:, :n], self.ones_all[:], xsq[:, k, :n], k == 0, k == KC - 1,
                                     [xsqk, "ones_all"], [sspk])
                            sq, sqk = rings["f32a"].next()
                            P.act(sq[:, :n], ssp[:, :n], AF.Sqrt, [sspk], [sqk], bias=self.eps_ap(), scale=1.0 / D)
                            rstd, rk = rings["f32b"].next()
                            P.recip(rstd[:, :n], sq[:, :n], [sqk], [rk])
                            for k in range(KC):
                                P.stt("vector", xf[:, k, :n], xf[:, k, :n], self.gfin_sb[:, k:k + 1], rstd[:, :n],
                                      ALU.mult, ALU.mult, [xfk, rk, "gfin"], [xfk])
                            P.dma(outv[:, :, g0 - LC:g0 - LC + n], xf[:, :, :n], reads=[xfk], writes=["outT"])
                s0 += stn


def _feat(v):
    return np.ascontiguousarray(np.asarray(v, np.float32).reshape(KC, 128).T)


def host_constants():
    cst = {}
    cst["ident"] = np.eye(128, dtype=np.float32)
    perm = np.zeros((128, 128), np.float32)
    for p in range(128):
        partner = p + 16 if (p % 32) < 16 else p - 16
        perm[partner, p] = 1.0
    cst["perm"] = perm
    t = np.arange(S)
    pos = np.stack([t // GRID, t % GRID], -1).astype(np.float32)
    inv = (10000.0 ** (-np.arange(16, dtype=np.float32) / 16)).astype(np.float32)
    C = np.ones((128, T), np.float32)
    Sn = np.zeros((128, T), np.float32)
    for p in range(128):
        pp = p % 64
        axis = pp // 32
        half = (pp % 32) // 16
        f = pp % 16
        ang = (pos[:, axis] * inv[f]).astype(np.float32)
        C[p, LC:] = np.cos(ang)
        Sn[p, LC:] = np.sin(ang) * (-1.0 if half == 0 else 1.0)
    cst["ropeC"] = C
    cst["ropeS"] = Sn
    ic = np.ones((128, 4, 2, 8), np.float32)
    for g, w in enumerate((2, 4, 8, 16)):
        for i in range(w // 2):
            ic[:, g, 0, i] = 1.0 / (i + w // 2)
        nr = w // 2 - 1
        for i in range(nr):
            ic[:, g, 1, i] = 1.0 / (nr - i + w // 2)
    cst["pool_ic"] = ic
    sel = np.zeros((32, NEXP, 128), np.float32)
    for e in range(NEXP):
        sel[e, e, :] = 1.0
    cst["sel"] = sel.reshape(32, NEXP * 128)
    return cst


def na_bias_tiles(rel_bias):
    H = rel_bias.shape[0]
    out = np.full((H, 14, 128, 256), NEG, np.float32)
    cols = np.arange(GRID)
    cstart = np.clip(cols - 8, 0, GRID - 16)

    def fill(tile, kr_abs, qr_abs_list, kh_slot):
        for qi, qr in enumerate(qr_abs_list):
            rs = min(max(qr - 4, 0), GRID - 8)
            if not (rs <= kr_abs < rs + 8):
                continue
            ro = kr_abs - qr + 7
            for qc in range(GRID):
                kcs = np.arange(cstart[qc], cstart[qc] + 16)
                out[:, tile, kh_slot * 64 + kcs, qi * 64 + qc] = rel_bias[:, ro, kcs - qc + 15]

    for j in range(6):
        for hslot in range(2):
            fill(j, 12 + 2 * j + hslot, [16, 17, 18, 19], hslot)
    for j in range(4):
        for hslot in range(2):
            fill(6 + j, 0 + 2 * j + hslot, [0, 1, 2, 3], hslot)
            fill(10 + j, 56 + 2 * j + hslot, [60, 61, 62, 63], hslot)
    return np.ascontiguousarray(out.transpose(0, 2, 1, 3))


def make_in_maps(inputs, ncores=8, layers=(0, 1), moe=True):
    f32 = lambda a: np.ascontiguousarray(np.asarray(a, np.float32))
    cst = host_constants()
    x = np.asarray(inputs["x"], np.float32)
    ctx = np.asarray(inputs["ctx"], np.float32)
    c = np.asarray(inputs["c"], np.float32)
    c_ctx = np.asarray(inputs["c_ctx"], np.float32)
    shared = {}
    shared["ada_w"] = f32(inputs["ada_w"])
    ada_b = np.asarray(inputs["ada_b"], np.float32)
    shared["adab"] = np.ascontiguousarray(ada_b.reshape(2, 48, 128).transpose(2, 0, 1))
    shared["gmix"] = np.ascontiguousarray(np.stack([_feat(inputs["norm_mix_g"][l]) for l in range(2)], 1))
    shared["gffn"] = np.ascontiguousarray(np.stack([_feat(inputs["norm_ffn_g"][l]) for l in range(2)], 1))
    shared["gfin"] = _feat(inputs["final_g"])
    shared["ident"] = cst["ident"]
    if 0 in layers:
        shared["e_win"] = f32(inputs["even_w_in"][0])
        shared["e_wout"] = f32(inputs["even_w_out"][0])
        gq = np.asarray(inputs["a_q_gain"][0], np.float32)
        gk = np.asarray(inputs["a_k_gain"][0], np.float32)
        shared["gqk"] = np.ascontiguousarray(np.stack([np.tile(gq, 2), np.tile(gk, 2)], 1))
        shared["perm"] = cst["perm"]
        shared["ropeC"] = cst["ropeC"]
        shared["ropeS"] = cst["ropeS"]
        shared["pool_w"] = f32(inputs["pool_w"][0])
        shared["pool_s"] = np.ascontiguousarray(np.asarray(inputs["pool_scale"][0], np.float32).reshape(4, 128).T)
        shared["pool_ic"] = cst["pool_ic"]
    if 1 in layers:
        shared["o_win"] = f32(inputs["odd_w_in"][0])
        shared["o_wout"] = f32(inputs["odd_w_out"][0])
        shared["nab"] = na_bias_tiles(np.asarray(inputs["na_rel_bias"][0], np.float32))
    shared["wr"] = np.ascontiguousarray(np.concatenate(
        [np.asarray(inputs["moe_w_group"], np.float32), np.asarray(inputs["moe_w_expert"], np.float32)], -1))
    brow = np.concatenate([np.asarray(inputs["moe_b_group"], np.float32),
                           np.asarray(inputs["moe_b_expert"], np.float32)], -1)
    shared["br"] = np.ascontiguousarray(np.broadcast_to(brow[None], (128, 2, 36)))
    if moe:
        shared["sel"] = cst["sel"]
        shared["moe_w1"] = f32(inputs["moe_w1"])
        shared["moe_w3"] = f32(inputs["moe_w3"])
        shared["moe_w2"] = f32(inputs["moe_w2"])
    maps = []
    for b in range(ncores):
        m = dict(shared)
        m["xin"] = np.ascontiguousarray(np.concatenate([ctx[b].T, x[b].T], 1))
        m["cc"] = np.ascontiguousarray(np.stack([_feat(c[b]), _feat(c_ctx)], -1))
        maps.append(m)
    return maps


def kernel(**inputs):
    nc = bass.Bass("TRN2", target_bir_lowering=False)
    bld = Builder(nc)
    bld.build()
    maps = make_in_maps(inputs, 8)
    maps = [{k: v for k, v in m.items() if k in bld.inputs} for m in maps]
    res = run_bass_kernel_spmd(nc, maps, core_ids=list(range(8)))
    bld.P.close()
    out = np.stack([np.ascontiguousarray(np.asarray(r["outT"]).T) for r in res.results], 0)
    return out.astype(np.float32)
```

```python
import numpy as np
import concourse.bass as bass
import concourse.mybir as mybir
from concourse.bass_utils import run_bass_kernel_spmd
from contextlib import ExitStack

F32 = mybir.dt.float32
BF16 = mybir.dt.bfloat16
AF = mybir.ActivationFunctionType
ALU = mybir.AluOpType
AX = mybir.AxisListType

COMPUTE = ("tensor", "vector", "scalar", "gpsimd")
ENGS = COMPUTE + ("sync",)
NDMASEM = 8

D = 1024
KC = 8
S = 4096
LC = 256
T = S + LC
EPS = 1e-6
NEXP = 32
DE = 512
GRID = 64
NEG = -30000.0


class Op:
    __slots__ = ("eng", "fn", "waits", "signal", "idx", "dma", "dsem", "dval")

    def __init__(self, eng, fn, dma):
        self.eng = eng
        self.fn = fn
        self.waits = []
        self.signal = False
        self.idx = None
        self.dma = dma
        self.dsem = None
        self.dval = None


class Prog:
    def __init__(self, nc):
        self.nc = nc
        self.es = ExitStack()
        self.queues = {e: [] for e in ENGS}
        self.lastw = {}
        self.readers = {}
        self.dma_ops = {e: [] for e in ENGS}
        self.n_tensors = 0
        self.fence = []
        self.fence_pending = {e: False for e in ENGS}

    def sb(self, shape, dt, name=None):
        self.n_tensors += 1
        return self.es.enter_context(self.nc.sbuf_tensor(f"sb{self.n_tensors}_{name or ''}", list(shape), dt))

    def ps(self, shape, dt=F32, name=None):
        self.n_tensors += 1
        return self.es.enter_context(self.nc.psum_tensor(f"ps{self.n_tensors}_{name or ''}", list(shape), dt))

    def scope(self):
        return Scope(self)

    def op(self, eng, fn, reads=(), writes=(), dma=False):
        o = Op(eng, fn, dma)
        deps = []
        for k in reads:
            w = self.lastw.get(k)
            if w is not None:
                deps.append(w)
        for k in writes:
            w = self.lastw.get(k)
            if w is not None:
                deps.append(w)
            deps.extend(self.readers.get(k, ()))
        seen = set()
        for d in deps:
            if id(d) in seen or d is o:
                continue
            seen.add(id(d))
            if (not d.dma) and (not dma) and d.eng == eng and eng == "tensor":
                continue
            o.waits.append(d)
            d.signal = True
        if self.fence_pending[eng]:
            for d in self.fence:
                if d is not o and d not in o.waits:
                    o.waits.append(d)
                    d.signal = True
            self.fence_pending[eng] = False
        if dma:
            lst = self.dma_ops[eng]
            if len(lst) >= NDMASEM:
                prev = lst[len(lst) - NDMASEM]
                if prev not in o.waits:
                    o.waits.append(prev)
            lst.append(o)
            o.signal = True
        for k in reads:
            self.readers.setdefault(k, []).append(o)
        for k in writes:
            self.lastw[k] = o
            self.readers[k] = []
        self.queues[eng].append(o)
        return o

    def barrier(self):
        fence = []
        for e in ENGS:
            for o in reversed(self.queues[e]):
                if not o.dma:
                    fence.append(o)
                    break
            fence.extend(self.dma_ops[e][-NDMASEM:])
        self.fence = fence
        self.fence_pending = {e: True for e in ENGS}

    def emit(self):
        nc = self.nc
        es = self.es
        csem = {e: es.enter_context(nc.semaphore(f"c_{e}")) for e in ENGS}
        dsem = {e: [es.enter_context(nc.semaphore(f"d_{e}{i}")) for i in range(NDMASEM)]
                for e in ENGS if self.dma_ops[e]}
        for e in ENGS:
            cnt = 0
            dcnt = [0] * NDMASEM
            n = 0
            for o in self.queues[e]:
                if o.dma:
                    slot = n % NDMASEM
                    n += 1
                    dcnt[slot] += 16
                    o.dsem = dsem[e][slot]
                    o.dval = dcnt[slot]
                elif o.signal:
                    cnt += 1
                    o.idx = cnt
        block = es.enter_context(nc.Block())
        queues = self.queues
        dma_ops = self.dma_ops

        def run(engname):
            def body(eng):
                seen = {}
                for o in queues[engname]:
                    for d in o.waits:
                        if d.dma:
                            sem, val = d.dsem, d.dval
                        else:
                            sem, val = csem[d.eng], d.idx
                        key = id(sem)
                        if seen.get(key, -1) >= val:
                            continue
                        seen[key] = val
                        eng.wait_ge(sem, val)
                    ins = o.fn(eng)
                    if o.dma:
                        ins.then_inc(o.dsem, 16)
                    elif o.signal:
                        ins.then_inc(csem[engname], 1)
                for d in dma_ops[engname][-NDMASEM:]:
                    if seen.get(id(d.dsem), -1) < d.dval:
                        seen[id(d.dsem)] = d.dval
                        eng.wait_ge(d.dsem, d.dval)
            return body

        block.tensor(run("tensor"))
        block.vector(run("vector"))
        block.scalar(run("scalar"))
        block.gpsimd(run("gpsimd"))
        block.sync(run("sync"))

    def close(self):
        self.es.close()

    def mm(self, out, lhsT, rhs, start, stop, reads, writes):
        return self.op("tensor", lambda e: e.matmul(out, lhsT, rhs, start=start, stop=stop), reads, writes)

    def act(self, out, in_, func, reads, writes, bias=None, scale=None, accum_out=None):
        kw = {}
        if bias is not None:
            kw["bias"] = bias
        if scale is not None:
            kw["scale"] = scale
        if accum_out is not None:
            kw["accum_out"] = accum_out
        return self.op("scalar", lambda e: e.activation(out=out, in_=in_, func=func, **kw), reads, writes)

    def tt(self, eng, out, in0, in1, op, reads, writes):
        return self.op(eng, lambda e: e.tensor_tensor(out, in0, in1, op), reads, writes)

    def stt(self, eng, out, in0, scalar, in1, op0, op1, reads, writes):
        return self.op(eng, lambda e: e.scalar_tensor_tensor(out=out, in0=in0, scalar=scalar, in1=in1,
                                                             op0=op0, op1=op1), reads, writes)

    def ts(self, eng, out, in0, s1, s2, op0, op1, reads, writes):
        if op1 is None:
            return self.op(eng, lambda e: e.tensor_scalar(out, in0, s1, None, op0), reads, writes)
        return self.op(eng, lambda e: e.tensor_scalar(out, in0, s1, s2, op0, op1), reads, writes)

    def cp(self, eng, out, in_, reads, writes):
        if eng == "scalar":
            return self.op(eng, lambda e: e.copy(out, in_), reads, writes)
        return self.op(eng, lambda e: e.tensor_copy(out, in_), reads, writes)

    def recip(self, out, in_, reads, writes):
        return self.op("vector", lambda e: e.reciprocal(out, in_), reads, writes)

    def memset(self, eng, ap, val, writes):
        return self.op(eng, lambda e: e.memset(ap, val), (), writes)

    def rmax(self, out, in_, reads, writes):
        return self.op("vector", lambda e: e.reduce_max(out, in_, AX.X), reads, writes)

    def dma(self, out, in_, reads=(), writes=(), eng="sync"):
        return self.op(eng, lambda e: e.dma_start(out=out, in_=in_), reads, writes, dma=True)


class Scope:
    def __init__(self, P):
        self.P = P
        self.es = ExitStack()

    def __enter__(self):
        self.es.__enter__()
        return self

    def __exit__(self, *a):
        r = self.es.__exit__(*a)
        self.P.barrier()
        return r

    def sb(self, shape, dt, name=None):
        self.P.n_tensors += 1
        return self.es.enter_context(self.P.nc.sbuf_tensor(f"sb{self.P.n_tensors}_{name or ''}", list(shape), dt))

    def ps(self, shape, dt=F32, name=None):
        self.P.n_tensors += 1
        return self.es.enter_context(self.P.nc.psum_tensor(f"ps{self.P.n_tensors}_{name or ''}", list(shape), dt))

    def ring(self, n, shape, dt, tag, psum=False):
        tiles = [(self.ps(shape, dt) if psum else self.sb(shape, dt)) for _ in range(n)]
        return Ring(tiles, tag)


class Ring:
    def __init__(self, tiles, tag):
        self.tiles = tiles
        self.tag = tag
        self.i = 0

    def next(self):
        j = self.i % len(self.tiles)
        self.i += 1
        return self.tiles[j], (self.tag, j)


class Builder:
    def __init__(self, nc, layers=(0, 1), moe=True, dbg=()):
        self.nc = nc
        self.P = Prog(nc)
        self.layers = layers
        self.moe = moe
        self.dbg = dbg
        self.inputs = {}
        self.outputs = {}

    def din(self, name, shape, dt=F32):
        t = self.nc.dram_tensor(name, list(shape), dt, kind="ExternalInput").ap()
        self.inputs[name] = t
        return t

    def dout(self, name, shape, dt=F32):
        t = self.nc.dram_tensor(name, list(shape), dt, kind="ExternalOutput").ap()
        self.outputs[name] = t
        return t

    def dscratch(self, name, shape, dt=F32):
        return self.nc.dram_tensor(name, list(shape), dt).ap()

    def build(self):
        P = self.P
        self.xin = self.din("xin", [D, T])
        self.cc = self.din("cc", [128, KC, 2])
        self.ada_w = self.din("ada_w", [2, D, 6 * D])
        self.adab = self.din("adab", [128, 2, 48])
        self.gmix = self.din("gmix", [128, 2, KC])
        self.gffn = self.din("gffn", [128, 2, KC])
        self.gfin = self.din("gfin", [128, KC])
        self.ident_d = self.din("ident", [128, 128])
        if 0 in self.layers:
            self.e_win = self.din("e_win", [D, 1280])
            self.e_wout = self.din("e_wout", [D, D])
            self.gqk = self.din("gqk", [128, 2])
            self.perm_d = self.din("perm", [128, 128])
            self.ropeC = self.din("ropeC", [128, T])
            self.ropeS = self.din("ropeS", [128, T])
            self.pool_w = self.din("pool_w", [4, 128, 128])
            self.pool_s = self.din("pool_s", [128, 4])
            self.pool_ic = self.din("pool_ic", [128, 4, 2, 8])
        if 1 in self.layers:
            self.o_win = self.din("o_win", [D, 3 * D])
            self.o_wout = self.din("o_wout", [D, D])
            self.nab = self.din("nab", [16, 128, 14, 256])
        self.wr = self.din("wr", [2, D, 36])
        self.br = self.din("br", [128, 2, 36])
        if self.moe:
            self.sel_d = self.din("sel", [32, NEXP * 128])
            self.w1 = self.din("moe_w1", [2, NEXP, D, DE])
            self.w3 = self.din("moe_w3", [2, NEXP, D, DE])
            self.w2 = self.din("moe_w2", [2, NEXP, DE, D])
        self.outT = self.dout("outT", [D, S])
        self.xres = self.dscratch("xres", [D, T])
        self.h2s = self.dscratch("h2s", [D, T], BF16)
        self.aTs = self.dscratch("aTs", [D, T], BF16)
        self.us = self.dscratch("us", [512, T])
        self.wts = self.dscratch("wts", [32, T])

        self.ident = P.sb([128, 128], F32, "ident")
        P.dma(self.ident[:], self.ident_d, writes=["ident"])
        self.ones_all = P.sb([128, 128], BF16, "ones_all")
        P.memset("vector", self.ones_all[:], 1.0, ["ones_all"])
        self.ones_bd = P.sb([128, 128], BF16, "ones_bd")
        P.memset("vector", self.ones_bd[:], 0.0, ["ones_bd"])
        P.memset("vector", self.ones_bd[0:64, 0:64], 1.0, ["ones_bd"])
        P.memset("vector", self.ones_bd[64:128, 64:128], 1.0, ["ones_bd"])
        self.mod = P.sb([128, 2, 48, 2], F32, "mod")
        self.S1 = P.sb([128, 2, KC, 2], F32, "S1")
        self.S2 = P.sb([128, 2, KC, 2], F32, "S2")
        self.gfin_sb = P.sb([128, KC], F32, "gfin_sb")
        P.dma(self.gfin_sb[:], self.gfin, writes=["gfin"])

        self.eps_ap()
        self.phase_mod()
        if 0 not in self.layers:
            xri = self.din("xres_in", [D, T])
            with P.scope() as sc:
                r = sc.ring(2, [128, T], F32, "xri")
                for r0 in range(0, D, 128):
                    t, k = r.next()
                    P.dma(t[:], xri[r0:r0 + 128, :], writes=[k])
                    P.dma(self.xres[r0:r0 + 128, :], t[:], reads=[k], writes=["xres"])
        if 0 in self.layers:
            self.layer0_front()
            self.dump("a0", self.aTs, D, T, BF16, "aTs")
            self.phase_post(0)
            self.dump("xp0", self.xres, D, T, F32, "xres")
            self.dump("h20", self.h2s, D, T, BF16, "h2s")
            self.dump("wt0", self.wts, 32, T, F32, "wts")
            if self.moe:
                self.phase_moe(0)
                self.dump("x0", self.xres, D, T, F32, "xres")
        if 1 in self.layers:
            self.layer1_front()
            self.dump("a1", self.aTs, D, T, BF16, "aTs")
            self.phase_post(1)
            self.dump("xp1", self.xres, D, T, F32, "xres")
            self.dump("h21", self.h2s, D, T, BF16, "h2s")
            self.dump("wt1", self.wts, 32, T, F32, "wts")
            if self.moe:
                self.phase_moe(1)
        P.emit()

    def dump(self, tag, src, R, C, dt, key):
        if tag not in self.dbg:
            return
        P = self.P
        o = self.dout("dbg_" + tag, [R, C], dt)
        with P.scope() as sc:
            r = sc.ring(2, [128, C], dt, "dump_" + tag)
            for r0 in range(0, R, 128):
                n = min(128, R - r0)
                t, k = r.next()
                P.dma(t[0:n, :], src[r0:r0 + n, :], reads=[key], writes=[k])
                P.dma(o[r0:r0 + n, :], t[0:n, :], reads=[k])

    def phase_mod(self):
        P = self.P
        with P.scope() as sc:
            cc = sc.sb([128, KC, 2], F32)
            cs = sc.sb([128, KC, 2], F32)
            adab = sc.sb([128, 2, 48], F32)
            gm = sc.sb([128, 2, KC], F32)
            gf = sc.sb([128, 2, KC], F32)
            P.dma(cc[:], self.cc, writes=["cc"])
            P.dma(adab[:], self.adab, writes=["adab"])
            P.dma(gm[:], self.gmix, writes=["gm"])
            P.dma(gf[:], self.gffn, writes=["gf"])
            P.act(cs[:], cc[:], AF.Silu, ["cc"], ["cs"])
            wring = sc.ring(2, [128, KC, 768], F32, "adaw")
            pm = sc.ps([128, 48, 2], F32)
            for l in self.layers:
                awl = self.ada_w[l].rearrange("(k p) n -> p k n", p=128)
                for blk in range(8):
                    wt, wk = wring.next()
                    P.dma(wt[:], awl[:, :, blk * 768:(blk + 1) * 768], writes=[wk])
                    for j in range(6):
                        oc = blk * 6 + j
                        for k in range(KC):
                            P.mm(pm[:, oc, :], wt[:, k, j * 128:(j + 1) * 128], cs[:, k, :],
                                 k == 0, k == KC - 1, [wk, "cs"], ["pm"])
                P.tt("vector", self.mod[:, l], pm[:], adab[:, l].unsqueeze(2).to_broadcast([128, 48, 2]),
                     ALU.add, ["pm", "adab"], ["mod"])
                P.stt("vector", self.S1[:, l], self.mod[:, l, 8:16, :], 1.0,
                      gm[:, l].unsqueeze(2).to_broadcast([128, KC, 2]), ALU.add, ALU.mult,
                      ["mod", "gm"], ["S1"])
                P.stt("vector", self.S2[:, l], self.mod[:, l, 32:40, :], 1.0,
                      gf[:, l].unsqueeze(2).to_broadcast([128, KC, 2]), ALU.add, ALU.mult,
                      ["mod", "gf"], ["S2"])
            if "mod" in self.dbg:
                o = self.dout("dbg_mod", [128, 2 * 48 * 2])
                P.dma(o, self.mod[:].rearrange("p a b c -> p (a b c)"), reads=["mod"])

    def mvec(self, l, kind, k, s):
        return self.mod[:, l, kind * 8 + k, s:s + 1]

    def load_cast(self, sc_ring, dst, src, shape, dkeys, eng="gpsimd"):
        P = self.P
        st, sk = sc_ring.next()
        view = st
        idx = tuple(slice(0, s) for s in shape)
        P.dma(view[idx], src, writes=[sk])
        P.cp(eng, dst, view[idx], [sk], dkeys)

    def rmsnorm_mod(self, sc, xt, xk, N, Svec, shk_l_kind, s, l, out_bf, out_bf_key, out_f32=None, out_f32_key=None,
                    rings=None):
        P = self.P
        xsq, xsqk = rings["xsq"].next()
        P.act(xsq[:, :, :N], xt[:, :, :N], AF.Square, [xk], [xsqk])
        ssp, sspk = rings["ssp"].next()
        for k in range(KC):
            P.mm(ssp[:, :N], self.ones_all[:], xsq[:, k, :N], k == 0, k == KC - 1, [xsqk, "ones_all"], [sspk])
        sq, sqk = rings["f32a"].next()
        P.act(sq[:, :N], ssp[:, :N], AF.Sqrt, [sspk], [sqk], bias=self.eps_ap(), scale=1.0 / D)
        rstd, rk = rings["f32b"].next()
        P.recip(rstd[:, :N], sq[:, :N], [sqk], [rk])
        for k in range(KC):
            tmp, tk = rings["f32c"].next()
            P.stt("vector", tmp[:, :N], xt[:, k, :N], Svec[:, l, k, s:s + 1], rstd[:, :N], ALU.mult, ALU.mult,
                  [xk, rk, "S1", "S2"], [tk])
            sh = self.mvec(l, shk_l_kind, k, s)
            if out_f32 is not None:
                P.act(out_f32[:, k, :N], tmp[:, :N], AF.Identity, [tk, "mod"], [out_f32_key], bias=sh, scale=1.0)
                P.cp("gpsimd", out_bf[:, k, :N], out_f32[:, k, :N], [out_f32_key], [out_bf_key])
            else:
                P.act(out_bf[:, k, :N], tmp[:, :N], AF.Identity, [tk, "mod"], [out_bf_key], bias=sh, scale=1.0)

    def eps_ap(self):
        if not hasattr(self, "_eps"):
            self._eps = self.P.sb([128, 1], F32, "eps")
            self.P.memset("vector", self._eps[:], EPS, ["eps"])
        return self._eps[:]

    def layer0_front(self):
        P = self.P
        l = 0
        TN = 256
        ntile = T // TN
        xin_v = self.xin.rearrange("(k p) t -> p k t", p=128)
        with P.scope() as sq_scope:
            QT = sq_scope.sb([128, 4, T], BF16, "QT")
            K2T = sq_scope.sb([128, 2, T], BF16, "K2T")
            Vaug = sq_scope.sb([128, 2, T // 128, 192], BF16, "Vaug")
            P.memset("gpsimd", Vaug[:, :, :, 64:128], 1.0, ["Vaug"])
            with P.scope() as sc:
                stg = sc.ring(2, [128, KC, 256], F32, "stg")
                Win = sc.sb([128, KC, 1280], BF16, "Win")
                Wk2 = sc.sb([128, KC, 256], BF16, "Wk2")
                ewv = self.e_win.rearrange("(k p) n -> p k n", p=128)
                for j in range(5):
                    self.load_cast(stg, Win[:, :, j * 256:(j + 1) * 256], ewv[:, :, j * 256:(j + 1) * 256],
                                   [128, KC, 256], ["Win"])
                for g in range(2):
                    for h in range(2):
                        P.cp("gpsimd", Wk2[:, :, g * 128 + h * 64: g * 128 + h * 64 + 64],
                             Win[:, :, 512 + g * 64: 512 + g * 64 + 64], ["Win"], ["Wk2"])
                gqk = sc.sb([128, 2], F32, "gqk")
                P.dma(gqk[:], self.gqk, writes=["gqk"])
                perm = sc.sb([128, 128], F32, "perm")
                P.dma(perm[:], self.perm_d, writes=["perm"])
                rings = {
                    "xsq": sc.ring(1, [128, KC, TN], BF16, "xsq"),
                    "ssp": sc.ring(1, [128, TN], F32, "ssp", psum=True),
                    "f32a": sc.ring(2, [128, TN], F32, "f32a"),
                    "f32b": sc.ring(2, [128, TN], F32, "f32b"),
                    "f32c": sc.ring(3, [128, TN], F32, "f32c"),
                }
                xring = sc.ring(2, [128, KC, TN], F32, "xt")
                hring = sc.ring(2, [128, KC, TN], BF16, "hT")
                cring = sc.ring(2, [128, TN], F32, "ropeC")
                sring = sc.ring(2, [128, TN], F32, "ropeS")
                pp = sc.ring(3, [128, TN], F32, "pp", psum=True)
                pq = sc.ring(1, [128, TN], F32, "pq", psum=True)
                pr = sc.ring(1, [128, TN], F32, "pr", psum=True)
                pv = sc.ring(1, [128, 128], F32, "pv", psum=True)
                qsqr = sc.ring(2, [128, TN], BF16, "qsq")
                qnr = sc.ring(2, [128, TN], F32, "qn")
                t1r = sc.ring(2, [128, TN], F32, "t1")
                t2r = sc.ring(2, [128, TN], F32, "t2")
                ur = sc.ring(2, [128, TN], F32, "ust")
                def load0(ti):
                    c0 = ti * TN
                    xt, xk = xring.next()
                    P.dma(xt[:], xin_v[:, :, c0:c0 + TN], writes=[xk])
                    ct, ck = cring.next()
                    st_, sk_ = sring.next()
                    P.dma(ct[:], self.ropeC[:, c0:c0 + TN], writes=[ck])
                    P.dma(st_[:], self.ropeS[:, c0:c0 + TN], writes=[sk_])
                    return xt, xk, ct, ck, st_, sk_
                nxt = load0(0)
                for ti in range(ntile):
                    c0 = ti * TN
                    s = 1 if ti == 0 else 0
                    xt, xk, ct, ck, st_, sk_ = nxt
                    if ti + 1 < ntile:
                        nxt = load0(ti + 1)
                    hT, hk = hring.next()
                    self.rmsnorm_mod(sc, xt, xk, TN, self.S1, 0, s, l, hT, hk, rings=rings)
                    for oc in range(6):
                        ps_, pk_ = pp.next()
                        for k in range(KC):
                            if oc < 4:
                                w = Win[:, k, oc * 128:(oc + 1) * 128]
                                wkey = "Win"
                            else:
                                w = Wk2[:, k, (oc - 4) * 128:(oc - 3) * 128]
                                wkey = "Wk2"
                            P.mm(ps_[:], w, hT[:, k, :], k == 0, k == KC - 1, [wkey, hk], [pk_])
                        gain = gqk[:, 0:1] if oc < 4 else gqk[:, 1:2]
                        qsq, qsqk = qsqr.next()
                        P.act(qsq[:], ps_[:], AF.Square, [pk_], [qsqk])
                        ssq, ssqk = pq.next()
                        P.mm(ssq[:], self.ones_bd[:], qsq[:], True, True, [qsqk, "ones_bd"], [ssqk])
                        sq, sqk = rings["f32a"].next()
                        P.act(sq[:], ssq[:], AF.Sqrt, [ssqk], [sqk], bias=self.eps_ap(), scale=1.0 / 64)
                        rs, rsk = rings["f32b"].next()
                        P.recip(rs[:], sq[:], [sqk], [rsk])
                        qn, qnk = qnr.next()
                        P.stt("vector", qn[:], ps_[:], gain, rs[:], ALU.mult, ALU.mult, [pk_, rsk, "gqk"], [qnk])
                        prp, prk = pr.next()
                        P.mm(prp[:], perm[:], qn[:], True, True, [qnk, "perm"], [prk])
                        t1, t1k = t1r.next()
                        P.tt("gpsimd", t1[:], qn[:], ct[:], ALU.mult, [qnk, ck], [t1k])
                        t2, t2k = t2r.next()
                        P.tt("vector", t2[:], prp[:], st_[:], ALU.mult, [prk, sk_], [t2k])
                        if oc < 4:
                            dst, dk = QT[:, oc, c0:c0 + TN], "QT"
                        else:
                            dst, dk = K2T[:, oc - 4, c0:c0 + TN], "K2T"
                        P.tt("gpsimd", dst, t1[:], t2[:], ALU.add, [t1k, t2k], [dk])
                    for j in range(TN // 128):
                        vp, vk = pv.next()
                        for k in range(KC):
                            P.mm(vp[:], hT[:, k, j * 128:(j + 1) * 128], Win[:, k, 640:768], k == 0, k == KC - 1,
                                 [hk, "Win"], [vk])
                        tix = c0 // 128 + j
                        for g in range(2):
                            P.cp("scalar", Vaug[:, g, tix, 0:64], vp[:, g * 64:(g + 1) * 64], [vk], ["Vaug"])
                            P.cp("vector", Vaug[:, g, tix, 128:192], vp[:, g * 64:(g + 1) * 64], [vk], ["Vaug"])
                    for g in range(4):
                        ps_, pk_ = pp.next()
                        for k in range(KC):
                            P.mm(ps_[:], Win[:, k, 768 + g * 128: 768 + (g + 1) * 128], hT[:, k, :], k == 0,
                                 k == KC - 1, ["Win", hk], [pk_])
                        ut, uk = ur.next()
                        P.cp("scalar", ut[:], ps_[:], [pk_], [uk])
                        P.dma(self.us[g * 128:(g + 1) * 128, c0:c0 + TN], ut[:], reads=[uk], writes=["us"])
                if "qkv" in self.dbg:
                    o = self.dout("dbg_QT", [128, 4 * T], BF16)
                    P.dma(o, QT[:].rearrange("p a t -> p (a t)"), reads=["QT"])
                    o = self.dout("dbg_K2T", [128, 2 * T], BF16)
                    P.dma(o, K2T[:].rearrange("p a t -> p (a t)"), reads=["K2T"])
                    o = self.dout("dbg_V", [128, 2 * (T // 128) * 192], BF16)
                    P.dma(o, Vaug[:].rearrange("p a t d -> p (a t d)"), reads=["Vaug"])
            with P.scope() as sc:
                PADL = 16
                WT = PADL + LC + PADL + S + PADL
                OFFC = PADL
                OFFX = PADL + LC + PADL
                pooled = sc.sb([128, 4, T], BF16, "pooled")
                pw = sc.sb([128, 4, 128], BF16, "pw")
                pws = sc.sb([128, 4, 128], F32, "pws")
                P.dma(pws[:], self.pool_w.rearrange("g c d -> c g d"), writes=["pws"])
                P.cp("gpsimd", pw[:], pws[:], ["pws"], ["pw"])
                psc = sc.sb([128, 4], F32, "psc")
                P.dma(psc[:], self.pool_s, writes=["psc"])
                pic = sc.sb([128, 4, 2, 8], F32, "pic")
                P.dma(pic[:], self.pool_ic, writes=["pic"])
                with P.scope() as sc2:
                    U = sc2.sb([128, WT], F32, "U")
                    A = sc2.sb([128, WT], F32, "A")
                    B = sc2.sb([128, WT], F32, "B")
                    for g in range(4):
                        w = (2, 4, 8, 16)[g]
                        P.memset("vector", U[:], 0.0, ["U"])
                        P.dma(U[:, OFFC:OFFC + LC], self.us[g * 128:(g + 1) * 128, 0:LC], reads=["us"], writes=["U"])
                        P.dma(U[:, OFFX:OFFX + S], self.us[g * 128:(g + 1) * 128, LC:T], reads=["us"], writes=["U"])
                        lo, hi = 8, WT - 8
                        P.memset("gpsimd", A[:], 0.0, ["A"])
                        P.memset("gpsimd", B[:], 0.0, ["B"])
                        P.tt("vector", A[:, lo:hi], U[:, lo - 1:hi - 1], U[:, lo:hi], ALU.add, ["U"], ["A"])
                        cur, curk, oth, othk = A, "A", B, "B"
                        sh = 1
                        ww = 2
                        while ww < w:
                            d = ww // 2
                            P.tt("vector", oth[:, lo:hi], cur[:, lo - d:hi - d], cur[:, lo + d:hi + d], ALU.add,
                                 [curk], [othk])
                            cur, curk, oth, othk = oth, othk, cur, curk
                            ww *= 2
                        pl, plk = oth, othk
                        P.stt("vector", pl[:, lo:hi], cur[:, lo:hi], 1.0 / w, U[:, lo:hi], ALU.mult, ALU.subtract,
                              [curk, "U"], [plk])
                        for (off, L) in ((OFFC, LC), (OFFX, S)):
                            nl = w // 2
                            tmpb = sc2.sb([128, 8], F32)
                            P.tt("vector", tmpb[:, 0:nl], cur[:, off:off + nl], pic[:, g, 0, 0:nl], ALU.mult,
                                 [curk, "pic"], ["tmpb"])
                            P.tt("vector", pl[:, off:off + nl], tmpb[:, 0:nl], U[:, off:off + nl], ALU.subtract,
                                 ["tmpb", "U"], [plk])
                            nr = w // 2 - 1
                            if nr > 0:
                                r0 = off + L - nr
                                tmpc = sc2.sb([128, 8], F32)
                                P.tt("vector", tmpc[:, 0:nr], cur[:, r0:r0 + nr], pic[:, g, 1, 0:nr], ALU.mult,
                                     [curk, "pic"], ["tmpc"])
                                P.tt("vector", pl[:, r0:r0 + nr], tmpc[:, 0:nr], U[:, r0:r0 + nr], ALU.subtract,
                                     ["tmpc", "U"], [plk])
                        P.cp("gpsimd", pooled[:, g, 0:LC], pl[:, OFFC:OFFC + LC], [plk], ["pooled"])
                        P.cp("gpsimd", pooled[:, g, LC:T], pl[:, OFFX:OFFX + S], [plk], ["pooled"])
                self.attention0(QT, K2T, Vaug)
                with P.scope() as sc2:
                    pmm = sc2.ring(2, [128, 512], F32, "pmm", psum=True)
                    ob = sc2.ring(2, [128, 512], BF16, "pob")
                    for g in range(4):
                        for c0 in range(0, T, 512):
                            n = min(512, T - c0)
                            ps_, pk_ = pmm.next()
                            P.mm(ps_[:, :n], pw[:, g, :], pooled[:, g, c0:c0 + n], True, True, ["pw", "pooled"], [pk_])
                            o_, ok_ = ob.next()
                            P.act(o_[:, :n], ps_[:, :n], AF.Identity, [pk_, "psc"], [ok_], scale=psc[:, g:g + 1])
                            P.dma(self.aTs[(4 + g) * 128:(5 + g) * 128, c0:c0 + n], o_[:, :n], reads=[ok_],
                                  writes=["aTs"])

    def attention0(self, QT, K2T, Vaug):
        P = self.P
        A_SCALE = 64 ** -0.5
        with P.scope() as sc:
            sps = sc.ring(4, [128, 512], F32, "sps", psum=True)
            oA = sc.ring(2, [128, 512], F32, "oA", psum=True)
            oB = sc.ring(2, [128, 512], F32, "oB", psum=True)
            pt = sc.ring(4, [128, 512], BF16, "pt")
            Rr = sc.ring(2, [128, 512], F32, "R")
            ao = sc.ring(2, [128, 512], BF16, "ao")
            qtiles = [(0, LC, 2)] + [(LC + i * 512, 512, T // 128) for i in range(S // 512)]
            for c in range(4):
                g = c // 2
                for (q0, N, nkc) in qtiles:
                    a_, ak = oA.next()
                    b_, bk = oB.next()
                    iters = [(kc, h) for kc in range(nkc) for h in range(2)]
                    LA = 3
                    pend = {}

                    def emitS(i, g=g, c=c, q0=q0, N=N, iters=iters, pend=pend):
                        kc, h = iters[i]
                        hp = slice(h * 64, h * 64 + 64)
                        sp_, spk = sps.next()
                        P.mm(sp_[:, :N], K2T[hp, g, kc * 128:(kc + 1) * 128], QT[hp, c, q0:q0 + N], True, True,
                             ["K2T", "QT"], [spk])
                        pend[i] = (sp_, spk)

                    for i in range(min(LA, len(iters))):
                        emitS(i)
                    for i in range(len(iters)):
                        if i + LA < len(iters):
                            emitS(i + LA)
                        kc, h = iters[i]
                        sp_, spk = pend.pop(i)
                        p_, pk_ = pt.next()
                        P.act(p_[:, :N], sp_[:, :N], AF.Exp, [spk], [pk_], scale=A_SCALE)
                        if h == 0:
                            P.mm(a_[:, :N], Vaug[:, g, kc, 0:128], p_[:, :N], kc == 0, kc == nkc - 1,
                                 ["Vaug", pk_], [ak])
                        else:
                            P.mm(b_[:, :N], Vaug[:, g, kc, 64:192], p_[:, :N], kc == 0, kc == nkc - 1,
                                 ["Vaug", pk_], [bk])
                    R, Rk = Rr.next()
                    P.recip(R[0:64, :N], a_[64:128, :N], [ak], [Rk])
                    P.recip(R[64:128, :N], b_[0:64, :N], [bk], [Rk])
                    o_, ok_ = ao.next()
                    P.tt("vector", o_[0:64, :N], a_[0:64, :N], R[0:64, :N], ALU.mult, [ak, Rk], [ok_])
                    P.tt("vector", o_[64:128, :N], b_[64:128, :N], R[64:128, :N], ALU.mult, [bk, Rk], [ok_])
                    P.dma(self.aTs[c * 128:(c + 1) * 128, q0:q0 + N], o_[:, :N], reads=[ok_], writes=["aTs"])

    def layer1_front(self):
        P = self.P
        l = 1
        TN = 256
        ntile = T // TN
        xv = self.xres.rearrange("(k p) t -> p k t", p=128)
        C_SCALE = 64 ** -0.5
        with P.scope() as sc0:
            hT = sc0.sb([128, KC, T], BF16, "hT1")
            with P.scope() as sc:
                rings = {
                    "xsq": sc.ring(1, [128, KC, TN], BF16, "xsq"),
                    "ssp": sc.ring(1, [128, TN], F32, "ssp", psum=True),
                    "f32a": sc.ring(2, [128, TN], F32, "f32a"),
                    "f32b": sc.ring(2, [128, TN], F32, "f32b"),
                    "f32c": sc.ring(3, [128, TN], F32, "f32c"),
                }
                xring = sc.ring(2, [128, KC, TN], F32, "xt")
                hring = sc.ring(2, [128, KC, TN], BF16, "hTt")
                def load1(ti):
                    c0 = ti * TN
                    xt, xk = xring.next()
                    P.dma(xt[:], xv[:, :, c0:c0 + TN], reads=["xres"], writes=[xk])
                    return xt, xk
                nxt = load1(0)
                for ti in range(ntile):
                    c0 = ti * TN
                    s = 1 if ti == 0 else 0
                    xt, xk = nxt
                    if ti + 1 < ntile:
                        nxt = load1(ti + 1)
                    ht, hk = hring.next()
                    self.rmsnorm_mod(sc, xt, xk, TN, self.S1, 0, s, l, ht, hk, rings=rings)
                    P.cp("gpsimd", hT[:, :, c0:c0 + TN], ht[:], [hk], ["hT1"])
            if "h1" in self.dbg:
                o = self.dout("dbg_h1", [128, KC * T], BF16)
                P.dma(o, hT[:].rearrange("p a t -> p (a t)"), reads=["hT1"])
            with P.scope() as sc:
                owv = self.o_win.rearrange("(k p) n -> p k n", p=128)
                stg = sc.ring(2, [128, KC, 128], F32, "stg1")
                wq = sc.ring(2, [128, KC, 128], BF16, "wq")
                wk = sc.ring(2, [128, KC, 128], BF16, "wk")
                wv = sc.ring(2, [128, KC, 128], BF16, "wv")
                qT = sc.sb([128, S], BF16, "qT1")
                kT = sc.sb([128, T], BF16, "kT1")
                Vaug = sc.sb([128, T // 128, 192], BF16, "Vaug1")
                P.memset("gpsimd", Vaug[:, :, 64:128], 1.0, ["Vaug1"])
                bias = sc.sb([128, 2, 14, 256], F32, "nab")
                pp = sc.ring(2, [128, 512], F32, "pp1", psum=True)
                pv = sc.ring(1, [128, 128], F32, "pv1", psum=True)
                sps = sc.ring(3, [128, 256], F32, "sps1", psum=True)
                oA = sc.ring(1, [128, 256], F32, "oA1", psum=True)
                oB = sc.ring(1, [128, 256], F32, "oB1", psum=True)
                ssb = sc.ring(3, [128, 256], F32, "ssb1")
                pt = sc.ring(4, [128, 256], BF16, "pt1")
                Rr = sc.ring(2, [128, 256], F32, "R1")
                ao = sc.ring(2, [128, 256], BF16, "ao1")
                for c in range(8):
                    wq_, wqk = wq.next()
                    wk_, wkk = wk.next()
                    wv_, wvk = wv.next()
                    self.load_cast(stg, wq_[:], owv[:, :, c * 128:(c + 1) * 128], [128, KC, 128], [wqk])
                    self.load_cast(stg, wk_[:], owv[:, :, D + c * 128:D + (c + 1) * 128], [128, KC, 128], [wkk])
                    self.load_cast(stg, wv_[:], owv[:, :, 2 * D + c * 128:2 * D + (c + 1) * 128], [128, KC, 128], [wvk])
                    for hh in range(2):
                        P.dma(bias[:, hh], self.nab[2 * c + hh], writes=["nab"])
                    for c0 in range(0, S, 512):
                        ps_, pk_ = pp.next()
                        for k in range(KC):
                            P.mm(ps_[:], wq_[:, k, :], hT[:, k, LC + c0:LC + c0 + 512], k == 0, k == KC - 1,
                                 [wqk, "hT1"], [pk_])
                        P.act(qT[:, c0:c0 + 512], ps_[:], AF.Identity, [pk_], ["qT1"], scale=C_SCALE)
                    for c0 in range(0, T, 512):
                        n = min(512, T - c0)
                        ps_, pk_ = pp.next()
                        for k in range(KC):
                            P.mm(ps_[:, :n], wk_[:, k, :], hT[:, k, c0:c0 + n], k == 0, k == KC - 1,
                                 [wkk, "hT1"], [pk_])
                        P.cp("vector", kT[:, c0:c0 + n], ps_[:, :n], [pk_], ["kT1"])
                    for tix in range(T // 128):
                        vp, vk = pv.next()
                        for k in range(KC):
                            P.mm(vp[:], hT[:, k, tix * 128:(tix + 1) * 128], wv_[:, k, :], k == 0, k == KC - 1,
                                 ["hT1", wvk], [vk])
                        P.cp("scalar", Vaug[:, tix, 0:64], vp[:, 0:64], [vk], ["Vaug1"])
                        P.cp("vector", Vaug[:, tix, 128:192], vp[:, 64:128], [vk], ["Vaug1"])
                    for qb in range(16):
                        if qb == 0:
                            kr0, nloc, tb = 0, 4, 6
                        elif qb == 15:
                            kr0, nloc, tb = 56, 4, 10
                        else:
                            kr0, nloc, tb = 4 * qb - 4, 6, 0
                        chunks = [(LC + 64 * kr0 + 128 * j, tb + j) for j in range(nloc)] + [(0, None), (128, None)]
                        a_, ak = oA.next()
                        b_, bk = oB.next()
                        q0 = qb * 256
                        nch = len(chunks)
                        iters = [(ci, h) for ci in range(nch) for h in range(2)]
                        LA = 2
                        pend = {}

                        def emitS1(i, iters=iters, chunks=chunks, q0=q0, pend=pend):
                            ci, h = iters[i]
                            k0 = chunks[ci][0]
                            hp = slice(h * 64, h * 64 + 64)
                            sp_, spk = sps.next()
                            P.mm(sp_[:], kT[hp, k0:k0 + 128], qT[hp, q0:q0 + 256], True, True,
                                 ["kT1", "qT1"], [spk])
                            pend[i] = (sp_, spk)

                        for i in range(LA):
                            emitS1(i)
                        for i in range(len(iters)):
                            if i + LA < len(iters):
                                emitS1(i + LA)
                            ci, h = iters[i]
                            k0, bt = chunks[ci]
                            sp_, spk = pend.pop(i)
                            p_, pk_ = pt.next()
                            if bt is not None:
                                sb_, sbk = ssb.next()
                                P.tt("vector", sb_[:], sp_[:], bias[:, h, bt, :], ALU.add, [spk, "nab"], [sbk])
                                P.act(p_[:], sb_[:], AF.Exp, [sbk], [pk_])
                            else:
                                P.act(p_[:], sp_[:], AF.Exp, [spk], [pk_])
                            tix = k0 // 128
                            if h == 0:
                                P.mm(a_[:], Vaug[:, tix, 0:128], p_[:], ci == 0, ci == nch - 1, ["Vaug1", pk_], [ak])
                            else:
                                P.mm(b_[:], Vaug[:, tix, 64:192], p_[:], ci == 0, ci == nch - 1, ["Vaug1", pk_], [bk])
                        R, Rk = Rr.next()
                        P.recip(R[0:64, :], a_[64:128, :], [ak], [Rk])
                        P.recip(R[64:128, :], b_[0:64, :], [bk], [Rk])
                        o_, ok_ = ao.next()
                        P.tt("vector", o_[0:64, :], a_[0:64, :], R[0:64, :], ALU.mult, [ak, Rk], [ok_])
                        P.tt("vector", o_[64:128, :], b_[64:128, :], R[64:128, :], ALU.mult, [bk, Rk], [ok_])
                        P.dma(self.aTs[c * 128:(c + 1) * 128, LC + q0:LC + q0 + 256], o_[:], reads=[ok_],
                              writes=["aTs"])

    def phase_post(self, l):
        P = self.P
        TN = 256
        t0 = 0 if l == 0 else LC
        src = self.xin if l == 0 else self.xres
        srck = "xin" if l == 0 else "xres"
        xv = src.rearrange("(k p) t -> p k t", p=128)
        xo = self.xres.rearrange("(k p) t -> p k t", p=128)
        h2v = self.h2s.rearrange("(k p) t -> p k t", p=128)
        aTv = self.aTs.rearrange("(k p) t -> p k t", p=128)
        wout_d = (self.e_wout if l == 0 else self.o_wout).rearrange("(k p) n -> p k n", p=128)
        with P.scope() as sc:
            stg = sc.ring(2, [128, KC, 256], F32, "stgp")
            Wout = sc.sb([128, KC, D], BF16, "Wout")
            for j in range(4):
                self.load_cast(stg, Wout[:, :, j * 256:(j + 1) * 256], wout_d[:, :, j * 256:(j + 1) * 256],
                               [128, KC, 256], ["Wout"])
            wr = sc.sb([128, KC, 36], F32, "wr")
            P.dma(wr[:], self.wr[l].rearrange("(k p) n -> p k n", p=128), writes=["wr"])
            br = sc.sb([128, 36], F32, "br")
            P.dma(br[:], self.br[:, l, :], writes=["br"])
            rings = {
                "xsq": sc.ring(1, [128, KC, TN], BF16, "xsq"),
                "ssp": sc.ring(1, [128, TN], F32, "ssp", psum=True),
                "f32a": sc.ring(2, [128, TN], F32, "f32a"),
                "f32b": sc.ring(2, [128, TN], F32, "f32b"),
                "f32c": sc.ring(3, [128, TN], F32, "f32c"),
            }
            xring = sc.ring(2, [128, KC, TN], F32, "xtp")
            aring = sc.ring(2, [128, KC, TN], BF16, "aTt")
            xnring = sc.ring(2, [128, KC, TN], F32, "xn")
            hfring = sc.ring(1, [128, KC, TN], F32, "h2f")
            hbring = sc.ring(2, [128, KC, TN], BF16, "h2b")
            yp = sc.ring(3, [128, TN], F32, "yp", psum=True)
            lp = sc.ring(1, [128, 36], F32, "lp", psum=True)
            wtp = sc.ring(1, [32, 128], F32, "wtp", psum=True)
            wtr = sc.ring(2, [32, TN], F32, "wtr")
            sm = sc.ring(2, [128, 160], F32, "sm")
            def loadp(c0):
                xt, xk = xring.next()
                P.dma(xt[:], xv[:, :, c0:c0 + TN], reads=[srck], writes=[xk])
                at, atk = aring.next()
                P.dma(at[:], aTv[:, :, c0:c0 + TN], reads=["aTs"], writes=[atk])
                return xt, xk, at, atk
            nxt = loadp(t0)
            for c0 in range(t0, T, TN):
                s = 1 if c0 < LC else 0
                xt, xk, at, atk = nxt
                if c0 + TN < T:
                    nxt = loadp(c0 + TN)
                xn, xnk = xnring.next()
                for oc in range(KC):
                    ps_, pk_ = yp.next()
                    for k in range(KC):
                        P.mm(ps_[:], Wout[:, k, oc * 128:(oc + 1) * 128], at[:, k, :], k == 0, k == KC - 1,
                             ["Wout", atk], [pk_])
                    P.stt("vector", xn[:, oc, :], ps_[:], self.mvec(l, 2, oc, s), xt[:, oc, :], ALU.mult, ALU.add,
                          [pk_, xk, "mod"], [xnk])
                P.dma(xo[:, :, c0:c0 + TN], xn[:], reads=[xnk], writes=["xres"])
                hf, hfk = hfring.next()
                hb, hbk = hbring.next()
                self.rmsnorm_mod(sc, xn, xnk, TN, self.S2, 3, s, l, hb, hbk, out_f32=hf, out_f32_key=hfk, rings=rings)
                P.dma(h2v[:, :, c0:c0 + TN], hb[:], reads=[hbk], writes=["h2s"])
                wt_, wtk = wtr.next()
                for j in range(TN // 128):
                    lg, lgk = lp.next()
                    for k in range(KC):
                        P.mm(lg[:], hf[:, k, j * 128:(j + 1) * 128], wr[:, k, :], k == 0, k == KC - 1, [hfk, "wr"], [lgk])
                    m, mk = sm.next()
                    L = m[:, 0:36]
                    gmax = m[:, 36:37]
                    ngmax = m[:, 37:38]
                    gsum = m[:, 38:39]
                    gw = m[:, 39:40]
                    gmk = m[:, 40:44]
                    pen = m[:, 44:48]
                    Lm = m[:, 48:80]
                    mk1 = m[:, 80:112]
                    Lm2 = m[:, 112:144]
                    m1 = m[:, 144:145]
                    m2 = m[:, 145:146]
                    dd = m[:, 146:147]
                    ee = m[:, 147:148]
                    w1 = m[:, 148:149]
                    w2 = m[:, 149:150]
                    gex = m[:, 150:154]
                    rk = [mk]
                    P.tt("vector", L, lg[:], br[:], ALU.add, [lgk, "br"], rk)
                    P.rmax(gmax, m[:, 0:4], rk, rk)
                    P.ts("vector", ngmax, gmax, -1.0, None, ALU.mult, None, rk, rk)
                    P.ts("vector", gmk, m[:, 0:4], gmax, None, ALU.is_ge, None, rk, rk)
                    P.act(gex, m[:, 0:4], AF.Exp, rk, rk, bias=ngmax, scale=1.0, accum_out=gsum)
                    P.recip(gw, gsum, rk, rk)
                    P.ts("vector", pen, gmk, 1e30, -1e30, ALU.mult, ALU.add, rk, rk)
                    P.tt("vector", Lm.rearrange("p (g e) -> p g e", g=4), m[:, 4:36].rearrange("p (g e) -> p g e", g=4),
                         pen.unsqueeze(2).to_broadcast([128, 4, 8]), ALU.add, rk, rk)
                    P.rmax(m1, Lm, rk, rk)
                    P.ts("vector", mk1, Lm, m1, None, ALU.is_ge, None, rk, rk)
                    P.stt("vector", Lm2, mk1, -1e30, Lm, ALU.mult, ALU.add, rk, rk)
                    P.rmax(m2, Lm2, rk, rk)
                    mk2 = m[:, 48:80]
                    P.ts("vector", mk2, Lm2, m2, None, ALU.is_ge, None, rk, rk)
                    P.tt("vector", dd, m2, m1, ALU.subtract, rk, rk)
                    P.act(ee, dd, AF.Exp, rk, rk)
                    P.ts("vector", ee, ee, 1.0, None, ALU.add, None, rk, rk)
                    P.recip(ee, ee, rk, rk)
                    P.tt("vector", w1, gw, ee, ALU.mult, rk, rk)
                    P.tt("vector", w2, gw, w1, ALU.subtract, rk, rk)
                    wm = m[:, 112:144]
                    P.ts("vector", mk1, mk1, w1, None, ALU.mult, None, rk, rk)
                    P.stt("vector", wm, mk2, w2, mk1, ALU.mult, ALU.add, rk, rk)
                    tp, tpk = wtp.next()
                    P.mm(tp[:], wm, self.ident[:], True, True, rk + ["ident"], [tpk])
                    P.cp("scalar", wt_[:, j * 128:(j + 1) * 128], tp[:], [tpk], [wtk])
                P.dma(self.wts[:, c0:c0 + TN], wt_[:], reads=[wtk], writes=["wts"])
            if f"post{l}" in self.dbg:
                pass

    def phase_moe(self, l):
        P = self.P
        last = (l == 1)
        t0 = 0 if l == 0 else LC
        if l == 0:
            stiles = [[(0, 384), (384, 384), (768, 384)], [(0, 384), (384, 384), (768, 384)],
                      [(0, 512), (512, 512)], [(0, 512), (512, 512)]]
        else:
            stiles = [[(0, 512), (512, 512)]] * 4
        st_sizes = [sum(n for _, n in st) for st in stiles]
        STN = max(st_sizes)
        NT = 512
        xv = self.xres.rearrange("(k p) t -> p k t", p=128)
        h2v = self.h2s.rearrange("(k p) t -> p k t", p=128)
        outv = self.outT.rearrange("(k p) t -> p k t", p=128)
        w1v = self.w1[l].rearrange("e (k p) n -> e p k n", p=128)
        w3v = self.w3[l].rearrange("e (k p) n -> e p k n", p=128)
        w2v = self.w2[l].rearrange("e (k p) n -> e p k n", p=128)
        with P.scope() as sc:
            stg = sc.ring(6, [128, 4, DE], F32, "stgm")
            sel = sc.sb([32, NEXP * 128], BF16, "sel")
            for q_ in range(4):
                self.load_cast(stg, sel[:, q_ * 1024:(q_ + 1) * 1024].rearrange("p (a b) -> p a b", a=2),
                               self.sel_d[:, q_ * 1024:(q_ + 1) * 1024].rearrange("p (a b) -> p a b", a=2),
                               [32, 2, 512], ["sel"], eng="vector")
            yacc = sc.sb([128, KC, STN], F32, "yacc")
            h2 = sc.sb([128, KC, STN], BF16, "h2")
            wts = sc.sb([32, STN], BF16, "wts_sb")
            wtsf = sc.sb([32, STN], F32, "wts_f")
            W1b = sc.ring(2, [128, KC, DE], BF16, "W1b")
            W3b = sc.ring(2, [128, KC, DE], BF16, "W3b")
            W2b = sc.ring(2, [128, 4, D], BF16, "W2b")
            wbp = sc.ring(1, [128, NT], F32, "wbp", psum=True)
            ap_ = sc.ring(2, [128, NT], F32, "aps", psum=True)
            bp_ = sc.ring(2, [128, NT], F32, "bps", psum=True)
            ypr = sc.ring(3, [128, NT], F32, "ypm", psum=True)
            wbs = sc.ring(2, [128, NT], F32, "wbs")
            sar = sc.ring(2, [128, NT], F32, "sa")
            tr = sc.ring(2, [128, NT], F32, "tmoe")
            hbr = sc.ring(2, [128, 4, NT], BF16, "hb")
            FN = 256
            xfr = sc.ring(2, [128, KC, FN], F32, "xf")
            if last:
                rings = {
                    "xsq": sc.ring(1, [128, KC, FN], BF16, "xsq"),
                    "f32a": sc.ring(1, [128, FN], F32, "f32a"),
                    "f32b": sc.ring(1, [128, FN], F32, "f32b"),
                }
            seq = [(st_, e_) for st_ in range(4) for e_ in range(NEXP)]
            loaded = {}
            staged = {}

            def issue_dma(j):
                if j >= len(seq):
                    return
                e = seq[j][1]
                srcs = [w1v[e][:, 0:4, :], w1v[e][:, 4:8, :], w3v[e][:, 0:4, :], w3v[e][:, 4:8, :],
                        w2v[e][:, :, 0:DE], w2v[e][:, :, DE:2 * DE]]
                pieces = []
                for src in srcs:
                    st_, sk = stg.next()
                    P.dma(st_[:], src, writes=[sk])
                    pieces.append((st_, sk))
                staged[j] = pieces

            def issue_cast(j):
                if j >= len(seq):
                    return
                pieces = staged.pop(j)
                w1b, w1k = W1b.next()
                w3b, w3k = W3b.next()
                w2b, w2k = W2b.next()
                dsts = [w1b[:, 0:4, :], w1b[:, 4:8, :], w3b[:, 0:4, :], w3b[:, 4:8, :],
                        w2b[:, :, 0:DE], w2b[:, :, DE:2 * DE]]
                keys = [w1k, w1k, w3k, w3k, w2k, w2k]
                for (st_, sk), dst, kk in zip(pieces, dsts, keys):
                    P.cp("scalar", dst, st_[:], [sk], [kk])
                loaded[j] = (w1b, w1k, w3b, w3k, w2b, w2k)

            def emit_ab(W, e, c0, n):
                w1b, w1k, w3b, w3k, w2b, w2k = W
                wb, wbk = wbp.next()
                P.mm(wb[:, :n], sel[:, e * 128:(e + 1) * 128], wts[:, c0:c0 + n], True, True,
                     ["sel", "wts_sb"], [wbk])
                wb_s, wbsk = wbs.next()
                P.cp("scalar", wb_s[:, :n], wb[:, :n], [wbk], [wbsk])
                hb, hbk = hbr.next()
                for hc in range(4):
                    a_, ak = ap_.next()
                    b_, bk = bp_.next()
                    for k in range(KC):
                        P.mm(a_[:, :n], w1b[:, k, hc * 128:(hc + 1) * 128], h2[:, k, c0:c0 + n], k == 0,
                             k == KC - 1, [w1k, "h2"], [ak])
                    for k in range(KC):
                        P.mm(b_[:, :n], w3b[:, k, hc * 128:(hc + 1) * 128], h2[:, k, c0:c0 + n], k == 0,
                             k == KC - 1, [w3k, "h2"], [bk])
                    sa, sak = sar.next()
                    P.act(sa[:, :n], a_[:, :n], AF.Silu, [ak], [sak])
                    t_, tk = tr.next()
                    P.tt("vector", t_[:, :n], b_[:, :n], wb_s[:, :n], ALU.mult, [bk, wbsk], [tk])
                    P.tt("gpsimd", hb[:, hc, :n], t_[:, :n], sa[:, :n], ALU.mult, [tk, sak], [hbk])
                return hb, hbk

            def emit_w2(W, hb, hbk, ti, c0, n):
                w1b, w1k, w3b, w3k, w2b, w2k = W
                for dc in range(KC):
                    y_, yk = ypr.next()
                    for hc in range(4):
                        P.mm(y_[:, :n], w2b[:, hc, dc * 128:(dc + 1) * 128], hb[:, hc, :n], hc == 0, hc == 3,
                             [w2k, hbk], [yk])
                    P.tt("vector", yacc[:, dc, c0:c0 + n], y_[:, :n], yacc[:, dc, c0:c0 + n], ALU.add,
                         [yk, ("yacc", dc, ti)], [("yacc", dc, ti)])

            def load_st(st, s0):
                stn = st_sizes[st]
                P.dma(h2[:, :, 0:stn], h2v[:, :, s0:s0 + stn], writes=["h2"])
                P.dma(wtsf[:, 0:stn], self.wts[:, s0:s0 + stn], writes=["wts_f"])
                P.cp("vector", wts[:, 0:stn], wtsf[:, 0:stn], ["wts_f"], ["wts_sb"])

            issue_dma(0)
            issue_cast(0)
            issue_dma(1)
            issue_cast(1)
            issue_dma(2)
            s0 = t0
            for st in range(4):
                tiles = stiles[st]
                stn = st_sizes[st]
                nti = len(tiles)
                if st == 0:
                    load_st(0, s0)
                P.memset("gpsimd", yacc[:], 0.0, [("yacc", dc_, ti_) for dc_ in range(KC) for ti_ in range(4)])
                prev = None

                def flush(prev):
                    W, hb, hbk, ti, c0, n, lastt, j = prev
                    emit_w2(W, hb, hbk, ti, c0, n)
                    if lastt:
                        del loaded[j]
                        issue_cast(j + 2)
                        issue_dma(j + 3)

                for e in range(NEXP):
                    j = st * NEXP + e
                    W = loaded[j]
                    for ti, (c0, n) in enumerate(tiles):
                        hb, hbk = emit_ab(W, e, c0, n)
                        if e == NEXP - 1 and ti == nti - 1 and st < 3:
                            load_st(st + 1, s0 + stn)
                        if prev is not None:
                            flush(prev)
                        prev = (W, hb, hbk, ti, c0, n, ti == nti - 1, j)
                flush(prev)
                for ti, (tc0, tn) in enumerate(tiles):
                    for cc in range(0, tn, FN):
                        n = min(FN, tn - cc)
                        c0 = tc0 + cc
                        g0 = s0 + c0
                        xf, xfk = xfr.next()
                        P.dma(xf[:, :, :n], xv[:, :, g0:g0 + n], writes=[xfk])
                        segs = []
                        if g0 < LC:
                            nb = min(LC, g0 + n) - g0
                            segs.append((0, nb, 1))
                            if nb < n:
                                segs.append((nb, n, 0))
                        else:
                            segs.append((0, n, 0))
                        for (a, b, s) in segs:
                            for k in range(KC):
                                P.stt("vector", xf[:, k, a:b], yacc[:, k, c0 + a:c0 + b], self.mvec(l, 5, k, s),
                                      xf[:, k, a:b], ALU.mult, ALU.add, [("yacc", k, ti), xfk, "mod"], [xfk])
                        if not last:
                            P.dma(xv[:, :, g0:g0 + n], xf[:, :, :n], reads=[xfk])
                        else:
                            xsq, xsqk = rings["xsq"].next()
                            P.act(xsq[:, :, :n], xf[:, :, :n], AF.Square, [xfk], [xsqk])
                            ssp, sspk = wbp.next()
                            for k in range(KC):
                                P.mm(ssp[:, :n], self.ones_all[:], xsq[:, k, :n], k == 0, k == KC - 1,
                                     [xsqk, "ones_all"], [sspk])
                            sq, sqk = rings["f32a"].next()
                            P.act(sq[:, :n], ssp[:, :n], AF.Sqrt, [sspk], [sqk], bias=self.eps_ap(), scale=1.0 / D)
                            rstd, rk = rings["f32b"].next()
                            P.recip(rstd[:, :n], sq[:, :n], [sqk], [rk])
                            for k in range(KC):
                                P.stt("vector", xf[:, k, :n], xf[:, k, :n], self.gfin_sb[:, k:k + 1], rstd[:, :n],
                                      ALU.mult, ALU.mult, [xfk, rk, "gfin"], [xfk])
                            P.dma(outv[:, :, g0 - LC:g0 - LC + n], xf[:, :, :n], reads=[xfk])
                s0 += stn


def _feat(v):
    return np.ascontiguousarray(np.asarray(v, np.float32).reshape(KC, 128).T)


def host_constants():
    cst = {}
    cst["ident"] = np.eye(128, dtype=np.float32)
    perm = np.zeros((128, 128), np.float32)
    for p in range(128):
        partner = p + 16 if (p % 32) < 16 else p - 16
        perm[partner, p] = 1.0
    cst["perm"] = perm
    t = np.arange(S)
    pos = np.stack([t // GRID, t % GRID], -1).astype(np.float32)
    inv = (10000.0 ** (-np.arange(16, dtype=np.float32) / 16)).astype(np.float32)
    C = np.ones((128, T), np.float32)
    Sn = np.zeros((128, T), np.float32)
    for p in range(128):
        pp = p % 64
        axis = pp // 32
        half = (pp % 32) // 16
        f = pp % 16
        ang = (pos[:, axis] * inv[f]).astype(np.float32)
        C[p, LC:] = np.cos(ang)
        Sn[p, LC:] = np.sin(ang) * (-1.0 if half == 0 else 1.0)
    cst["ropeC"] = C
    cst["ropeS"] = Sn
    ic = np.ones((128, 4, 2, 8), np.float32)
    for g, w in enumerate((2, 4, 8, 16)):
        for i in range(w // 2):
            ic[:, g, 0, i] = 1.0 / (i + w // 2)
        nr = w // 2 - 1
        for i in range(nr):
            ic[:, g, 1, i] = 1.0 / (nr - i + w // 2)
    cst["pool_ic"] = ic
    sel = np.zeros((32, NEXP, 128), np.float32)
    for e in range(NEXP):
        sel[e, e, :] = 1.0
    cst["sel"] = sel.reshape(32, NEXP * 128)
    return cst


def na_bias_tiles(rel_bias):
    H = rel_bias.shape[0]
    out = np.full((H, 14, 128, 256), NEG, np.float32)
    cols = np.arange(GRID)
    cstart = np.clip(cols - 8, 0, GRID - 16)

    def fill(tile, kr_abs, qr_abs_list, kh_slot):
        for qi, qr in enumerate(qr_abs_list):
            rs = min(max(qr - 4, 0), GRID - 8)
            if not (rs <= kr_abs < rs + 8):
                continue
            ro = kr_abs - qr + 7
            for qc in range(GRID):
                kcs = np.arange(cstart[qc], cstart[qc] + 16)
                out[:, tile, kh_slot * 64 + kcs, qi * 64 + qc] = rel_bias[:, ro, kcs - qc + 15]

    for j in range(6):
        for hslot in range(2):
            fill(j, 12 + 2 * j + hslot, [16, 17, 18, 19], hslot)
    for j in range(4):
        for hslot in range(2):
            fill(6 + j, 0 + 2 * j + hslot, [0, 1, 2, 3], hslot)
            fill(10 + j, 56 + 2 * j + hslot, [60, 61, 62, 63], hslot)
    return np.ascontiguousarray(out.transpose(0, 2, 1, 3))


def make_in_maps(inputs, ncores=8, layers=(0, 1), moe=True):
    f32 = lambda a: np.ascontiguousarray(np.asarray(a, np.float32))
    cst = host_constants()
    x = np.asarray(inputs["x"], np.float32)
    ctx = np.asarray(inputs["ctx"], np.float32)
    c = np.asarray(inputs["c"], np.float32)
    c_ctx = np.asarray(inputs["c_ctx"], np.float32)
    shared = {}
    shared["ada_w"] = f32(inputs["ada_w"])
    ada_b = np.asarray(inputs["ada_b"], np.float32)
    shared["adab"] = np.ascontiguousarray(ada_b.reshape(2, 48, 128).transpose(2, 0, 1))
    shared["gmix"] = np.ascontiguousarray(np.stack([_feat(inputs["norm_mix_g"][l]) for l in range(2)], 1))
    shared["gffn"] = np.ascontiguousarray(np.stack([_feat(inputs["norm_ffn_g"][l]) for l in range(2)], 1))
    shared["gfin"] = _feat(inputs["final_g"])
    shared["ident"] = cst["ident"]
    if 0 in layers:
        shared["e_win"] = f32(inputs["even_w_in"][0])
        shared["e_wout"] = f32(inputs["even_w_out"][0])
        gq = np.asarray(inputs["a_q_gain"][0], np.float32)
        gk = np.asarray(inputs["a_k_gain"][0], np.float32)
        shared["gqk"] = np.ascontiguousarray(np.stack([np.tile(gq, 2), np.tile(gk, 2)], 1))
        shared["perm"] = cst["perm"]
        shared["ropeC"] = cst["ropeC"]
        shared["ropeS"] = cst["ropeS"]
        shared["pool_w"] = f32(inputs["pool_w"][0])
        shared["pool_s"] = np.ascontiguousarray(np.asarray(inputs["pool_scale"][0], np.float32).reshape(4, 128).T)
        shared["pool_ic"] = cst["pool_ic"]
    if 1 in layers:
        shared["o_win"] = f32(inputs["odd_w_in"][0])
        shared["o_wout"] = f32(inputs["odd_w_out"][0])
        shared["nab"] = na_bias_tiles(np.asarray(inputs["na_rel_bias"][0], np.float32))
    shared["wr"] = np.ascontiguousarray(np.concatenate(
        [np.asarray(inputs["moe_w_group"], np.float32), np.asarray(inputs["moe_w_expert"], np.float32)], -1))
    brow = np.concatenate([np.asarray(inputs["moe_b_group"], np.float32),
                           np.asarray(inputs["moe_b_expert"], np.float32)], -1)
    shared["br"] = np.ascontiguousarray(np.broadcast_to(brow[None], (128, 2, 36)))
    if moe:
        shared["sel"] = cst["sel"]
        shared["moe_w1"] = f32(inputs["moe_w1"])
        shared["moe_w3"] = f32(inputs["moe_w3"])
        shared["moe_w2"] = f32(inputs["moe_w2"])
    maps = []
    for b in range(ncores):
        m = dict(shared)
        m["xin"] = np.ascontiguousarray(np.concatenate([ctx[b].T, x[b].T], 1))
        m["cc"] = np.ascontiguousarray(np.stack([_feat(c[b]), _feat(c_ctx)], -1))
        maps.append(m)
    return maps


def kernel(**inputs):
    nc = bass.Bass("TRN2", target_bir_lowering=False)
    bld = Builder(nc)
    bld.build()
    maps = make_in_maps(inputs, 8)
    maps = [{k: v for k, v in m.items() if k in bld.inputs} for m in maps]
    res = run_bass_kernel_spmd(nc, maps, core_ids=list(range(8)))
    bld.P.close()
    out = np.stack([np.ascontiguousarray(np.asarray(r["outT"]).T) for r in res.results], 0)
    return out.astype(np.float32)
```

```python
import numpy as np
import concourse.bass as bass
import concourse.mybir as mybir
from concourse.bass_utils import run_bass_kernel_spmd
from contextlib import ExitStack

F32 = mybir.dt.float32
BF16 = mybir.dt.bfloat16
AF = mybir.ActivationFunctionType
ALU = mybir.AluOpType
AX = mybir.AxisListType

COMPUTE = ("tensor", "vector", "scalar", "gpsimd")
ENGS = COMPUTE + ("sync",)
NDMASEM = 8

D = 1024
KC = 8
S = 4096
LC = 256
T = S + LC
EPS = 1e-6
NEXP = 32
DE = 512
GRID = 64
NEG = -30000.0


class Op:
    __slots__ = ("eng", "fn", "waits", "signal", "idx", "dma", "dsem", "dval")

    def __init__(self, eng, fn, dma):
        self.eng = eng
        self.fn = fn
        self.waits = []
        self.signal = False
        self.idx = None
        self.dma = dma
        self.dsem = None
        self.dval = None


class Prog:
    def __init__(self, nc):
        self.nc = nc
        self.es = ExitStack()
        self.queues = {e: [] for e in ENGS}
        self.lastw = {}
        self.readers = {}
        self.dma_ops = {e: [] for e in ENGS}
        self.n_tensors = 0
        self.fence = []
        self.fence_pending = {e: False for e in ENGS}

    def sb(self, shape, dt, name=None):
        self.n_tensors += 1
        return self.es.enter_context(self.nc.sbuf_tensor(f"sb{self.n_tensors}_{name or ''}", list(shape), dt))

    def ps(self, shape, dt=F32, name=None):
        self.n_tensors += 1
        return self.es.enter_context(self.nc.psum_tensor(f"ps{self.n_tensors}_{name or ''}", list(shape), dt))

    def scope(self):
        return Scope(self)

    def op(self, eng, fn, reads=(), writes=(), dma=False):
        o = Op(eng, fn, dma)
        deps = []
        for k in reads:
            w = self.lastw.get(k)
            if w is not None:
                deps.append(w)
        for k in writes:
            w = self.lastw.get(k)
            if w is not None:
                deps.append(w)
            deps.extend(self.readers.get(k, ()))
        seen = set()
        for d in deps:
            if id(d) in seen or d is o:
                continue
            seen.add(id(d))
            if (not d.dma) and (not dma) and d.eng == eng and eng == "tensor":
                continue
            o.waits.append(d)
            d.signal = True
        if self.fence_pending[eng]:
            for d in self.fence:
                if d is not o and d not in o.waits:
                    o.waits.append(d)
                    d.signal = True
            self.fence_pending[eng] = False
        if dma:
            lst = self.dma_ops[eng]
            if len(lst) >= NDMASEM:
                prev = lst[len(lst) - NDMASEM]
                if prev not in o.waits:
                    o.waits.append(prev)
            lst.append(o)
            o.signal = True
        for k in reads:
            self.readers.setdefault(k, []).append(o)
        for k in writes:
            self.lastw[k] = o
            self.readers[k] = []
        self.queues[eng].append(o)
        return o

    def barrier(self):
        fence = []
        for e in ENGS:
            for o in reversed(self.queues[e]):
                if not o.dma:
                    fence.append(o)
                    break
            fence.extend(self.dma_ops[e][-NDMASEM:])
        self.fence = fence
        self.fence_pending = {e: True for e in ENGS}

    def emit(self):
        nc = self.nc
        es = self.es
        csem = {e: es.enter_context(nc.semaphore(f"c_{e}")) for e in ENGS}
        dsem = {e: [es.enter_context(nc.semaphore(f"d_{e}{i}")) for i in range(NDMASEM)]
                for e in ENGS if self.dma_ops[e]}
        for e in ENGS:
            cnt = 0
            dcnt = [0] * NDMASEM
            n = 0
            for o in self.queues[e]:
                if o.dma:
                    slot = n % NDMASEM
                    n += 1
                    dcnt[slot] += 16
                    o.dsem = dsem[e][slot]
                    o.dval = dcnt[slot]
                elif o.signal:
                    cnt += 1
                    o.idx = cnt
        block = es.enter_context(nc.Block())
        queues = self.queues
        dma_ops = self.dma_ops

        def run(engname):
            def body(eng):
                seen = {}
                for o in queues[engname]:
                    need = {}
                    for d in o.waits:
                        if d.dma:
                            sem, val = d.dsem, d.dval
                        else:
                            sem, val = csem[d.eng], d.idx
                        key = id(sem)
                        cur = need.get(key)
                        if cur is None or val > cur[1]:
                            need[key] = (sem, val)
                    for key, (sem, val) in need.items():
                        if seen.get(key, -1) >= val:
                            continue
                        seen[key] = val
                        eng.wait_ge(sem, val)
                    ins = o.fn(eng)
                    if o.dma:
                        ins.then_inc(o.dsem, 16)
                    elif o.signal:
                        ins.then_inc(csem[engname], 1)
                for d in dma_ops[engname][-NDMASEM:]:
                    if seen.get(id(d.dsem), -1) < d.dval:
                        seen[id(d.dsem)] = d.dval
                        eng.wait_ge(d.dsem, d.dval)
            return body

        block.tensor(run("tensor"))
        block.vector(run("vector"))
        block.scalar(run("scalar"))
        block.gpsimd(run("gpsimd"))
        block.sync(run("sync"))

    def close(self):
        self.es.close()

    def mm(self, out, lhsT, rhs, start, stop, reads, writes):
        return self.op("tensor", lambda e: e.matmul(out, lhsT, rhs, start=start, stop=stop), reads, writes)

    def act(self, out, in_, func, reads, writes, bias=None, scale=None, accum_out=None):
        kw = {}
        if bias is not None:
            kw["bias"] = bias
        if scale is not None:
            kw["scale"] = scale
        if accum_out is not None:
            kw["accum_out"] = accum_out
        return self.op("scalar", lambda e: e.activation(out=out, in_=in_, func=func, **kw), reads, writes)

    def tt(self, eng, out, in0, in1, op, reads, writes):
        return self.op(eng, lambda e: e.tensor_tensor(out, in0, in1, op), reads, writes)

    def stt(self, eng, out, in0, scalar, in1, op0, op1, reads, writes):
        return self.op(eng, lambda e: e.scalar_tensor_tensor(out=out, in0=in0, scalar=scalar, in1=in1,
                                                             op0=op0, op1=op1), reads, writes)

    def ts(self, eng, out, in0, s1, s2, op0, op1, reads, writes):
        if op1 is None:
            return self.op(eng, lambda e: e.tensor_scalar(out, in0, s1, None, op0), reads, writes)
        return self.op(eng, lambda e: e.tensor_scalar(out, in0, s1, s2, op0, op1), reads, writes)

    def cp(self, eng, out, in_, reads, writes):
        if eng == "scalar":
            return self.op(eng, lambda e: e.copy(out, in_), reads, writes)
        return self.op(eng, lambda e: e.tensor_copy(out, in_), reads, writes)

    def recip(self, out, in_, reads, writes):
        return self.op("vector", lambda e: e.reciprocal(out, in_), reads, writes)

    def memset(self, eng, ap, val, writes):
        return self.op(eng, lambda e: e.memset(ap, val), (), writes)

    def rmax(self, out, in_, reads, writes):
        return self.op("vector", lambda e: e.reduce_max(out, in_, AX.X), reads, writes)

    def dma(self, out, in_, reads=(), writes=(), eng="sync"):
        return self.op(eng, lambda e: e.dma_start(out=out, in_=in_), reads, writes, dma=True)


class Scope:
    def __init__(self, P):
        self.P = P
        self.es = ExitStack()

    def __enter__(self):
        self.es.__enter__()
        return self

    def __exit__(self, *a):
        r = self.es.__exit__(*a)
        self.P.barrier()
        return r

    def sb(self, shape, dt, name=None):
        self.P.n_tensors += 1
        return self.es.enter_context(self.P.nc.sbuf_tensor(f"sb{self.P.n_tensors}_{name or ''}", list(shape), dt))

    def ps(self, shape, dt=F32, name=None):
        self.P.n_tensors += 1
        return self.es.enter_context(self.P.nc.psum_tensor(f"ps{self.P.n_tensors}_{name or ''}", list(shape), dt))

    def ring(self, n, shape, dt, tag, psum=False):
        tiles = [(self.ps(shape, dt) if psum else self.sb(shape, dt)) for _ in range(n)]
        return Ring(tiles, tag)


class Ring:
    def __init__(self, tiles, tag):
        self.tiles = tiles
        self.tag = tag
        self.i = 0

    def next(self):
        j = self.i % len(self.tiles)
        self.i += 1
        return self.tiles[j], (self.tag, j)


class Builder:
    def __init__(self, nc, layers=(0, 1), moe=True, dbg=()):
        self.nc = nc
        self.P = Prog(nc)
        self.layers = layers
        self.moe = moe
        self.dbg = dbg
        self.inputs = {}
        self.outputs = {}

    def din(self, name, shape, dt=F32):
        t = self.nc.dram_tensor(name, list(shape), dt, kind="ExternalInput").ap()
        self.inputs[name] = t
        return t

    def dout(self, name, shape, dt=F32):
        t = self.nc.dram_tensor(name, list(shape), dt, kind="ExternalOutput").ap()
        self.outputs[name] = t
        return t

    def dscratch(self, name, shape, dt=F32):
        return self.nc.dram_tensor(name, list(shape), dt).ap()

    def build(self):
        P = self.P
        self.xin = self.din("xin", [D, T])
        self.cc = self.din("cc", [128, KC, 2])
        self.ada_w = self.din("ada_w", [2, D, 6 * D])
        self.adab = self.din("adab", [128, 2, 48])
        self.gmix = self.din("gmix", [128, 2, KC])
        self.gffn = self.din("gffn", [128, 2, KC])
        self.gfin = self.din("gfin", [128, KC])
        self.ident_d = self.din("ident", [128, 128])
        if 0 in self.layers:
            self.e_win = self.din("e_win", [D, 1280])
            self.e_wout = self.din("e_wout", [D, D])
            self.gqk = self.din("gqk", [128, 2])
            self.perm_d = self.din("perm", [128, 128])
            self.ropeC = self.din("ropeC", [128, T])
            self.ropeS = self.din("ropeS", [128, T])
            self.pool_w = self.din("pool_w", [4, 128, 128])
            self.pool_s = self.din("pool_s", [128, 4])
            self.pool_ic = self.din("pool_ic", [128, 4, 2, 8])
        if 1 in self.layers:
            self.o_win = self.din("o_win", [D, 3 * D])
            self.o_wout = self.din("o_wout", [D, D])
            self.nab = self.din("nab", [16, 128, 14, 256])
        self.wr = self.din("wr", [2, D, 36])
        self.br = self.din("br", [128, 2, 36])
        if self.moe:
            self.sel_d = self.din("sel", [32, NEXP * 128])
            self.w1 = self.din("moe_w1", [2, NEXP, D, DE])
            self.w3 = self.din("moe_w3", [2, NEXP, D, DE])
            self.w2 = self.din("moe_w2", [2, NEXP, DE, D])
        self.outT = self.dout("outT", [D, S])
        self.xres = self.dscratch("xres", [D, T])
        self.h2s = self.dscratch("h2s", [D, T], BF16)
        self.aTs = self.dscratch("aTs", [D, T], BF16)
        self.us = self.dscratch("us", [512, T])
        self.wts = self.dscratch("wts", [32, T])

        self.ident = P.sb([128, 128], F32, "ident")
        P.dma(self.ident[:], self.ident_d, writes=["ident"])
        self.ones_all = P.sb([128, 128], BF16, "ones_all")
        P.memset("vector", self.ones_all[:], 1.0, ["ones_all"])
        self.ones_bd = P.sb([128, 128], BF16, "ones_bd")
        P.memset("vector", self.ones_bd[:], 0.0, ["ones_bd"])
        P.memset("vector", self.ones_bd[0:64, 0:64], 1.0, ["ones_bd"])
        P.memset("vector", self.ones_bd[64:128, 64:128], 1.0, ["ones_bd"])
        self.mod = P.sb([128, 2, 48, 2], F32, "mod")
        self.S1 = P.sb([128, 2, KC, 2], F32, "S1")
        self.S2 = P.sb([128, 2, KC, 2], F32, "S2")
        self.gfin_sb = P.sb([128, KC], F32, "gfin_sb")
        P.dma(self.gfin_sb[:], self.gfin, writes=["gfin"])

        self.eps_ap()
        self.phase_mod()
        if 0 not in self.layers:
            xri = self.din("xres_in", [D, T])
            with P.scope() as sc:
                r = sc.ring(2, [128, T], F32, "xri")
                for r0 in range(0, D, 128):
                    t, k = r.next()
                    P.dma(t[:], xri[r0:r0 + 128, :], writes=[k])
                    P.dma(self.xres[r0:r0 + 128, :], t[:], reads=[k], writes=["xres"])
        if 0 in self.layers:
            self.layer0_front()
            self.dump("a0", self.aTs, D, T, BF16, "aTs")
            self.phase_post(0)
            self.dump("xp0", self.xres, D, T, F32, "xres")
            self.dump("h20", self.h2s, D, T, BF16, "h2s")
            self.dump("wt0", self.wts, 32, T, F32, "wts")
            if self.moe:
                self.phase_moe(0)
                self.dump("x0", self.xres, D, T, F32, "xres")
        if 1 in self.layers:
            self.layer1_front()
            self.dump("a1", self.aTs, D, T, BF16, "aTs")
            self.phase_post(1)
            self.dump("xp1", self.xres, D, T, F32, "xres")
            self.dump("h21", self.h2s, D, T, BF16, "h2s")
            self.dump("wt1", self.wts, 32, T, F32, "wts")
            if self.moe:
                self.phase_moe(1)
        P.emit()

    def dump(self, tag, src, R, C, dt, key):
        if tag not in self.dbg:
            return
        P = self.P
        o = self.dout("dbg_" + tag, [R, C], dt)
        with P.scope() as sc:
            r = sc.ring(2, [128, C], dt, "dump_" + tag)
            for r0 in range(0, R, 128):
                n = min(128, R - r0)
                t, k = r.next()
                P.dma(t[0:n, :], src[r0:r0 + n, :], reads=[key], writes=[k])
                P.dma(o[r0:r0 + n, :], t[0:n, :], reads=[k])

    def phase_mod(self):
        P = self.P
        with P.scope() as sc:
            cc = sc.sb([128, KC, 2], F32)
            cs = sc.sb([128, KC, 2], F32)
            adab = sc.sb([128, 2, 48], F32)
            gm = sc.sb([128, 2, KC], F32)
            gf = sc.sb([128, 2, KC], F32)
            P.dma(cc[:], self.cc, writes=["cc"])
            P.dma(adab[:], self.adab, writes=["adab"])
            P.dma(gm[:], self.gmix, writes=["gm"])
            P.dma(gf[:], self.gffn, writes=["gf"])
            P.act(cs[:], cc[:], AF.Silu, ["cc"], ["cs"])
            wring = sc.ring(2, [128, KC, 768], F32, "adaw")
            pm = sc.ps([128, 48, 2], F32)
            for l in self.layers:
                awl = self.ada_w[l].rearrange("(k p) n -> p k n", p=128)
                for blk in range(8):
                    wt, wk = wring.next()
                    P.dma(wt[:], awl[:, :, blk * 768:(blk + 1) * 768], writes=[wk])
                    for j in range(6):
                        oc = blk * 6 + j
                        for k in range(KC):
                            P.mm(pm[:, oc, :], wt[:, k, j * 128:(j + 1) * 128], cs[:, k, :],
                                 k == 0, k == KC - 1, [wk, "cs"], ["pm"])
                P.tt("vector", self.mod[:, l], pm[:], adab[:, l].unsqueeze(2).to_broadcast([128, 48, 2]),
                     ALU.add, ["pm", "adab"], ["mod"])
                P.stt("vector", self.S1[:, l], self.mod[:, l, 8:16, :], 1.0,
                      gm[:, l].unsqueeze(2).to_broadcast([128, KC, 2]), ALU.add, ALU.mult,
                      ["mod", "gm"], ["S1"])
                P.stt("vector", self.S2[:, l], self.mod[:, l, 32:40, :], 1.0,
                      gf[:, l].unsqueeze(2).to_broadcast([128, KC, 2]), ALU.add, ALU.mult,
                      ["mod", "gf"], ["S2"])
            if "mod" in self.dbg:
                o = self.dout("dbg_mod", [128, 2 * 48 * 2])
                P.dma(o, self.mod[:].rearrange("p a b c -> p (a b c)"), reads=["mod"])

    def mvec(self, l, kind, k, s):
        return self.mod[:, l, kind * 8 + k, s:s + 1]

    def load_cast(self, sc_ring, dst, src, shape, dkeys, eng="gpsimd"):
        P = self.P
        st, sk = sc_ring.next()
        view = st
        idx = tuple(slice(0, s) for s in shape)
        P.dma(view[idx], src, writes=[sk])
        P.cp(eng, dst, view[idx], [sk], dkeys)

    def rmsnorm_mod(self, sc, xt, xk, N, Svec, shk_l_kind, s, l, out_bf, out_bf_key, out_f32=None, out_f32_key=None,
                    rings=None):
        P = self.P
        xsq, xsqk = rings["xsq"].next()
        P.act(xsq[:, :, :N], xt[:, :, :N], AF.Square, [xk], [xsqk])
        ssp, sspk = rings["ssp"].next()
        for k in range(KC):
            P.mm(ssp[:, :N], self.ones_all[:], xsq[:, k, :N], k == 0, k == KC - 1, [xsqk, "ones_all"], [sspk])
        sq, sqk = rings["f32a"].next()
        P.act(sq[:, :N], ssp[:, :N], AF.Sqrt, [sspk], [sqk], bias=self.eps_ap(), scale=1.0 / D)
        rstd, rk = rings["f32b"].next()
        P.recip(rstd[:, :N], sq[:, :N], [sqk], [rk])
        for k in range(KC):
            tmp, tk = rings["f32c"].next()
            P.stt("vector", tmp[:, :N], xt[:, k, :N], Svec[:, l, k, s:s + 1], rstd[:, :N], ALU.mult, ALU.mult,
                  [xk, rk, "S1", "S2"], [tk])
            sh = self.mvec(l, shk_l_kind, k, s)
            if out_f32 is not None:
                P.act(out_f32[:, k, :N], tmp[:, :N], AF.Identity, [tk, "mod"], [out_f32_key], bias=sh, scale=1.0)
                P.cp("gpsimd", out_bf[:, k, :N], out_f32[:, k, :N], [out_f32_key], [out_bf_key])
            else:
                P.act(out_bf[:, k, :N], tmp[:, :N], AF.Identity, [tk, "mod"], [out_bf_key], bias=sh, scale=1.0)

    def eps_ap(self):
        if not hasattr(self, "_eps"):
            self._eps = self.P.sb([128, 1], F32, "eps")
            self.P.memset("vector", self._eps[:], EPS, ["eps"])
        return self._eps[:]

    def layer0_front(self):
        P = self.P
        l = 0
        TN = 256
        ntile = T // TN
        xin_v = self.xin.rearrange("(k p) t -> p k t", p=128)
        with P.scope() as sq_scope:
            QT = sq_scope.sb([128, 4, T], BF16, "QT")
            K2T = sq_scope.sb([128, 2, T], BF16, "K2T")
            Vaug = sq_scope.sb([128, 2, T // 128, 192], BF16, "Vaug")
            P.memset("gpsimd", Vaug[:, :, :, 64:128], 1.0, ["Vaug"])
            with P.scope() as sc:
                stg = sc.ring(2, [128, KC, 256], F32, "stg")
                Win = sc.sb([128, KC, 1280], BF16, "Win")
                Wk2 = sc.sb([128, KC, 256], BF16, "Wk2")
                ewv = self.e_win.rearrange("(k p) n -> p k n", p=128)
                for j in range(5):
                    self.load_cast(stg, Win[:, :, j * 256:(j + 1) * 256], ewv[:, :, j * 256:(j + 1) * 256],
                                   [128, KC, 256], ["Win"])
                for g in range(2):
                    for h in range(2):
                        P.cp("gpsimd", Wk2[:, :, g * 128 + h * 64: g * 128 + h * 64 + 64],
                             Win[:, :, 512 + g * 64: 512 + g * 64 + 64], ["Win"], ["Wk2"])
                gqk = sc.sb([128, 2], F32, "gqk")
                P.dma(gqk[:], self.gqk, writes=["gqk"])
                perm = sc.sb([128, 128], F32, "perm")
                P.dma(perm[:], self.perm_d, writes=["perm"])
                rings = {
                    "xsq": sc.ring(1, [128, KC, TN], BF16, "xsq"),
                    "ssp": sc.ring(1, [128, TN], F32, "ssp", psum=True),
                    "f32a": sc.ring(2, [128, TN], F32, "f32a"),
                    "f32b": sc.ring(2, [128, TN], F32, "f32b"),
                    "f32c": sc.ring(3, [128, TN], F32, "f32c"),
                }
                xring = sc.ring(2, [128, KC, TN], F32, "xt")
                hring = sc.ring(2, [128, KC, TN], BF16, "hT")
                cring = sc.ring(2, [128, TN], F32, "ropeC")
                sring = sc.ring(2, [128, TN], F32, "ropeS")
                pp = sc.ring(3, [128, TN], F32, "pp", psum=True)
                pq = sc.ring(1, [128, TN], F32, "pq", psum=True)
                pr = sc.ring(1, [128, TN], F32, "pr", psum=True)
                pv = sc.ring(1, [128, 128], F32, "pv", psum=True)
                qsqr = sc.ring(2, [128, TN], BF16, "qsq")
                qnr = sc.ring(2, [128, TN], F32, "qn")
                t1r = sc.ring(2, [128, TN], F32, "t1")
                t2r = sc.ring(2, [128, TN], F32, "t2")
                ur = sc.ring(2, [128, TN], F32, "ust")
                def load0(ti):
                    c0 = ti * TN
                    xt, xk = xring.next()
                    P.dma(xt[:], xin_v[:, :, c0:c0 + TN], writes=[xk])
                    ct, ck = cring.next()
                    st_, sk_ = sring.next()
                    P.dma(ct[:], self.ropeC[:, c0:c0 + TN], writes=[ck])
                    P.dma(st_[:], self.ropeS[:, c0:c0 + TN], writes=[sk_])
                    return xt, xk, ct, ck, st_, sk_
                nxt = load0(0)
                for ti in range(ntile):
                    c0 = ti * TN
                    s = 1 if ti == 0 else 0
                    xt, xk, ct, ck, st_, sk_ = nxt
                    if ti + 1 < ntile:
                        nxt = load0(ti + 1)
                    hT, hk = hring.next()
                    self.rmsnorm_mod(sc, xt, xk, TN, self.S1, 0, s, l, hT, hk, rings=rings)
                    for oc in range(6):
                        ps_, pk_ = pp.next()
                        for k in range(KC):
                            if oc < 4:
                                w = Win[:, k, oc * 128:(oc + 1) * 128]
                                wkey = "Win"
                            else:
                                w = Wk2[:, k, (oc - 4) * 128:(oc - 3) * 128]
                                wkey = "Wk2"
                            P.mm(ps_[:], w, hT[:, k, :], k == 0, k == KC - 1, [wkey, hk], [pk_])
                        gain = gqk[:, 0:1] if oc < 4 else gqk[:, 1:2]
                        qsq, qsqk = qsqr.next()
                        P.act(qsq[:], ps_[:], AF.Square, [pk_], [qsqk])
                        ssq, ssqk = pq.next()
                        P.mm(ssq[:], self.ones_bd[:], qsq[:], True, True, [qsqk, "ones_bd"], [ssqk])
                        sq, sqk = rings["f32a"].next()
                        P.act(sq[:], ssq[:], AF.Sqrt, [ssqk], [sqk], bias=self.eps_ap(), scale=1.0 / 64)
                        rs, rsk = rings["f32b"].next()
                        P.recip(rs[:], sq[:], [sqk], [rsk])
                        qn, qnk = qnr.next()
                        P.stt("vector", qn[:], ps_[:], gain, rs[:], ALU.mult, ALU.mult, [pk_, rsk, "gqk"], [qnk])
                        prp, prk = pr.next()
                        P.mm(prp[:], perm[:], qn[:], True, True, [qnk, "perm"], [prk])
                        t1, t1k = t1r.next()
                        P.tt("gpsimd", t1[:], qn[:], ct[:], ALU.mult, [qnk, ck], [t1k])
                        t2, t2k = t2r.next()
                        P.tt("vector", t2[:], prp[:], st_[:], ALU.mult, [prk, sk_], [t2k])
                        if oc < 4:
                            dst, dk = QT[:, oc, c0:c0 + TN], "QT"
                        else:
                            dst, dk = K2T[:, oc - 4, c0:c0 + TN], "K2T"
                        P.tt("gpsimd", dst, t1[:], t2[:], ALU.add, [t1k, t2k], [dk])
                    for j in range(TN // 128):
                        vp, vk = pv.next()
                        for k in range(KC):
                            P.mm(vp[:], hT[:, k, j * 128:(j + 1) * 128], Win[:, k, 640:768], k == 0, k == KC - 1,
                                 [hk, "Win"], [vk])
                        tix = c0 // 128 + j
                        for g in range(2):
                            P.cp("scalar", Vaug[:, g, tix, 0:64], vp[:, g * 64:(g + 1) * 64], [vk], ["Vaug"])
                            P.cp("vector", Vaug[:, g, tix, 128:192], vp[:, g * 64:(g + 1) * 64], [vk], ["Vaug"])
                    for g in range(4):
                        ps_, pk_ = pp.next()
                        for k in range(KC):
                            P.mm(ps_[:], Win[:, k, 768 + g * 128: 768 + (g + 1) * 128], hT[:, k, :], k == 0,
                                 k == KC - 1, ["Win", hk], [pk_])
                        ut, uk = ur.next()
                        P.cp("scalar", ut[:], ps_[:], [pk_], [uk])
                        P.dma(self.us[g * 128:(g + 1) * 128, c0:c0 + TN], ut[:], reads=[uk], writes=["us"])
                if "qkv" in self.dbg:
                    o = self.dout("dbg_QT", [128, 4 * T], BF16)
                    P.dma(o, QT[:].rearrange("p a t -> p (a t)"), reads=["QT"])
                    o = self.dout("dbg_K2T", [128, 2 * T], BF16)
                    P.dma(o, K2T[:].rearrange("p a t -> p (a t)"), reads=["K2T"])
                    o = self.dout("dbg_V", [128, 2 * (T // 128) * 192], BF16)
                    P.dma(o, Vaug[:].rearrange("p a t d -> p (a t d)"), reads=["Vaug"])
            with P.scope() as sc:
                PADL = 16
                WT = PADL + LC + PADL + S + PADL
                OFFC = PADL
                OFFX = PADL + LC + PADL
                pooled = sc.sb([128, 4, T], BF16, "pooled")
                pw = sc.sb([128, 4, 128], BF16, "pw")
                pws = sc.sb([128, 4, 128], F32, "pws")
                P.dma(pws[:], self.pool_w.rearrange("g c d -> c g d"), writes=["pws"])
                P.cp("gpsimd", pw[:], pws[:], ["pws"], ["pw"])
                psc = sc.sb([128, 4], F32, "psc")
                P.dma(psc[:], self.pool_s, writes=["psc"])
                pic = sc.sb([128, 4, 2, 8], F32, "pic")
                P.dma(pic[:], self.pool_ic, writes=["pic"])
                with P.scope() as sc2:
                    U = sc2.sb([128, WT], F32, "U")
                    A = sc2.sb([128, WT], F32, "A")
                    B = sc2.sb([128, WT], F32, "B")
                    for g in range(4):
                        w = (2, 4, 8, 16)[g]
                        P.memset("vector", U[:], 0.0, ["U"])
                        P.dma(U[:, OFFC:OFFC + LC], self.us[g * 128:(g + 1) * 128, 0:LC], reads=["us"], writes=["U"])
                        P.dma(U[:, OFFX:OFFX + S], self.us[g * 128:(g + 1) * 128, LC:T], reads=["us"], writes=["U"])
                        lo, hi = 8, WT - 8
                        P.memset("gpsimd", A[:], 0.0, ["A"])
                        P.memset("gpsimd", B[:], 0.0, ["B"])
                        P.tt("vector", A[:, lo:hi], U[:, lo - 1:hi - 1], U[:, lo:hi], ALU.add, ["U"], ["A"])
                        cur, curk, oth, othk = A, "A", B, "B"
                        sh = 1
                        ww = 2
                        while ww < w:
                            d = ww // 2
                            P.tt("vector", oth[:, lo:hi], cur[:, lo - d:hi - d], cur[:, lo + d:hi + d], ALU.add,
                                 [curk], [othk])
                            cur, curk, oth, othk = oth, othk, cur, curk
                            ww *= 2
                        pl, plk = oth, othk
                        P.stt("vector", pl[:, lo:hi], cur[:, lo:hi], 1.0 / w, U[:, lo:hi], ALU.mult, ALU.subtract,
                              [curk, "U"], [plk])
                        for (off, L) in ((OFFC, LC), (OFFX, S)):
                            nl = w // 2
                            tmpb = sc2.sb([128, 8], F32)
                            P.tt("vector", tmpb[:, 0:nl], cur[:, off:off + nl], pic[:, g, 0, 0:nl], ALU.mult,
                                 [curk, "pic"], ["tmpb"])
                            P.tt("vector", pl[:, off:off + nl], tmpb[:, 0:nl], U[:, off:off + nl], ALU.subtract,
                                 ["tmpb", "U"], [plk])
                            nr = w // 2 - 1
                            if nr > 0:
                                r0 = off + L - nr
                                tmpc = sc2.sb([128, 8], F32)
                                P.tt("vector", tmpc[:, 0:nr], cur[:, r0:r0 + nr], pic[:, g, 1, 0:nr], ALU.mult,
                                     [curk, "pic"], ["tmpc"])
                                P.tt("vector", pl[:, r0:r0 + nr], tmpc[:, 0:nr], U[:, r0:r0 + nr], ALU.subtract,
                                     ["tmpc", "U"], [plk])
                        P.cp("gpsimd", pooled[:, g, 0:LC], pl[:, OFFC:OFFC + LC], [plk], ["pooled"])
                        P.cp("gpsimd", pooled[:, g, LC:T], pl[:, OFFX:OFFX + S], [plk], ["pooled"])
                self.attention0(QT, K2T, Vaug)
                with P.scope() as sc2:
                    pmm = sc2.ring(2, [128, 512], F32, "pmm", psum=True)
                    ob = sc2.ring(2, [128, 512], BF16, "pob")
                    for g in range(4):
                        for c0 in range(0, T, 512):
                            n = min(512, T - c0)
                            ps_, pk_ = pmm.next()
                            P.mm(ps_[:, :n], pw[:, g, :], pooled[:, g, c0:c0 + n], True, True, ["pw", "pooled"], [pk_])
                            o_, ok_ = ob.next()
                            P.act(o_[:, :n], ps_[:, :n], AF.Identity, [pk_, "psc"], [ok_], scale=psc[:, g:g + 1])
                            P.dma(self.aTs[(4 + g) * 128:(5 + g) * 128, c0:c0 + n], o_[:, :n], reads=[ok_],
                                  writes=["aTs"])

    def attention0(self, QT, K2T, Vaug):
        P = self.P
        A_SCALE = 64 ** -0.5
        with P.scope() as sc:
            sps = sc.ring(4, [128, 512], F32, "sps", psum=True)
            oA = sc.ring(2, [128, 512], F32, "oA", psum=True)
            oB = sc.ring(2, [128, 512], F32, "oB", psum=True)
            pt = sc.ring(4, [128, 512], BF16, "pt")
            Rr = sc.ring(2, [128, 512], F32, "R")
            ao = sc.ring(2, [128, 512], BF16, "ao")
            qtiles = [(0, LC, 2)] + [(LC + i * 512, 512, T // 128) for i in range(S // 512)]
            for c in range(4):
                g = c // 2
                for (q0, N, nkc) in qtiles:
                    a_, ak = oA.next()
                    b_, bk = oB.next()
                    iters = [(kc, h) for kc in range(nkc) for h in range(2)]
                    LA = 3
                    pend = {}

                    def emitS(i, g=g, c=c, q0=q0, N=N, iters=iters, pend=pend):
                        kc, h = iters[i]
                        hp = slice(h * 64, h * 64 + 64)
                        sp_, spk = sps.next()
                        P.mm(sp_[:, :N], K2T[hp, g, kc * 128:(kc + 1) * 128], QT[hp, c, q0:q0 + N], True, True,
                             ["K2T", "QT"], [spk])
                        pend[i] = (sp_, spk)

                    for i in range(min(LA, len(iters))):
                        emitS(i)
                    for i in range(len(iters)):
                        if i + LA < len(iters):
                            emitS(i + LA)
                        kc, h = iters[i]
                        sp_, spk = pend.pop(i)
                        p_, pk_ = pt.next()
                        P.act(p_[:, :N], sp_[:, :N], AF.Exp, [spk], [pk_], scale=A_SCALE)
                        if h == 0:
                            P.mm(a_[:, :N], Vaug[:, g, kc, 0:128], p_[:, :N], kc == 0, kc == nkc - 1,
                                 ["Vaug", pk_], [ak])
                        else:
                            P.mm(b_[:, :N], Vaug[:, g, kc, 64:192], p_[:, :N], kc == 0, kc == nkc - 1,
                                 ["Vaug", pk_], [bk])
                    R, Rk = Rr.next()
                    P.recip(R[0:64, :N], a_[64:128, :N], [ak], [Rk])
                    P.recip(R[64:128, :N], b_[0:64, :N], [bk], [Rk])
                    o_, ok_ = ao.next()
                    P.tt("vector", o_[0:64, :N], a_[0:64, :N], R[0:64, :N], ALU.mult, [ak, Rk], [ok_])
                    P.tt("vector", o_[64:128, :N], b_[64:128, :N], R[64:128, :N], ALU.mult, [bk, Rk], [ok_])
                    P.dma(self.aTs[c * 128:(c + 1) * 128, q0:q0 + N], o_[:, :N], reads=[ok_], writes=["aTs"])

    def layer1_front(self):
        P = self.P
        l = 1
        TN = 256
        ntile = T // TN
        xv = self.xres.rearrange("(k p) t -> p k t", p=128)
        C_SCALE = 64 ** -0.5
        with P.scope() as sc0:
            hT = sc0.sb([128, KC, T], BF16, "hT1")
            with P.scope() as sc:
                rings = {
                    "xsq": sc.ring(1, [128, KC, TN], BF16, "xsq"),
                    "ssp": sc.ring(1, [128, TN], F32, "ssp", psum=True),
                    "f32a": sc.ring(2, [128, TN], F32, "f32a"),
                    "f32b": sc.ring(2, [128, TN], F32, "f32b"),
                    "f32c": sc.ring(3, [128, TN], F32, "f32c"),
                }
                xring = sc.ring(2, [128, KC, TN], F32, "xt")
                hring = sc.ring(2, [128, KC, TN], BF16, "hTt")
                def load1(ti):
                    c0 = ti * TN
                    xt, xk = xring.next()
                    P.dma(xt[:], xv[:, :, c0:c0 + TN], reads=["xres"], writes=[xk])
                    return xt, xk
                nxt = load1(0)
                for ti in range(ntile):
                    c0 = ti * TN
                    s = 1 if ti == 0 else 0
                    xt, xk = nxt
                    if ti + 1 < ntile:
                        nxt = load1(ti + 1)
                    ht, hk = hring.next()
                    self.rmsnorm_mod(sc, xt, xk, TN, self.S1, 0, s, l, ht, hk, rings=rings)
                    P.cp("gpsimd", hT[:, :, c0:c0 + TN], ht[:], [hk], ["hT1"])
            if "h1" in self.dbg:
                o = self.dout("dbg_h1", [128, KC * T], BF16)
                P.dma(o, hT[:].rearrange("p a t -> p (a t)"), reads=["hT1"])
            with P.scope() as sc:
                owv = self.o_win.rearrange("(k p) n -> p k n", p=128)
                stg = sc.ring(2, [128, KC, 128], F32, "stg1")
                wq = sc.ring(2, [128, KC, 128], BF16, "wq")
                wk = sc.ring(2, [128, KC, 128], BF16, "wk")
                wv = sc.ring(2, [128, KC, 128], BF16, "wv")
                qT = sc.sb([128, S], BF16, "qT1")
                kT = sc.sb([128, T], BF16, "kT1")
                Vaug = sc.sb([128, T // 128, 192], BF16, "Vaug1")
                P.memset("gpsimd", Vaug[:, :, 64:128], 1.0, ["Vaug1"])
                bias = sc.sb([128, 2, 14, 256], F32, "nab")
                pp = sc.ring(2, [128, 512], F32, "pp1", psum=True)
                pv = sc.ring(1, [128, 128], F32, "pv1", psum=True)
                sps = sc.ring(3, [128, 256], F32, "sps1", psum=True)
                oA = sc.ring(1, [128, 256], F32, "oA1", psum=True)
                oB = sc.ring(1, [128, 256], F32, "oB1", psum=True)
                ssb = sc.ring(3, [128, 256], F32, "ssb1")
                pt = sc.ring(4, [128, 256], BF16, "pt1")
                Rr = sc.ring(2, [128, 256], F32, "R1")
                ao = sc.ring(2, [128, 256], BF16, "ao1")
                for c in range(8):
                    wq_, wqk = wq.next()
                    wk_, wkk = wk.next()
                    wv_, wvk = wv.next()
                    self.load_cast(stg, wq_[:], owv[:, :, c * 128:(c + 1) * 128], [128, KC, 128], [wqk])
                    self.load_cast(stg, wk_[:], owv[:, :, D + c * 128:D + (c + 1) * 128], [128, KC, 128], [wkk])
                    self.load_cast(stg, wv_[:], owv[:, :, 2 * D + c * 128:2 * D + (c + 1) * 128], [128, KC, 128], [wvk])
                    for hh in range(2):
                        P.dma(bias[:, hh], self.nab[2 * c + hh], writes=["nab"])
                    for c0 in range(0, S, 512):
                        ps_, pk_ = pp.next()
                        for k in range(KC):
                            P.mm(ps_[:], wq_[:, k, :], hT[:, k, LC + c0:LC + c0 + 512], k == 0, k == KC - 1,
                                 [wqk, "hT1"], [pk_])
                        P.act(qT[:, c0:c0 + 512], ps_[:], AF.Identity, [pk_], ["qT1"], scale=C_SCALE)
                    for c0 in range(0, T, 512):
                        n = min(512, T - c0)
                        ps_, pk_ = pp.next()
                        for k in range(KC):
                            P.mm(ps_[:, :n], wk_[:, k, :], hT[:, k, c0:c0 + n], k == 0, k == KC - 1,
                                 [wkk, "hT1"], [pk_])
                        P.cp("vector", kT[:, c0:c0 + n], ps_[:, :n], [pk_], ["kT1"])
                    for tix in range(T // 128):
                        vp, vk = pv.next()
                        for k in range(KC):
                            P.mm(vp[:], hT[:, k, tix * 128:(tix + 1) * 128], wv_[:, k, :], k == 0, k == KC - 1,
                                 ["hT1", wvk], [vk])
                        P.cp("scalar", Vaug[:, tix, 0:64], vp[:, 0:64], [vk], ["Vaug1"])
                        P.cp("vector", Vaug[:, tix, 128:192], vp[:, 64:128], [vk], ["Vaug1"])
                    for qb in range(16):
                        if qb == 0:
                            kr0, nloc, tb = 0, 4, 6
                        elif qb == 15:
                            kr0, nloc, tb = 56, 4, 10
                        else:
                            kr0, nloc, tb = 4 * qb - 4, 6, 0
                        chunks = [(LC + 64 * kr0 + 128 * j, tb + j) for j in range(nloc)] + [(0, None), (128, None)]
                        a_, ak = oA.next()
                        b_, bk = oB.next()
                        q0 = qb * 256
                        nch = len(chunks)
                        iters = [(ci, h) for ci in range(nch) for h in range(2)]
                        LA = 2
                        pend = {}

                        def emitS1(i, iters=iters, chunks=chunks, q0=q0, pend=pend):
                            ci, h = iters[i]
                            k0 = chunks[ci][0]
                            hp = slice(h * 64, h * 64 + 64)
                            sp_, spk = sps.next()
                            P.mm(sp_[:], kT[hp, k0:k0 + 128], qT[hp, q0:q0 + 256], True, True,
                                 ["kT1", "qT1"], [spk])
                            pend[i] = (sp_, spk)

                        for i in range(LA):
                            emitS1(i)
                        for i in range(len(iters)):
                            if i + LA < len(iters):
                                emitS1(i + LA)
                            ci, h = iters[i]
                            k0, bt = chunks[ci]
                            sp_, spk = pend.pop(i)
                            p_, pk_ = pt.next()
                            if bt is not None:
                                sb_, sbk = ssb.next()
                                P.tt("vector", sb_[:], sp_[:], bias[:, h, bt, :], ALU.add, [spk, "nab"], [sbk])
                                P.act(p_[:], sb_[:], AF.Exp, [sbk], [pk_])
                            else:
                                P.act(p_[:], sp_[:], AF.Exp, [spk], [pk_])
                            tix = k0 // 128
                            if h == 0:
                                P.mm(a_[:], Vaug[:, tix, 0:128], p_[:], ci == 0, ci == nch - 1, ["Vaug1", pk_], [ak])
                            else:
                                P.mm(b_[:], Vaug[:, tix, 64:192], p_[:], ci == 0, ci == nch - 1, ["Vaug1", pk_], [bk])
                        R, Rk = Rr.next()
                        P.recip(R[0:64, :], a_[64:128, :], [ak], [Rk])
                        P.recip(R[64:128, :], b_[0:64, :], [bk], [Rk])
                        o_, ok_ = ao.next()
                        P.tt("vector", o_[0:64, :], a_[0:64, :], R[0:64, :], ALU.mult, [ak, Rk], [ok_])
                        P.tt("vector", o_[64:128, :], b_[64:128, :], R[64:128, :], ALU.mult, [bk, Rk], [ok_])
                        P.dma(self.aTs[c * 128:(c + 1) * 128, LC + q0:LC + q0 + 256], o_[:], reads=[ok_],
                              writes=["aTs"])

    def phase_post(self, l):
        P = self.P
        TN = 256
        t0 = 0 if l == 0 else LC
        src = self.xin if l == 0 else self.xres
        srck = "xin" if l == 0 else "xres"
        xv = src.rearrange("(k p) t -> p k t", p=128)
        xo = self.xres.rearrange("(k p) t -> p k t", p=128)
        h2v = self.h2s.rearrange("(k p) t -> p k t", p=128)
        aTv = self.aTs.rearrange("(k p) t -> p k t", p=128)
        wout_d = (self.e_wout if l == 0 else self.o_wout).rearrange("(k p) n -> p k n", p=128)
        with P.scope() as sc:
            stg = sc.ring(2, [128, KC, 256], F32, "stgp")
            Wout = sc.sb([128, KC, D], BF16, "Wout")
            for j in range(4):
                self.load_cast(stg, Wout[:, :, j * 256:(j + 1) * 256], wout_d[:, :, j * 256:(j + 1) * 256],
                               [128, KC, 256], ["Wout"])
            wr = sc.sb([128, KC, 36], F32, "wr")
            P.dma(wr[:], self.wr[l].rearrange("(k p) n -> p k n", p=128), writes=["wr"])
            br = sc.sb([128, 36], F32, "br")
            P.dma(br[:], self.br[:, l, :], writes=["br"])
            rings = {
                "xsq": sc.ring(1, [128, KC, TN], BF16, "xsq"),
                "ssp": sc.ring(1, [128, TN], F32, "ssp", psum=True),
                "f32a": sc.ring(2, [128, TN], F32, "f32a"),
                "f32b": sc.ring(2, [128, TN], F32, "f32b"),
                "f32c": sc.ring(3, [128, TN], F32, "f32c"),
            }
            xring = sc.ring(2, [128, KC, TN], F32, "xtp")
            aring = sc.ring(2, [128, KC, TN], BF16, "aTt")
            xnring = sc.ring(2, [128, KC, TN], F32, "xn")
            hfring = sc.ring(1, [128, KC, TN], F32, "h2f")
            hbring = sc.ring(2, [128, KC, TN], BF16, "h2b")
            yp = sc.ring(3, [128, TN], F32, "yp", psum=True)
            lp = sc.ring(1, [128, 36], F32, "lp", psum=True)
            wtp = sc.ring(1, [32, 128], F32, "wtp", psum=True)
            wtr = sc.ring(2, [32, TN], F32, "wtr")
            sm = sc.ring(2, [128, 160], F32, "sm")
            def loadp(c0):
                xt, xk = xring.next()
                P.dma(xt[:], xv[:, :, c0:c0 + TN], reads=[srck], writes=[xk])
                at, atk = aring.next()
                P.dma(at[:], aTv[:, :, c0:c0 + TN], reads=["aTs"], writes=[atk])
                return xt, xk, at, atk
            nxt = loadp(t0)
            for c0 in range(t0, T, TN):
                s = 1 if c0 < LC else 0
                xt, xk, at, atk = nxt
                if c0 + TN < T:
                    nxt = loadp(c0 + TN)
                xn, xnk = xnring.next()
                for oc in range(KC):
                    ps_, pk_ = yp.next()
                    for k in range(KC):
                        P.mm(ps_[:], Wout[:, k, oc * 128:(oc + 1) * 128], at[:, k, :], k == 0, k == KC - 1,
                             ["Wout", atk], [pk_])
                    P.stt("vector", xn[:, oc, :], ps_[:], self.mvec(l, 2, oc, s), xt[:, oc, :], ALU.mult, ALU.add,
                          [pk_, xk, "mod"], [xnk])
                P.dma(xo[:, :, c0:c0 + TN], xn[:], reads=[xnk], writes=["xres"])
                hf, hfk = hfring.next()
                hb, hbk = hbring.next()
                self.rmsnorm_mod(sc, xn, xnk, TN, self.S2, 3, s, l, hb, hbk, out_f32=hf, out_f32_key=hfk, rings=rings)
                P.dma(h2v[:, :, c0:c0 + TN], hb[:], reads=[hbk], writes=["h2s"])
                wt_, wtk = wtr.next()
                for j in range(TN // 128):
                    lg, lgk = lp.next()
                    for k in range(KC):
                        P.mm(lg[:], hf[:, k, j * 128:(j + 1) * 128], wr[:, k, :], k == 0, k == KC - 1, [hfk, "wr"], [lgk])
                    m, mk = sm.next()
                    L = m[:, 0:36]
                    gmax = m[:, 36:37]
                    ngmax = m[:, 37:38]
                    gsum = m[:, 38:39]
                    gw = m[:, 39:40]
                    gmk = m[:, 40:44]
                    pen = m[:, 44:48]
                    Lm = m[:, 48:80]
                    mk1 = m[:, 80:112]
                    Lm2 = m[:, 112:144]
                    m1 = m[:, 144:145]
                    m2 = m[:, 145:146]
                    dd = m[:, 146:147]
                    ee = m[:, 147:148]
                    w1 = m[:, 148:149]
                    w2 = m[:, 149:150]
                    gex = m[:, 150:154]
                    rk = [mk]
                    P.tt("vector", L, lg[:], br[:], ALU.add, [lgk, "br"], rk)
                    P.rmax(gmax, m[:, 0:4], rk, rk)
                    P.ts("vector", ngmax, gmax, -1.0, None, ALU.mult, None, rk, rk)
                    P.ts("vector", gmk, m[:, 0:4], gmax, None, ALU.is_ge, None, rk, rk)
                    P.act(gex, m[:, 0:4], AF.Exp, rk, rk, bias=ngmax, scale=1.0, accum_out=gsum)
                    P.recip(gw, gsum, rk, rk)
                    P.ts("vector", pen, gmk, 1e30, -1e30, ALU.mult, ALU.add, rk, rk)
                    P.tt("vector", Lm.rearrange("p (g e) -> p g e", g=4), m[:, 4:36].rearrange("p (g e) -> p g e", g=4),
                         pen.unsqueeze(2).to_broadcast([128, 4, 8]), ALU.add, rk, rk)
                    P.rmax(m1, Lm, rk, rk)
                    P.ts("vector", mk1, Lm, m1, None, ALU.is_ge, None, rk, rk)
                    P.stt("vector", Lm2, mk1, -1e30, Lm, ALU.mult, ALU.add, rk, rk)
                    P.rmax(m2, Lm2, rk, rk)
                    mk2 = m[:, 48:80]
                    P.ts("vector", mk2, Lm2, m2, None, ALU.is_ge, None, rk, rk)
                    P.tt("vector", dd, m2, m1, ALU.subtract, rk, rk)
                    P.act(ee, dd, AF.Exp, rk, rk)
                    P.ts("vector", ee, ee, 1.0, None, ALU.add, None, rk, rk)
                    P.recip(ee, ee, rk, rk)
                    P.tt("vector", w1, gw, ee, ALU.mult, rk, rk)
                    P.tt("vector", w2, gw, w1, ALU.subtract, rk, rk)
                    wm = m[:, 112:144]
                    P.ts("vector", mk1, mk1, w1, None, ALU.mult, None, rk, rk)
                    P.stt("vector", wm, mk2, w2, mk1, ALU.mult, ALU.add, rk, rk)
                    tp, tpk = wtp.next()
                    P.mm(tp[:], wm, self.ident[:], True, True, rk + ["ident"], [tpk])
                    P.cp("scalar", wt_[:, j * 128:(j + 1) * 128], tp[:], [tpk], [wtk])
                P.dma(self.wts[:, c0:c0 + TN], wt_[:], reads=[wtk], writes=["wts"])
            if f"post{l}" in self.dbg:
                pass

    def phase_moe(self, l):
        P = self.P
        last = (l == 1)
        t0 = 0 if l == 0 else LC
        if l == 0:
            stiles = [[(0, 384), (384, 384), (768, 384)], [(0, 384), (384, 384), (768, 384)],
                      [(0, 512), (512, 512)], [(0, 512), (512, 512)]]
        else:
            stiles = [[(0, 512), (512, 512)]] * 4
        st_sizes = [sum(n for _, n in st) for st in stiles]
        STN = max(st_sizes)
        NT = 512
        xv = self.xres.rearrange("(k p) t -> p k t", p=128)
        h2v = self.h2s.rearrange("(k p) t -> p k t", p=128)
        outv = self.outT.rearrange("(k p) t -> p k t", p=128)
        w1v = self.w1[l].rearrange("e (k p) n -> e p k n", p=128)
        w3v = self.w3[l].rearrange("e (k p) n -> e p k n", p=128)
        w2v = self.w2[l].rearrange("e (k p) n -> e p k n", p=128)
        with P.scope() as sc:
            stg = sc.ring(6, [128, 4, DE], F32, "stgm")
            sel = sc.sb([32, NEXP * 128], BF16, "sel")
            for q_ in range(4):
                self.load_cast(stg, sel[:, q_ * 1024:(q_ + 1) * 1024].rearrange("p (a b) -> p a b", a=2),
                               self.sel_d[:, q_ * 1024:(q_ + 1) * 1024].rearrange("p (a b) -> p a b", a=2),
                               [32, 2, 512], ["sel"], eng="vector")
            yacc = sc.sb([128, KC, STN], F32, "yacc")
            h2 = sc.sb([128, KC, STN], BF16, "h2")
            wts = sc.sb([32, STN], BF16, "wts_sb")
            wtsf = sc.sb([32, STN], F32, "wts_f")
            W1b = sc.ring(2, [128, KC, DE], BF16, "W1b")
            W3b = sc.ring(2, [128, KC, DE], BF16, "W3b")
            W2b = sc.ring(2, [128, 4, D], BF16, "W2b")
            wbp = sc.ring(1, [128, NT], F32, "wbp", psum=True)
            ap_ = sc.ring(2, [128, NT], F32, "aps", psum=True)
            bp_ = sc.ring(2, [128, NT], F32, "bps", psum=True)
            ypr = sc.ring(3, [128, NT], F32, "ypm", psum=True)
            wbs = sc.ring(2, [128, NT], F32, "wbs")
            sar = sc.ring(2, [128, NT], F32, "sa")
            tr = sc.ring(2, [128, NT], F32, "tmoe")
            hbr = sc.ring(2, [128, 4, NT], BF16, "hb")
            FN = 256
            xfr = sc.ring(2, [128, KC, FN], F32, "xf")
            if last:
                rings = {
                    "xsq": sc.ring(1, [128, KC, FN], BF16, "xsq"),
                    "f32a": sc.ring(1, [128, FN], F32, "f32a"),
                    "f32b": sc.ring(1, [128, FN], F32, "f32b"),
                }
            seq = [(st_, e_) for st_ in range(4) for e_ in range(NEXP)]
            loaded = {}
            staged = {}

            def issue_dma(j):
                if j >= len(seq):
                    return
                e = seq[j][1]
                srcs = [w1v[e][:, 0:4, :], w1v[e][:, 4:8, :], w3v[e][:, 0:4, :], w3v[e][:, 4:8, :],
                        w2v[e][:, :, 0:DE], w2v[e][:, :, DE:2 * DE]]
                pieces = []
                for src in srcs:
                    st_, sk = stg.next()
                    P.dma(st_[:], src, writes=[sk])
                    pieces.append((st_, sk))
                staged[j] = pieces

            def issue_cast(j):
                if j >= len(seq):
                    return
                pieces = staged.pop(j)
                w1b, w1k = W1b.next()
                w3b, w3k = W3b.next()
                w2b, w2k = W2b.next()
                dsts = [w1b[:, 0:4, :], w1b[:, 4:8, :], w3b[:, 0:4, :], w3b[:, 4:8, :],
                        w2b[:, :, 0:DE], w2b[:, :, DE:2 * DE]]
                keys = [w1k, w1k, w3k, w3k, w2k, w2k]
                for (st_, sk), dst, kk in zip(pieces, dsts, keys):
                    P.cp("scalar", dst, st_[:], [sk], [kk])
                loaded[j] = (w1b, w1k, w3b, w3k, w2b, w2k)

            def emit_ab(W, e, c0, n):
                w1b, w1k, w3b, w3k, w2b, w2k = W
                wb, wbk = wbp.next()
                P.mm(wb[:, :n], sel[:, e * 128:(e + 1) * 128], wts[:, c0:c0 + n], True, True,
                     ["sel", "wts_sb"], [wbk])
                wb_s, wbsk = wbs.next()
                P.cp("scalar", wb_s[:, :n], wb[:, :n], [wbk], [wbsk])
                hb, hbk = hbr.next()
                for hc in range(4):
                    a_, ak = ap_.next()
                    b_, bk = bp_.next()
                    for k in range(KC):
                        P.mm(a_[:, :n], w1b[:, k, hc * 128:(hc + 1) * 128], h2[:, k, c0:c0 + n], k == 0,
                             k == KC - 1, [w1k, "h2"], [ak])
                    for k in range(KC):
                        P.mm(b_[:, :n], w3b[:, k, hc * 128:(hc + 1) * 128], h2[:, k, c0:c0 + n], k == 0,
                             k == KC - 1, [w3k, "h2"], [bk])
                    sa, sak = sar.next()
                    P.act(sa[:, :n], a_[:, :n], AF.Silu, [ak], [sak])
                    t_, tk = tr.next()
                    P.tt("vector", t_[:, :n], b_[:, :n], wb_s[:, :n], ALU.mult, [bk, wbsk], [tk])
                    P.tt("gpsimd", hb[:, hc, :n], t_[:, :n], sa[:, :n], ALU.mult, [tk, sak], [hbk])
                return hb, hbk

            def emit_w2(W, hb, hbk, ti, c0, n):
                w1b, w1k, w3b, w3k, w2b, w2k = W
                for dc in range(KC):
                    y_, yk = ypr.next()
                    for hc in range(4):
                        P.mm(y_[:, :n], w2b[:, hc, dc * 128:(dc + 1) * 128], hb[:, hc, :n], hc == 0, hc == 3,
                             [w2k, hbk], [yk])
                    P.tt("vector", yacc[:, dc, c0:c0 + n], y_[:, :n], yacc[:, dc, c0:c0 + n], ALU.add,
                         [yk, ("yacc", dc, ti)], [("yacc", dc, ti)])

            def load_st(st, s0):
                stn = st_sizes[st]
                P.dma(h2[:, :, 0:stn], h2v[:, :, s0:s0 + stn], writes=["h2"])
                P.dma(wtsf[:, 0:stn], self.wts[:, s0:s0 + stn], writes=["wts_f"])
                P.cp("vector", wts[:, 0:stn], wtsf[:, 0:stn], ["wts_f"], ["wts_sb"])

            issue_dma(0)
            issue_cast(0)
            issue_dma(1)
            issue_cast(1)
            issue_dma(2)
            s0 = t0
            for st in range(4):
                tiles = stiles[st]
                stn = st_sizes[st]
                nti = len(tiles)
                if st == 0:
                    load_st(0, s0)
                P.memset("gpsimd", yacc[:], 0.0, [("yacc", dc_, ti_) for dc_ in range(KC) for ti_ in range(4)])
                prev = None

                def flush(prev):
                    W, hb, hbk, ti, c0, n, lastt, j = prev
                    emit_w2(W, hb, hbk, ti, c0, n)
                    if lastt:
                        del loaded[j]
                        issue_cast(j + 2)
                        issue_dma(j + 3)

                for e in range(NEXP):
                    j = st * NEXP + e
                    W = loaded[j]
                    for ti, (c0, n) in enumerate(tiles):
                        hb, hbk = emit_ab(W, e, c0, n)
                        if e == NEXP - 1 and ti == nti - 1 and st < 3:
                            load_st(st + 1, s0 + stn)
                        if prev is not None:
                            flush(prev)
                        prev = (W, hb, hbk, ti, c0, n, ti == nti - 1, j)
                flush(prev)
                for ti, (tc0, tn) in enumerate(tiles):
                    for cc in range(0, tn, FN):
                        n = min(FN, tn - cc)
                        c0 = tc0 + cc
                        g0 = s0 + c0
                        xf, xfk = xfr.next()
                        P.dma(xf[:, :, :n], xv[:, :, g0:g0 + n], writes=[xfk])
                        segs = []
                        if g0 < LC:
                            nb = min(LC, g0 + n) - g0
                            segs.append((0, nb, 1))
                            if nb < n:
                                segs.append((nb, n, 0))
                        else:
                            segs.append((0, n, 0))
                        for (a, b, s) in segs:
                            for k in range(KC):
                                P.stt("vector", xf[:, k, a:b], yacc[:, k, c0 + a:c0 + b], self.mvec(l, 5, k, s),
                                      xf[:, k, a:b], ALU.mult, ALU.add, [("yacc", k, ti), xfk, "mod"], [xfk])
                        if not last:
                            P.dma(xv[:, :, g0:g0 + n], xf[:, :, :n], reads=[xfk])
                        else:
                            xsq, xsqk = rings["xsq"].next()
                            P.act(xsq[:, :, :n], xf[:, :, :n], AF.Square, [xfk], [xsqk])
                            ssp, sspk = wbp.next()
                            for k in range(KC):
                                P.mm(ssp[:, :n], self.ones_all[:], xsq[:, k, :n], k == 0, k == KC - 1,
                                     [xsqk, "ones_all"], [sspk])
                            sq, sqk = rings["f32a"].next()
                            P.act(sq[:, :n], ssp[:, :n], AF.Sqrt, [sspk], [sqk], bias=self.eps_ap(), scale=1.0 / D)
                            rstd, rk = rings["f32b"].next()
                            P.recip(rstd[:, :n], sq[:, :n], [sqk], [rk])
                            for k in range(KC):
                                P.stt("vector", xf[:, k, :n], xf[:, k, :n], self.gfin_sb[:, k:k + 1], rstd[:, :n],
                                      ALU.mult, ALU.mult, [xfk, rk, "gfin"], [xfk])
                            P.dma(outv[:, :, g0 - LC:g0 - LC + n], xf[:, :, :n], reads=[xfk])
                s0 += stn


def _feat(v):
    return np.ascontiguousarray(np.asarray(v, np.float32).reshape(KC, 128).T)


def host_constants():
    cst = {}
    cst["ident"] = np.eye(128, dtype=np.float32)
    perm = np.zeros((128, 128), np.float32)
    for p in range(128):
        partner = p + 16 if (p % 32) < 16 else p - 16
        perm[partner, p] = 1.0
    cst["perm"] = perm
    t = np.arange(S)
    pos = np.stack([t // GRID, t % GRID], -1).astype(np.float32)
    inv = (10000.0 ** (-np.arange(16, dtype=np.float32) / 16)).astype(np.float32)
    C = np.ones((128, T), np.float32)
    Sn = np.zeros((128, T), np.float32)
    for p in range(128):
        pp = p % 64
        axis = pp // 32
        half = (pp % 32) // 16
        f = pp % 16
        ang = (pos[:, axis] * inv[f]).astype(np.float32)
        C[p, LC:] = np.cos(ang)
        Sn[p, LC:] = np.sin(ang) * (-1.0 if half == 0 else 1.0)
    cst["ropeC"] = C
    cst["ropeS"] = Sn
    ic = np.ones((128, 4, 2, 8), np.float32)
    for g, w in enumerate((2, 4, 8, 16)):
        for i in range(w // 2):
            ic[:, g, 0, i] = 1.0 / (i + w // 2)
        nr = w // 2 - 1
        for i in range(nr):
            ic[:, g, 1, i] = 1.0 / (nr - i + w // 2)
    cst["pool_ic"] = ic
    sel = np.zeros((32, NEXP, 128), np.float32)
    for e in range(NEXP):
        sel[e, e, :] = 1.0
    cst["sel"] = sel.reshape(32, NEXP * 128)
    return cst


def na_bias_tiles(rel_bias):
    H = rel_bias.shape[0]
    out = np.full((H, 14, 128, 256), NEG, np.float32)
    cols = np.arange(GRID)
    cstart = np.clip(cols - 8, 0, GRID - 16)

    def fill(tile, kr_abs, qr_abs_list, kh_slot):
        for qi, qr in enumerate(qr_abs_list):
            rs = min(max(qr - 4, 0), GRID - 8)
            if not (rs <= kr_abs < rs + 8):
                continue
            ro = kr_abs - qr + 7
            for qc in range(GRID):
                kcs = np.arange(cstart[qc], cstart[qc] + 16)
                out[:, tile, kh_slot * 64 + kcs, qi * 64 + qc] = rel_bias[:, ro, kcs - qc + 15]

    for j in range(6):
        for hslot in range(2):
            fill(j, 12 + 2 * j + hslot, [16, 17, 18, 19], hslot)
    for j in range(4):
        for hslot in range(2):
            fill(6 + j, 0 + 2 * j + hslot, [0, 1, 2, 3], hslot)
            fill(10 + j, 56 + 2 * j + hslot, [60, 61, 62, 63], hslot)
    return np.ascontiguousarray(out.transpose(0, 2, 1, 3))


def make_in_maps(inputs, ncores=8, layers=(0, 1), moe=True):
    f32 = lambda a: np.ascontiguousarray(np.asarray(a, np.float32))
    cst = host_constants()
    x = np.asarray(inputs["x"], np.float32)
    ctx = np.asarray(inputs["ctx"], np.float32)
    c = np.asarray(inputs["c"], np.float32)
    c_ctx = np.asarray(inputs["c_ctx"], np.float32)
    shared = {}
    shared["ada_w"] = f32(inputs["ada_w"])
    ada_b = np.asarray(inputs["ada_b"], np.float32)
    shared["adab"] = np.ascontiguousarray(ada_b.reshape(2, 48, 128).transpose(2, 0, 1))
    shared["gmix"] = np.ascontiguousarray(np.stack([_feat(inputs["norm_mix_g"][l]) for l in range(2)], 1))
    shared["gffn"] = np.ascontiguousarray(np.stack([_feat(inputs["norm_ffn_g"][l]) for l in range(2)], 1))
    shared["gfin"] = _feat(inputs["final_g"])
    shared["ident"] = cst["ident"]
    if 0 in layers:
        shared["e_win"] = f32(inputs["even_w_in"][0])
        shared["e_wout"] = f32(inputs["even_w_out"][0])
        gq = np.asarray(inputs["a_q_gain"][0], np.float32)
        gk = np.asarray(inputs["a_k_gain"][0], np.float32)
        shared["gqk"] = np.ascontiguousarray(np.stack([np.tile(gq, 2), np.tile(gk, 2)], 1))
        shared["perm"] = cst["perm"]
        shared["ropeC"] = cst["ropeC"]
        shared["ropeS"] = cst["ropeS"]
        shared["pool_w"] = f32(inputs["pool_w"][0])
        shared["pool_s"] = np.ascontiguousarray(np.asarray(inputs["pool_scale"][0], np.float32).reshape(4, 128).T)
        shared["pool_ic"] = cst["pool_ic"]
    if 1 in layers:
        shared["o_win"] = f32(inputs["odd_w_in"][0])
        shared["o_wout"] = f32(inputs["odd_w_out"][0])
        shared["nab"] = na_bias_tiles(np.asarray(inputs["na_rel_bias"][0], np.float32))
    shared["wr"] = np.ascontiguousarray(np.concatenate(
        [np.asarray(inputs["moe_w_group"], np.float32), np.asarray(inputs["moe_w_expert"], np.float32)], -1))
    brow = np.concatenate([np.asarray(inputs["moe_b_group"], np.float32),
                           np.asarray(inputs["moe_b_expert"], np.float32)], -1)
    shared["br"] = np.ascontiguousarray(np.broadcast_to(brow[None], (128, 2, 36)))
    if moe:
        shared["sel"] = cst["sel"]
        shared["moe_w1"] = f32(inputs["moe_w1"])
        shared["moe_w3"] = f32(inputs["moe_w3"])
        shared["moe_w2"] = f32(inputs["moe_w2"])
    maps = []
    for b in range(ncores):
        m = dict(shared)
        m["xin"] = np.ascontiguousarray(np.concatenate([ctx[b].T, x[b].T], 1))
        m["cc"] = np.ascontiguousarray(np.stack([_feat(c[b]), _feat(c_ctx)], -1))
        maps.append(m)
    return maps


def kernel(**inputs):
    nc = bass.Bass("TRN2", target_bir_lowering=False)
    bld = Builder(nc)
    bld.build()
    maps = make_in_maps(inputs, 8)
    maps = [{k: v for k, v in m.items() if k in bld.inputs} for m in maps]
    res = run_bass_kernel_spmd(nc, maps, core_ids=list(range(8)))
    bld.P.close()
    out = np.stack([np.ascontiguousarray(np.asarray(r["outT"]).T) for r in res.results], 0)
    return out.astype(np.float32)
```

```python
import numpy as np
import concourse.bass as bass
import concourse.mybir as mybir
from concourse.bass_utils import run_bass_kernel_spmd
from contextlib import ExitStack

F32 = mybir.dt.float32
BF16 = mybir.dt.bfloat16
AF = mybir.ActivationFunctionType
ALU = mybir.AluOpType
AX = mybir.AxisListType

COMPUTE = ("tensor", "vector", "scalar", "gpsimd")
ENGS = COMPUTE + ("sync",)
NDMASEM = 8

D = 1024
KC = 8
S = 4096
LC = 256
T = S + LC
EPS = 1e-6
NEXP = 32
DE = 512
GRID = 64
NEG = -30000.0


class Op:
    __slots__ = ("eng", "fn", "waits", "signal", "idx", "dma", "dsem", "dval")

    def __init__(self, eng, fn, dma):
        self.eng = eng
        self.fn = fn
        self.waits = []
        self.signal = False
        self.idx = None
        self.dma = dma
        self.dsem = None
        self.dval = None


class Prog:
    def __init__(self, nc):
        self.nc = nc
        self.es = ExitStack()
        self.queues = {e: [] for e in ENGS}
        self.lastw = {}
        self.readers = {}
        self.dma_ops = {e: [] for e in ENGS}
        self.n_tensors = 0
        self.fence = []
        self.fence_pending = {e: False for e in ENGS}

    def sb(self, shape, dt, name=None):
        self.n_tensors += 1
        return self.es.enter_context(self.nc.sbuf_tensor(f"sb{self.n_tensors}_{name or ''}", list(shape), dt))

    def ps(self, shape, dt=F32, name=None):
        self.n_tensors += 1
        return self.es.enter_context(self.nc.psum_tensor(f"ps{self.n_tensors}_{name or ''}", list(shape), dt))

    def scope(self):
        return Scope(self)

    def op(self, eng, fn, reads=(), writes=(), dma=False):
        o = Op(eng, fn, dma)
        deps = []
        for k in reads:
            w = self.lastw.get(k)
            if w is not None:
                deps.append(w)
        for k in writes:
            w = self.lastw.get(k)
            if w is not None:
                deps.append(w)
            deps.extend(self.readers.get(k, ()))
        seen = set()
        for d in deps:
            if id(d) in seen or d is o:
                continue
            seen.add(id(d))
            if (not d.dma) and (not dma) and d.eng == eng and eng == "tensor":
                continue
            o.waits.append(d)
            d.signal = True
        if self.fence_pending[eng]:
            for d in self.fence:
                if d is not o and d not in o.waits:
                    o.waits.append(d)
                    d.signal = True
            self.fence_pending[eng] = False
        if dma:
            lst = self.dma_ops[eng]
            if len(lst) >= NDMASEM:
                prev = lst[len(lst) - NDMASEM]
                if prev not in o.waits:
                    o.waits.append(prev)
            lst.append(o)
            o.signal = True
        for k in reads:
            self.readers.setdefault(k, []).append(o)
        for k in writes:
            self.lastw[k] = o
            self.readers[k] = []
        self.queues[eng].append(o)
        return o

    def barrier(self):
        fence = []
        for e in ENGS:
            for o in reversed(self.queues[e]):
                if not o.dma:
                    fence.append(o)
                    break
            fence.extend(self.dma_ops[e][-NDMASEM:])
        self.fence = fence
        self.fence_pending = {e: True for e in ENGS}

    def emit(self):
        nc = self.nc
        es = self.es
        csem = {e: es.enter_context(nc.semaphore(f"c_{e}")) for e in ENGS}
        dsem = {e: [es.enter_context(nc.semaphore(f"d_{e}{i}")) for i in range(NDMASEM)]
                for e in ENGS if self.dma_ops[e]}
        for e in ENGS:
            cnt = 0
            dcnt = [0] * NDMASEM
            n = 0
            for o in self.queues[e]:
                if o.dma:
                    slot = n % NDMASEM
                    n += 1
                    dcnt[slot] += 16
                    o.dsem = dsem[e][slot]
                    o.dval = dcnt[slot]
                elif o.signal:
                    cnt += 1
                    o.idx = cnt
        block = es.enter_context(nc.Block())
        queues = self.queues
        dma_ops = self.dma_ops

        def run(engname):
            def body(eng):
                seen = {}
                for o in queues[engname]:
                    need = {}
                    for d in o.waits:
                        if d.dma:
                            sem, val = d.dsem, d.dval
                        else:
                            sem, val = csem[d.eng], d.idx
                        key = id(sem)
                        cur = need.get(key)
                        if cur is None or val > cur[1]:
                            need[key] = (sem, val)
                    for key, (sem, val) in need.items():
                        if seen.get(key, -1) >= val:
                            continue
                        seen[key] = val
                        eng.wait_ge(sem, val)
                    ins = o.fn(eng)
                    if o.dma:
                        ins.then_inc(o.dsem, 16)
                    elif o.signal:
                        ins.then_inc(csem[engname], 1)
                for d in dma_ops[engname][-NDMASEM:]:
                    if seen.get(id(d.dsem), -1) < d.dval:
                        seen[id(d.dsem)] = d.dval
                        eng.wait_ge(d.dsem, d.dval)
            return body

        block.tensor(run("tensor"))
        block.vector(run("vector"))
        block.scalar(run("scalar"))
        block.gpsimd(run("gpsimd"))
        block.sync(run("sync"))

    def close(self):
        self.es.close()

    def mm(self, out, lhsT, rhs, start, stop, reads, writes):
        return self.op("tensor", lambda e: e.matmul(out, lhsT, rhs, start=start, stop=stop), reads, writes)

    def act(self, out, in_, func, reads, writes, bias=None, scale=None, accum_out=None):
        kw = {}
        if bias is not None:
            kw["bias"] = bias
        if scale is not None:
            kw["scale"] = scale
        if accum_out is not None:
            kw["accum_out"] = accum_out
        return self.op("scalar", lambda e: e.activation(out=out, in_=in_, func=func, **kw), reads, writes)

    def tt(self, eng, out, in0, in1, op, reads, writes):
        return self.op(eng, lambda e: e.tensor_tensor(out, in0, in1, op), reads, writes)

    def stt(self, eng, out, in0, scalar, in1, op0, op1, reads, writes):
        return self.op(eng, lambda e: e.scalar_tensor_tensor(out=out, in0=in0, scalar=scalar, in1=in1,
                                                             op0=op0, op1=op1), reads, writes)

    def ts(self, eng, out, in0, s1, s2, op0, op1, reads, writes):
        if op1 is None:
            return self.op(eng, lambda e: e.tensor_scalar(out, in0, s1, None, op0), reads, writes)
        return self.op(eng, lambda e: e.tensor_scalar(out, in0, s1, s2, op0, op1), reads, writes)

    def cp(self, eng, out, in_, reads, writes):
        if eng == "scalar":
            return self.op(eng, lambda e: e.copy(out, in_), reads, writes)
        return self.op(eng, lambda e: e.tensor_copy(out, in_), reads, writes)

    def recip(self, out, in_, reads, writes):
        return self.op("vector", lambda e: e.reciprocal(out, in_), reads, writes)

    def memset(self, eng, ap, val, writes):
        return self.op(eng, lambda e: e.memset(ap, val), (), writes)

    def rmax(self, out, in_, reads, writes):
        return self.op("vector", lambda e: e.reduce_max(out, in_, AX.X), reads, writes)

    def dma(self, out, in_, reads=(), writes=(), eng="sync"):
        return self.op(eng, lambda e: e.dma_start(out=out, in_=in_), reads, writes, dma=True)


class Scope:
    def __init__(self, P):
        self.P = P
        self.es = ExitStack()

    def __enter__(self):
        self.es.__enter__()
        return self

    def __exit__(self, *a):
        r = self.es.__exit__(*a)
        self.P.barrier()
        return r

    def sb(self, shape, dt, name=None):
        self.P.n_tensors += 1
        return self.es.enter_context(self.P.nc.sbuf_tensor(f"sb{self.P.n_tensors}_{name or ''}", list(shape), dt))

    def ps(self, shape, dt=F32, name=None):
        self.P.n_tensors += 1
        return self.es.enter_context(self.P.nc.psum_tensor(f"ps{self.P.n_tensors}_{name or ''}", list(shape), dt))

    def ring(self, n, shape, dt, tag, psum=False):
        tiles = [(self.ps(shape, dt) if psum else self.sb(shape, dt)) for _ in range(n)]
        return Ring(tiles, tag)


class Ring:
    def __init__(self, tiles, tag):
        self.tiles = tiles
        self.tag = tag
        self.i = 0

    def next(self):
        j = self.i % len(self.tiles)
        self.i += 1
        return self.tiles[j], (self.tag, j)


class Builder:
    def __init__(self, nc, layers=(0, 1), moe=True, dbg=()):
        self.nc = nc
        self.P = Prog(nc)
        self.layers = layers
        self.moe = moe
        self.dbg = dbg
        self.inputs = {}
        self.outputs = {}

    def din(self, name, shape, dt=F32):
        t = self.nc.dram_tensor(name, list(shape), dt, kind="ExternalInput").ap()
        self.inputs[name] = t
        return t

    def dout(self, name, shape, dt=F32):
        t = self.nc.dram_tensor(name, list(shape), dt, kind="ExternalOutput").ap()
        self.outputs[name] = t
        return t

    def dscratch(self, name, shape, dt=F32):
        return self.nc.dram_tensor(name, list(shape), dt).ap()

    def build(self):
        P = self.P
        self.xin = self.din("xin", [D, T])
        self.cc = self.din("cc", [128, KC, 2])
        self.ada_w = self.din("ada_w", [2, D, 6 * D])
        self.adab = self.din("adab", [128, 2, 48])
        self.gmix = self.din("gmix", [128, 2, KC])
        self.gffn = self.din("gffn", [128, 2, KC])
        self.gfin = self.din("gfin", [128, KC])
        self.ident_d = self.din("ident", [128, 128])
        if 0 in self.layers:
            self.e_win = self.din("e_win", [D, 1280])
            self.e_wout = self.din("e_wout", [D, D])
            self.gqk = self.din("gqk", [128, 2])
            self.perm_d = self.din("perm", [128, 128])
            self.ropeC = self.din("ropeC", [128, T])
            self.ropeS = self.din("ropeS", [128, T])
            self.pool_w = self.din("pool_w", [4, 128, 128])
            self.pool_s = self.din("pool_s", [128, 4])
            self.pool_ic = self.din("pool_ic", [128, 4, 2, 8])
        if 1 in self.layers:
            self.o_win = self.din("o_win", [D, 3 * D])
            self.o_wout = self.din("o_wout", [D, D])
            self.nab = self.din("nab", [16, 128, 14, 256])
        self.wr = self.din("wr", [2, D, 36])
        self.br = self.din("br", [128, 2, 36])
        if self.moe:
            self.sel_d = self.din("sel", [32, NEXP * 128])
            self.w1 = self.din("moe_w1", [2, NEXP, D, DE])
            self.w3 = self.din("moe_w3", [2, NEXP, D, DE])
            self.w2 = self.din("moe_w2", [2, NEXP, DE, D])
        self.outT = self.dout("outT", [D, S])
        self.xres = self.dscratch("xres", [D, T])
        self.h2s = self.dscratch("h2s", [D, T], BF16)
        self.aTs = self.dscratch("aTs", [D, T], BF16)
        self.us = self.dscratch("us", [512, T])
        self.wts = self.dscratch("wts", [32, T])

        self.ident = P.sb([128, 128], F32, "ident")
        P.dma(self.ident[:], self.ident_d, writes=["ident"])
        self.ones_all = P.sb([128, 128], BF16, "ones_all")
        P.memset("vector", self.ones_all[:], 1.0, ["ones_all"])
        self.ones_bd = P.sb([128, 128], BF16, "ones_bd")
        P.memset("vector", self.ones_bd[:], 0.0, ["ones_bd"])
        P.memset("vector", self.ones_bd[0:64, 0:64], 1.0, ["ones_bd"])
        P.memset("vector", self.ones_bd[64:128, 64:128], 1.0, ["ones_bd"])
        self.mod = P.sb([128, 2, 48, 2], F32, "mod")
        self.S1 = P.sb([128, 2, KC, 2], F32, "S1")
        self.S2 = P.sb([128, 2, KC, 2], F32, "S2")
        self.gfin_sb = P.sb([128, KC], F32, "gfin_sb")
        P.dma(self.gfin_sb[:], self.gfin, writes=["gfin"])

        self.eps_ap()
        self.phase_mod()
        if 0 not in self.layers:
            xri = self.din("xres_in", [D, T])
            with P.scope() as sc:
                r = sc.ring(2, [128, T], F32, "xri")
                for r0 in range(0, D, 128):
                    t, k = r.next()
                    P.dma(t[:], xri[r0:r0 + 128, :], writes=[k])
                    P.dma(self.xres[r0:r0 + 128, :], t[:], reads=[k], writes=["xres"])
        if 0 in self.layers:
            self.layer0_front()
            self.dump("a0", self.aTs, D, T, BF16, "aTs")
            self.phase_post(0)
            self.dump("xp0", self.xres, D, T, F32, "xres")
            self.dump("h20", self.h2s, D, T, BF16, "h2s")
            self.dump("wt0", self.wts, 32, T, F32, "wts")
            if self.moe:
                self.phase_moe(0)
                self.dump("x0", self.xres, D, T, F32, "xres")
        if 1 in self.layers:
            self.layer1_front()
            self.dump("a1", self.aTs, D, T, BF16, "aTs")
            self.phase_post(1)
            self.dump("xp1", self.xres, D, T, F32, "xres")
            self.dump("h21", self.h2s, D, T, BF16, "h2s")
            self.dump("wt1", self.wts, 32, T, F32, "wts")
            if self.moe:
                self.phase_moe(1)
        P.emit()

    def dump(self, tag, src, R, C, dt, key):
        if tag not in self.dbg:
            return
        P = self.P
        o = self.dout("dbg_" + tag, [R, C], dt)
        with P.scope() as sc:
            r = sc.ring(2, [128, C], dt, "dump_" + tag)
            for r0 in range(0, R, 128):
                n = min(128, R - r0)
                t, k = r.next()
                P.dma(t[0:n, :], src[r0:r0 + n, :], reads=[key], writes=[k])
                P.dma(o[r0:r0 + n, :], t[0:n, :], reads=[k])

    def phase_mod(self):
        P = self.P
        with P.scope() as sc:
            cc = sc.sb([128, KC, 2], F32)
            cs = sc.sb([128, KC, 2], F32)
            adab = sc.sb([128, 2, 48], F32)
            gm = sc.sb([128, 2, KC], F32)
            gf = sc.sb([128, 2, KC], F32)
            P.dma(cc[:], self.cc, writes=["cc"])
            P.dma(adab[:], self.adab, writes=["adab"])
            P.dma(gm[:], self.gmix, writes=["gm"])
            P.dma(gf[:], self.gffn, writes=["gf"])
            P.act(cs[:], cc[:], AF.Silu, ["cc"], ["cs"])
            wring = sc.ring(2, [128, KC, 768], F32, "adaw")
            pm = sc.ps([128, 48, 2], F32)
            for l in self.layers:
                awl = self.ada_w[l].rearrange("(k p) n -> p k n", p=128)
                for blk in range(8):
                    wt, wk = wring.next()
                    P.dma(wt[:], awl[:, :, blk * 768:(blk + 1) * 768], writes=[wk])
                    for j in range(6):
                        oc = blk * 6 + j
                        for k in range(KC):
                            P.mm(pm[:, oc, :], wt[:, k, j * 128:(j + 1) * 128], cs[:, k, :],
                                 k == 0, k == KC - 1, [wk, "cs"], ["pm"])
                P.tt("vector", self.mod[:, l], pm[:], adab[:, l].unsqueeze(2).to_broadcast([128, 48, 2]),
                     ALU.add, ["pm", "adab"], ["mod"])
                P.stt("vector", self.S1[:, l], self.mod[:, l, 8:16, :], 1.0,
                      gm[:, l].unsqueeze(2).to_broadcast([128, KC, 2]), ALU.add, ALU.mult,
                      ["mod", "gm"], ["S1"])
                P.stt("vector", self.S2[:, l], self.mod[:, l, 32:40, :], 1.0,
                      gf[:, l].unsqueeze(2).to_broadcast([128, KC, 2]), ALU.add, ALU.mult,
                      ["mod", "gf"], ["S2"])
            if "mod" in self.dbg:
                o = self.dout("dbg_mod", [128, 2 * 48 * 2])
                P.dma(o, self.mod[:].rearrange("p a b c -> p (a b c)"), reads=["mod"])

    def mvec(self, l, kind, k, s):
        return self.mod[:, l, kind * 8 + k, s:s + 1]

    def load_cast(self, sc_ring, dst, src, shape, dkeys, eng="gpsimd"):
        P = self.P
        st, sk = sc_ring.next()
        view = st
        idx = tuple(slice(0, s) for s in shape)
        P.dma(view[idx], src, writes=[sk])
        P.cp(eng, dst, view[idx], [sk], dkeys)

    def rmsnorm_mod(self, sc, xt, xk, N, Svec, shk_l_kind, s, l, out_bf, out_bf_key, out_f32=None, out_f32_key=None,
                    rings=None):
        P = self.P
        xsq, xsqk = rings["xsq"].next()
        P.act(xsq[:, :, :N], xt[:, :, :N], AF.Square, [xk], [xsqk])
        ssp, sspk = rings["ssp"].next()
        for k in range(KC):
            P.mm(ssp[:, :N], self.ones_all[:], xsq[:, k, :N], k == 0, k == KC - 1, [xsqk, "ones_all"], [sspk])
        sq, sqk = rings["f32a"].next()
        P.act(sq[:, :N], ssp[:, :N], AF.Sqrt, [sspk], [sqk], bias=self.eps_ap(), scale=1.0 / D)
        rstd, rk = rings["f32b"].next()
        P.recip(rstd[:, :N], sq[:, :N], [sqk], [rk])
        for k in range(KC):
            tmp, tk = rings["f32c"].next()
            P.stt("vector", tmp[:, :N], xt[:, k, :N], Svec[:, l, k, s:s + 1], rstd[:, :N], ALU.mult, ALU.mult,
                  [xk, rk, "S1", "S2"], [tk])
            sh = self.mvec(l, shk_l_kind, k, s)
            if out_f32 is not None:
                P.act(out_f32[:, k, :N], tmp[:, :N], AF.Identity, [tk, "mod"], [out_f32_key], bias=sh, scale=1.0)
                P.cp("gpsimd", out_bf[:, k, :N], out_f32[:, k, :N], [out_f32_key], [out_bf_key])
            else:
                P.act(out_bf[:, k, :N], tmp[:, :N], AF.Identity, [tk, "mod"], [out_bf_key], bias=sh, scale=1.0)

    def eps_ap(self):
        if not hasattr(self, "_eps"):
            self._eps = self.P.sb([128, 1], F32, "eps")
            self.P.memset("vector", self._eps[:], EPS, ["eps"])
        return self._eps[:]

    def layer0_front(self):
        P = self.P
        l = 0
        TN = 256
        ntile = T // TN
        xin_v = self.xin.rearrange("(k p) t -> p k t", p=128)
        with P.scope() as sq_scope:
            QT = sq_scope.sb([128, 4, T], BF16, "QT")
            K2T = sq_scope.sb([128, 2, T], BF16, "K2T")
            Vaug = sq_scope.sb([128, 2, T // 128, 192], BF16, "Vaug")
            P.memset("gpsimd", Vaug[:, :, :, 64:128], 1.0, ["Vaug"])
            with P.scope() as sc:
                stg = sc.ring(2, [128, KC, 256], F32, "stg")
                Win = sc.sb([128, KC, 1280], BF16, "Win")
                Wk2 = sc.sb([128, KC, 256], BF16, "Wk2")
                ewv = self.e_win.rearrange("(k p) n -> p k n", p=128)
                for j in range(5):
                    self.load_cast(stg, Win[:, :, j * 256:(j + 1) * 256], ewv[:, :, j * 256:(j + 1) * 256],
                                   [128, KC, 256], ["Win"])
                for g in range(2):
                    for h in range(2):
                        P.cp("gpsimd", Wk2[:, :, g * 128 + h * 64: g * 128 + h * 64 + 64],
                             Win[:, :, 512 + g * 64: 512 + g * 64 + 64], ["Win"], ["Wk2"])
                gqk = sc.sb([128, 2], F32, "gqk")
                P.dma(gqk[:], self.gqk, writes=["gqk"])
                perm = sc.sb([128, 128], F32, "perm")
                P.dma(perm[:], self.perm_d, writes=["perm"])
                rings = {
                    "xsq": sc.ring(1, [128, KC, TN], BF16, "xsq"),
                    "ssp": sc.ring(1, [128, TN], F32, "ssp", psum=True),
                    "f32a": sc.ring(2, [128, TN], F32, "f32a"),
                    "f32b": sc.ring(2, [128, TN], F32, "f32b"),
                    "f32c": sc.ring(3, [128, TN], F32, "f32c"),
                }
                xring = sc.ring(2, [128, KC, TN], F32, "xt")
                hring = sc.ring(2, [128, KC, TN], BF16, "hT")
                cring = sc.ring(2, [128, TN], F32, "ropeC")
                sring = sc.ring(2, [128, TN], F32, "ropeS")
                pp = sc.ring(3, [128, TN], F32, "pp", psum=True)
                pq = sc.ring(1, [128, TN], F32, "pq", psum=True)
                pr = sc.ring(1, [128, TN], F32, "pr", psum=True)
                pv = sc.ring(1, [128, 128], F32, "pv", psum=True)
                qsqr = sc.ring(2, [128, TN], BF16, "qsq")
                qnr = sc.ring(2, [128, TN], F32, "qn")
                t1r = sc.ring(2, [128, TN], F32, "t1")
                t2r = sc.ring(2, [128, TN], F32, "t2")
                ur = sc.ring(2, [128, TN], F32, "ust")
                def load0(ti):
                    c0 = ti * TN
                    xt, xk = xring.next()
                    P.dma(xt[:], xin_v[:, :, c0:c0 + TN], writes=[xk])
                    ct, ck = cring.next()
                    st_, sk_ = sring.next()
                    P.dma(ct[:], self.ropeC[:, c0:c0 + TN], writes=[ck])
                    P.dma(st_[:], self.ropeS[:, c0:c0 + TN], writes=[sk_])
                    return xt, xk, ct, ck, st_, sk_
                nxt = load0(0)
                for ti in range(ntile):
                    c0 = ti * TN
                    s = 1 if ti == 0 else 0
                    xt, xk, ct, ck, st_, sk_ = nxt
                    if ti + 1 < ntile:
                        nxt = load0(ti + 1)
                    hT, hk = hring.next()
                    self.rmsnorm_mod(sc, xt, xk, TN, self.S1, 0, s, l, hT, hk, rings=rings)
                    for oc in range(6):
                        ps_, pk_ = pp.next()
                        for k in range(KC):
                            if oc < 4:
                                w = Win[:, k, oc * 128:(oc + 1) * 128]
                                wkey = "Win"
                            else:
                                w = Wk2[:, k, (oc - 4) * 128:(oc - 3) * 128]
                                wkey = "Wk2"
                            P.mm(ps_[:], w, hT[:, k, :], k == 0, k == KC - 1, [wkey, hk], [pk_])
                        gain = gqk[:, 0:1] if oc < 4 else gqk[:, 1:2]
                        qsq, qsqk = qsqr.next()
                        P.act(qsq[:], ps_[:], AF.Square, [pk_], [qsqk])
                        ssq, ssqk = pq.next()
                        P.mm(ssq[:], self.ones_bd[:], qsq[:], True, True, [qsqk, "ones_bd"], [ssqk])
                        sq, sqk = rings["f32a"].next()
                        P.act(sq[:], ssq[:], AF.Sqrt, [ssqk], [sqk], bias=self.eps_ap(), scale=1.0 / 64)
                        rs, rsk = rings["f32b"].next()
                        P.recip(rs[:], sq[:], [sqk], [rsk])
                        qn, qnk = qnr.next()
                        P.stt("vector", qn[:], ps_[:], gain, rs[:], ALU.mult, ALU.mult, [pk_, rsk, "gqk"], [qnk])
                        prp, prk = pr.next()
                        P.mm(prp[:], perm[:], qn[:], True, True, [qnk, "perm"], [prk])
                        t1, t1k = t1r.next()
                        P.tt("gpsimd", t1[:], qn[:], ct[:], ALU.mult, [qnk, ck], [t1k])
                        t2, t2k = t2r.next()
                        P.tt("vector", t2[:], prp[:], st_[:], ALU.mult, [prk, sk_], [t2k])
                        if oc < 4:
                            dst, dk = QT[:, oc, c0:c0 + TN], "QT"
                        else:
                            dst, dk = K2T[:, oc - 4, c0:c0 + TN], "K2T"
                        P.tt("gpsimd", dst, t1[:], t2[:], ALU.add, [t1k, t2k], [dk])
                    for j in range(TN // 128):
                        vp, vk = pv.next()
                        for k in range(KC):
                            P.mm(vp[:], hT[:, k, j * 128:(j + 1) * 128], Win[:, k, 640:768], k == 0, k == KC - 1,
                                 [hk, "Win"], [vk])
                        tix = c0 // 128 + j
                        for g in range(2):
                            P.cp("scalar", Vaug[:, g, tix, 0:64], vp[:, g * 64:(g + 1) * 64], [vk], ["Vaug"])
                            P.cp("vector", Vaug[:, g, tix, 128:192], vp[:, g * 64:(g + 1) * 64], [vk], ["Vaug"])
                    for g in range(4):
                        ps_, pk_ = pp.next()
                        for k in range(KC):
                            P.mm(ps_[:], Win[:, k, 768 + g * 128: 768 + (g + 1) * 128], hT[:, k, :], k == 0,
                                 k == KC - 1, ["Win", hk], [pk_])
                        ut, uk = ur.next()
                        P.cp("scalar", ut[:], ps_[:], [pk_], [uk])
                        P.dma(self.us[g * 128:(g + 1) * 128, c0:c0 + TN], ut[:], reads=[uk], writes=["us"])
                if "qkv" in self.dbg:
                    o = self.dout("dbg_QT", [128, 4 * T], BF16)
                    P.dma(o, QT[:].rearrange("p a t -> p (a t)"), reads=["QT"])
                    o = self.dout("dbg_K2T", [128, 2 * T], BF16)
                    P.dma(o, K2T[:].rearrange("p a t -> p (a t)"), reads=["K2T"])
                    o = self.dout("dbg_V", [128, 2 * (T // 128) * 192], BF16)
                    P.dma(o, Vaug[:].rearrange("p a t d -> p (a t d)"), reads=["Vaug"])
            with P.scope() as sc:
                PADL = 16
                WT = PADL + LC + PADL + S + PADL
                OFFC = PADL
                OFFX = PADL + LC + PADL
                pooled = sc.sb([128, 4, T], BF16, "pooled")
                pw = sc.sb([128, 4, 128], BF16, "pw")
                pws = sc.sb([128, 4, 128], F32, "pws")
                P.dma(pws[:], self.pool_w.rearrange("g c d -> c g d"), writes=["pws"])
                P.cp("gpsimd", pw[:], pws[:], ["pws"], ["pw"])
                psc = sc.sb([128, 4], F32, "psc")
                P.dma(psc[:], self.pool_s, writes=["psc"])
                pic = sc.sb([128, 4, 2, 8], F32, "pic")
                P.dma(pic[:], self.pool_ic, writes=["pic"])
                with P.scope() as sc2:
                    U = sc2.sb([128, WT], F32, "U")
                    A = sc2.sb([128, WT], F32, "A")
                    B = sc2.sb([128, WT], F32, "B")
                    for g in range(4):
                        w = (2, 4, 8, 16)[g]
                        P.memset("vector", U[:], 0.0, ["U"])
                        P.dma(U[:, OFFC:OFFC + LC], self.us[g * 128:(g + 1) * 128, 0:LC], reads=["us"], writes=["U"])
                        P.dma(U[:, OFFX:OFFX + S], self.us[g * 128:(g + 1) * 128, LC:T], reads=["us"], writes=["U"])
                        lo, hi = 8, WT - 8
                        P.memset("gpsimd", A[:], 0.0, ["A"])
                        P.memset("gpsimd", B[:], 0.0, ["B"])
                        P.tt("vector", A[:, lo:hi], U[:, lo - 1:hi - 1], U[:, lo:hi], ALU.add, ["U"], ["A"])
                        cur, curk, oth, othk = A, "A", B, "B"
                        sh = 1
                        ww = 2
                        while ww < w:
                            d = ww // 2
                            P.tt("vector", oth[:, lo:hi], cur[:, lo - d:hi - d], cur[:, lo + d:hi + d], ALU.add,
                                 [curk], [othk])
                            cur, curk, oth, othk = oth, othk, cur, curk
                            ww *= 2
                        pl, plk = oth, othk
                        P.stt("vector", pl[:, lo:hi], cur[:, lo:hi], 1.0 / w, U[:, lo:hi], ALU.mult, ALU.subtract,
                              [curk, "U"], [plk])
                        for (off, L) in ((OFFC, LC), (OFFX, S)):
                            nl = w // 2
                            tmpb = sc2.sb([128, 8], F32)
                            P.tt("vector", tmpb[:, 0:nl], cur[:, off:off + nl], pic[:, g, 0, 0:nl], ALU.mult,
                                 [curk, "pic"], ["tmpb"])
                            P.tt("vector", pl[:, off:off + nl], tmpb[:, 0:nl], U[:, off:off + nl], ALU.subtract,
                                 ["tmpb", "U"], [plk])
                            nr = w // 2 - 1
                            if nr > 0:
                                r0 = off + L - nr
                                tmpc = sc2.sb([128, 8], F32)
                                P.tt("vector", tmpc[:, 0:nr], cur[:, r0:r0 + nr], pic[:, g, 1, 0:nr], ALU.mult,
                                     [curk, "pic"], ["tmpc"])
                                P.tt("vector", pl[:, r0:r0 + nr], tmpc[:, 0:nr], U[:, r0:r0 + nr], ALU.subtract,
                                     ["tmpc", "U"], [plk])
                        P.cp("gpsimd", pooled[:, g, 0:LC], pl[:, OFFC:OFFC + LC], [plk], ["pooled"])
                        P.cp("gpsimd", pooled[:, g, LC:T], pl[:, OFFX:OFFX + S], [plk], ["pooled"])
                self.attention0(QT, K2T, Vaug)
                with P.scope() as sc2:
                    pmm = sc2.ring(2, [128, 512], F32, "pmm", psum=True)
                    ob = sc2.ring(2, [128, 512], BF16, "pob")
                    for g in range(4):
                        for c0 in range(0, T, 512):
                            n = min(512, T - c0)
                            ps_, pk_ = pmm.next()
                            P.mm(ps_[:, :n], pw[:, g, :], pooled[:, g, c0:c0 + n], True, True, ["pw", "pooled"], [pk_])
                            o_, ok_ = ob.next()
                            P.act(o_[:, :n], ps_[:, :n], AF.Identity, [pk_, "psc"], [ok_], scale=psc[:, g:g + 1])
                            P.dma(self.aTs[(4 + g) * 128:(5 + g) * 128, c0:c0 + n], o_[:, :n], reads=[ok_],
                                  writes=["aTs"])

    def attention0(self, QT, K2T, Vaug):
        P = self.P
        A_SCALE = 64 ** -0.5
        with P.scope() as sc:
            sps = sc.ring(4, [128, 512], F32, "sps", psum=True)
            oA = sc.ring(2, [128, 512], F32, "oA", psum=True)
            oB = sc.ring(2, [128, 512], F32, "oB", psum=True)
            pt = sc.ring(4, [128, 512], BF16, "pt")
            Rr = sc.ring(2, [128, 512], F32, "R")
            ao = sc.ring(2, [128, 512], BF16, "ao")
            qtiles = [(0, LC, 2)] + [(LC + i * 512, 512, T // 128) for i in range(S // 512)]
            for c in range(4):
                g = c // 2
                for (q0, N, nkc) in qtiles:
                    a_, ak = oA.next()
                    b_, bk = oB.next()
                    iters = [(kc, h) for kc in range(nkc) for h in range(2)]
                    LA = 3
                    pend = {}

                    def emitS(i, g=g, c=c, q0=q0, N=N, iters=iters, pend=pend):
                        kc, h = iters[i]
                        hp = slice(h * 64, h * 64 + 64)
                        sp_, spk = sps.next()
                        P.mm(sp_[:, :N], K2T[hp, g, kc * 128:(kc + 1) * 128], QT[hp, c, q0:q0 + N], True, True,
                             ["K2T", "QT"], [spk])
                        pend[i] = (sp_, spk)

                    for i in range(min(LA, len(iters))):
                        emitS(i)
                    for i in range(len(iters)):
                        if i + LA < len(iters):
                            emitS(i + LA)
                        kc, h = iters[i]
                        sp_, spk = pend.pop(i)
                        p_, pk_ = pt.next()
                        P.act(p_[:, :N], sp_[:, :N], AF.Exp, [spk], [pk_], scale=A_SCALE)
                        if h == 0:
                            P.mm(a_[:, :N], Vaug[:, g, kc, 0:128], p_[:, :N], kc == 0, kc == nkc - 1,
                                 ["Vaug", pk_], [ak])
                        else:
                            P.mm(b_[:, :N], Vaug[:, g, kc, 64:192], p_[:, :N], kc == 0, kc == nkc - 1,
                                 ["Vaug", pk_], [bk])
                    R, Rk = Rr.next()
                    P.recip(R[0:64, :N], a_[64:128, :N], [ak], [Rk])
                    P.recip(R[64:128, :N], b_[0:64, :N], [bk], [Rk])
                    o_, ok_ = ao.next()
                    P.tt("vector", o_[0:64, :N], a_[0:64, :N], R[0:64, :N], ALU.mult, [ak, Rk], [ok_])
                    P.tt("vector", o_[64:128, :N], b_[64:128, :N], R[64:128, :N], ALU.mult, [bk, Rk], [ok_])
                    P.dma(self.aTs[c * 128:(c + 1) * 128, q0:q0 + N], o_[:, :N], reads=[ok_], writes=["aTs"])

    def layer1_front(self):
        P = self.P
        l = 1
        TN = 256
        ntile = T // TN
        xv = self.xres.rearrange("(k p) t -> p k t", p=128)
        C_SCALE = 64 ** -0.5
        with P.scope() as sc0:
            hT = sc0.sb([128, KC, T], BF16, "hT1")
            with P.scope() as sc:
                rings = {
                    "xsq": sc.ring(1, [128, KC, TN], BF16, "xsq"),
                    "ssp": sc.ring(1, [128, TN], F32, "ssp", psum=True),
                    "f32a": sc.ring(2, [128, TN], F32, "f32a"),
                    "f32b": sc.ring(2, [128, TN], F32, "f32b"),
                    "f32c": sc.ring(3, [128, TN], F32, "f32c"),
                }
                xring = sc.ring(2, [128, KC, TN], F32, "xt")
                hring = sc.ring(2, [128, KC, TN], BF16, "hTt")
                def load1(ti):
                    c0 = ti * TN
                    xt, xk = xring.next()
                    P.dma(xt[:], xv[:, :, c0:c0 + TN], reads=["xres"], writes=[xk])
                    return xt, xk
                nxt = load1(0)
                for ti in range(ntile):
                    c0 = ti * TN
                    s = 1 if ti == 0 else 0
                    xt, xk = nxt
                    if ti + 1 < ntile:
                        nxt = load1(ti + 1)
                    ht, hk = hring.next()
                    self.rmsnorm_mod(sc, xt, xk, TN, self.S1, 0, s, l, ht, hk, rings=rings)
                    P.cp("gpsimd", hT[:, :, c0:c0 + TN], ht[:], [hk], ["hT1"])
            if "h1" in self.dbg:
                o = self.dout("dbg_h1", [128, KC * T], BF16)
                P.dma(o, hT[:].rearrange("p a t -> p (a t)"), reads=["hT1"])
            with P.scope() as sc:
                owv = self.o_win.rearrange("(k p) n -> p k n", p=128)
                stg = sc.ring(2, [128, KC, 128], F32, "stg1")
                wq = sc.ring(2, [128, KC, 128], BF16, "wq")
                wk = sc.ring(2, [128, KC, 128], BF16, "wk")
                wv = sc.ring(2, [128, KC, 128], BF16, "wv")
                qT = sc.sb([128, S], BF16, "qT1")
                kT = sc.sb([128, T], BF16, "kT1")
                Vaug = sc.sb([128, T // 128, 192], BF16, "Vaug1")
                P.memset("gpsimd", Vaug[:, :, 64:128], 1.0, ["Vaug1"])
                bias = sc.sb([128, 2, 14, 256], F32, "nab")
                pp = sc.ring(2, [128, 512], F32, "pp1", psum=True)
                pv = sc.ring(1, [128, 128], F32, "pv1", psum=True)
                sps = sc.ring(3, [128, 256], F32, "sps1", psum=True)
                oA = sc.ring(1, [128, 256], F32, "oA1", psum=True)
                oB = sc.ring(1, [128, 256], F32, "oB1", psum=True)
                ssb = sc.ring(3, [128, 256], F32, "ssb1")
                pt = sc.ring(4, [128, 256], BF16, "pt1")
                Rr = sc.ring(2, [128, 256], F32, "R1")
                ao = sc.ring(2, [128, 256], BF16, "ao1")
                for c in range(8):
                    wq_, wqk = wq.next()
                    wk_, wkk = wk.next()
                    wv_, wvk = wv.next()
                    self.load_cast(stg, wq_[:], owv[:, :, c * 128:(c + 1) * 128], [128, KC, 128], [wqk])
                    self.load_cast(stg, wk_[:], owv[:, :, D + c * 128:D + (c + 1) * 128], [128, KC, 128], [wkk])
                    self.load_cast(stg, wv_[:], owv[:, :, 2 * D + c * 128:2 * D + (c + 1) * 128], [128, KC, 128], [wvk])
                    for hh in range(2):
                        P.dma(bias[:, hh], self.nab[2 * c + hh], writes=["nab"])
                    for c0 in range(0, S, 512):
                        ps_, pk_ = pp.next()
                        for k in range(KC):
                            P.mm(ps_[:], wq_[:, k, :], hT[:, k, LC + c0:LC + c0 + 512], k == 0, k == KC - 1,
                                 [wqk, "hT1"], [pk_])
                        P.act(qT[:, c0:c0 + 512], ps_[:], AF.Identity, [pk_], ["qT1"], scale=C_SCALE)
                    for c0 in range(0, T, 512):
                        n = min(512, T - c0)
                        ps_, pk_ = pp.next()
                        for k in range(KC):
                            P.mm(ps_[:, :n], wk_[:, k, :], hT[:, k, c0:c0 + n], k == 0, k == KC - 1,
                                 [wkk, "hT1"], [pk_])
                        P.cp("vector", kT[:, c0:c0 + n], ps_[:, :n], [pk_], ["kT1"])
                    for tix in range(T // 128):
                        vp, vk = pv.next()
                        for k in range(KC):
                            P.mm(vp[:], hT[:, k, tix * 128:(tix + 1) * 128], wv_[:, k, :], k == 0, k == KC - 1,
                                 ["hT1", wvk], [vk])
                        P.cp("scalar", Vaug[:, tix, 0:64], vp[:, 0:64], [vk], ["Vaug1"])
                        P.cp("vector", Vaug[:, tix, 128:192], vp[:, 64:128], [vk], ["Vaug1"])
                    for qb in range(16):
                        if qb == 0:
                            kr0, nloc, tb = 0, 4, 6
                        elif qb == 15:
                            kr0, nloc, tb = 56, 4, 10
                        else:
                            kr0, nloc, tb = 4 * qb - 4, 6, 0
                        chunks = [(LC + 64 * kr0 + 128 * j, tb + j) for j in range(nloc)] + [(0, None), (128, None)]
                        a_, ak = oA.next()
                        b_, bk = oB.next()
                        q0 = qb * 256
                        nch = len(chunks)
                        iters = [(ci, h) for ci in range(nch) for h in range(2)]
                        LA = 2
                        pend = {}

                        def emitS1(i, iters=iters, chunks=chunks, q0=q0, pend=pend):
                            ci, h = iters[i]
                            k0 = chunks[ci][0]
                            hp = slice(h * 64, h * 64 + 64)
                            sp_, spk = sps.next()
                            P.mm(sp_[:], kT[hp, k0:k0 + 128], qT[hp, q0:q0 + 256], True, True,
                                 ["kT1", "qT1"], [spk])
                            pend[i] = (sp_, spk)

                        for i in range(LA):
                            emitS1(i)
                        for i in range(len(iters)):
                            if i + LA < len(iters):
                                emitS1(i + LA)
                            ci, h = iters[i]
                            k0, bt = chunks[ci]
                            sp_, spk = pend.pop(i)
                            p_, pk_ = pt.next()
                            if bt is not None:
                                sb_, sbk = ssb.next()
                                P.tt("vector", sb_[:], sp_[:], bias[:, h, bt, :], ALU.add, [spk, "nab"], [sbk])
                                P.act(p_[:], sb_[:], AF.Exp, [sbk], [pk_])
                            else:
                                P.act(p_[:], sp_[:], AF.Exp, [spk], [pk_])
                            tix = k0 // 128
                            if h == 0:
                                P.mm(a_[:], Vaug[:, tix, 0:128], p_[:], ci == 0, ci == nch - 1, ["Vaug1", pk_], [ak])
                            else:
                                P.mm(b_[:], Vaug[:, tix, 64:192], p_[:], ci == 0, ci == nch - 1, ["Vaug1", pk_], [bk])
                        R, Rk = Rr.next()
                        P.recip(R[0:64, :], a_[64:128, :], [ak], [Rk])
                        P.recip(R[64:128, :], b_[0:64, :], [bk], [Rk])
                        o_, ok_ = ao.next()
                        P.tt("vector", o_[0:64, :], a_[0:64, :], R[0:64, :], ALU.mult, [ak, Rk], [ok_])
                        P.tt("vector", o_[64:128, :], b_[64:128, :], R[64:128, :], ALU.mult, [bk, Rk], [ok_])
                        P.dma(self.aTs[c * 128:(c + 1) * 128, LC + q0:LC + q0 + 256], o_[:], reads=[ok_],
                              writes=["aTs"])

    def phase_post(self, l):
        P = self.P
        TN = 256
        t0 = 0 if l == 0 else LC
        src = self.xin if l == 0 else self.xres
        srck = "xin" if l == 0 else "xres"
        xv = src.rearrange("(k p) t -> p k t", p=128)
        xo = self.xres.rearrange("(k p) t -> p k t", p=128)
        h2v = self.h2s.rearrange("(k p) t -> p k t", p=128)
        aTv = self.aTs.rearrange("(k p) t -> p k t", p=128)
        wout_d = (self.e_wout if l == 0 else self.o_wout).rearrange("(k p) n -> p k n", p=128)
        with P.scope() as sc:
            stg = sc.ring(2, [128, KC, 256], F32, "stgp")
            Wout = sc.sb([128, KC, D], BF16, "Wout")
            for j in range(4):
                self.load_cast(stg, Wout[:, :, j * 256:(j + 1) * 256], wout_d[:, :, j * 256:(j + 1) * 256],
                               [128, KC, 256], ["Wout"])
            wr = sc.sb([128, KC, 36], F32, "wr")
            P.dma(wr[:], self.wr[l].rearrange("(k p) n -> p k n", p=128), writes=["wr"])
            br = sc.sb([128, 36], F32, "br")
            P.dma(br[:], self.br[:, l, :], writes=["br"])
            rings = {
                "xsq": sc.ring(1, [128, KC, TN], BF16, "xsq"),
                "ssp": sc.ring(1, [128, TN], F32, "ssp", psum=True),
                "f32a": sc.ring(2, [128, TN], F32, "f32a"),
                "f32b": sc.ring(2, [128, TN], F32, "f32b"),
                "f32c": sc.ring(3, [128, TN], F32, "f32c"),
            }
            xring = sc.ring(2, [128, KC, TN], F32, "xtp")
            aring = sc.ring(2, [128, KC, TN], BF16, "aTt")
            xnring = sc.ring(2, [128, KC, TN], F32, "xn")
            hfring = sc.ring(1, [128, KC, TN], F32, "h2f")
            hbring = sc.ring(2, [128, KC, TN], BF16, "h2b")
            yp = sc.ring(3, [128, TN], F32, "yp", psum=True)
            lp = sc.ring(1, [128, 36], F32, "lp", psum=True)
            wtp = sc.ring(1, [32, 128], F32, "wtp", psum=True)
            wtr = sc.ring(2, [32, TN], F32, "wtr")
            sm = sc.ring(2, [128, 160], F32, "sm")
            def loadp(c0):
                xt, xk = xring.next()
                P.dma(xt[:], xv[:, :, c0:c0 + TN], reads=[srck], writes=[xk])
                at, atk = aring.next()
                P.dma(at[:], aTv[:, :, c0:c0 + TN], reads=["aTs"], writes=[atk])
                return xt, xk, at, atk
            tiles_p = list(range(t0, T, TN))

            def emitA(c0, ld):
                s = 1 if c0 < LC else 0
                xt, xk, at, atk = ld
                xn, xnk = xnring.next()
                for oc in range(KC):
                    ps_, pk_ = yp.next()
                    for k in range(KC):
                        P.mm(ps_[:], Wout[:, k, oc * 128:(oc + 1) * 128], at[:, k, :], k == 0, k == KC - 1,
                             ["Wout", atk], [pk_])
                    P.stt("vector", xn[:, oc, :], ps_[:], self.mvec(l, 2, oc, s), xt[:, oc, :], ALU.mult, ALU.add,
                          [pk_, xk, "mod"], [xnk])
                P.dma(xo[:, :, c0:c0 + TN], xn[:], reads=[xnk], writes=["xres"])
                return xn, xnk

            ld0 = loadp(tiles_p[0])
            ld1 = loadp(tiles_p[1])
            nxtA = emitA(tiles_p[0], ld0)
            for i, c0 in enumerate(tiles_p):
                s = 1 if c0 < LC else 0
                xn, xnk = nxtA
                if i + 1 < len(tiles_p):
                    ld_next = ld1
                    if i + 2 < len(tiles_p):
                        ld1 = loadp(tiles_p[i + 2])
                    nxtA = emitA(tiles_p[i + 1], ld_next)
                hf, hfk = hfring.next()
                hb, hbk = hbring.next()
                self.rmsnorm_mod(sc, xn, xnk, TN, self.S2, 3, s, l, hb, hbk, out_f32=hf, out_f32_key=hfk, rings=rings)
                P.dma(h2v[:, :, c0:c0 + TN], hb[:], reads=[hbk], writes=["h2s"])
                wt_, wtk = wtr.next()
                for j in range(TN // 128):
                    lg, lgk = lp.next()
                    for k in range(KC):
                        P.mm(lg[:], hf[:, k, j * 128:(j + 1) * 128], wr[:, k, :], k == 0, k == KC - 1, [hfk, "wr"], [lgk])
                    m, mk = sm.next()
                    L = m[:, 0:36]
                    gmax = m[:, 36:37]
                    ngmax = m[:, 37:38]
                    gsum = m[:, 38:39]
                    gw = m[:, 39:40]
                    gmk = m[:, 40:44]
                    pen = m[:, 44:48]
                    Lm = m[:, 48:80]
                    mk1 = m[:, 80:112]
                    Lm2 = m[:, 112:144]
                    m1 = m[:, 144:145]
                    m2 = m[:, 145:146]
                    dd = m[:, 146:147]
                    ee = m[:, 147:148]
                    w1 = m[:, 148:149]
                    w2 = m[:, 149:150]
                    gex = m[:, 150:154]
                    rk = [mk]
                    P.tt("vector", L, lg[:], br[:], ALU.add, [lgk, "br"], rk)
                    P.rmax(gmax, m[:, 0:4], rk, rk)
                    P.ts("vector", ngmax, gmax, -1.0, None, ALU.mult, None, rk, rk)
                    P.ts("vector", gmk, m[:, 0:4], gmax, None, ALU.is_ge, None, rk, rk)
                    P.act(gex, m[:, 0:4], AF.Exp, rk, rk, bias=ngmax, scale=1.0, accum_out=gsum)
                    P.recip(gw, gsum, rk, rk)
                    P.ts("vector", pen, gmk, 1e30, -1e30, ALU.mult, ALU.add, rk, rk)
                    P.tt("vector", Lm.rearrange("p (g e) -> p g e", g=4), m[:, 4:36].rearrange("p (g e) -> p g e", g=4),
                         pen.unsqueeze(2).to_broadcast([128, 4, 8]), ALU.add, rk, rk)
                    P.rmax(m1, Lm, rk, rk)
                    P.ts("vector", mk1, Lm, m1, None, ALU.is_ge, None, rk, rk)
                    P.stt("vector", Lm2, mk1, -1e30, Lm, ALU.mult, ALU.add, rk, rk)
                    P.rmax(m2, Lm2, rk, rk)
                    mk2 = m[:, 48:80]
                    P.ts("vector", mk2, Lm2, m2, None, ALU.is_ge, None, rk, rk)
                    P.tt("vector", dd, m2, m1, ALU.subtract, rk, rk)
                    P.act(ee, dd, AF.Exp, rk, rk)
                    P.ts("vector", ee, ee, 1.0, None, ALU.add, None, rk, rk)
                    P.recip(ee, ee, rk, rk)
                    P.tt("vector", w1, gw, ee, ALU.mult, rk, rk)
                    P.tt("vector", w2, gw, w1, ALU.subtract, rk, rk)
                    wm = m[:, 112:144]
                    P.ts("vector", mk1, mk1, w1, None, ALU.mult, None, rk, rk)
                    P.stt("vector", wm, mk2, w2, mk1, ALU.mult, ALU.add, rk, rk)
                    tp, tpk = wtp.next()
                    P.mm(tp[:], wm, self.ident[:], True, True, rk + ["ident"], [tpk])
                    P.cp("scalar", wt_[:, j * 128:(j + 1) * 128], tp[:], [tpk], [wtk])
                P.dma(self.wts[:, c0:c0 + TN], wt_[:], reads=[wtk], writes=["wts"])
            if f"post{l}" in self.dbg:
                pass

    def phase_moe(self, l):
        P = self.P
        last = (l == 1)
        t0 = 0 if l == 0 else LC
        if l == 0:
            stiles = [[(0, 384), (384, 384), (768, 384)], [(0, 384), (384, 384), (768, 384)],
                      [(0, 512), (512, 512)], [(0, 512), (512, 512)]]
        else:
            stiles = [[(0, 512), (512, 512)]] * 4
        st_sizes = [sum(n for _, n in st) for st in stiles]
        STN = max(st_sizes)
        NT = 512
        xv = self.xres.rearrange("(k p) t -> p k t", p=128)
        h2v = self.h2s.rearrange("(k p) t -> p k t", p=128)
        outv = self.outT.rearrange("(k p) t -> p k t", p=128)
        w1v = self.w1[l].rearrange("e (k p) n -> e p k n", p=128)
        w3v = self.w3[l].rearrange("e (k p) n -> e p k n", p=128)
        w2v = self.w2[l].rearrange("e (k p) n -> e p k n", p=128)
        with P.scope() as sc:
            stg = sc.ring(6, [128, 4, DE], F32, "stgm")
            sel = sc.sb([32, NEXP * 128], BF16, "sel")
            for q_ in range(4):
                self.load_cast(stg, sel[:, q_ * 1024:(q_ + 1) * 1024].rearrange("p (a b) -> p a b", a=2),
                               self.sel_d[:, q_ * 1024:(q_ + 1) * 1024].rearrange("p (a b) -> p a b", a=2),
                               [32, 2, 512], ["sel"], eng="vector")
            yacc = sc.sb([128, KC, STN], F32, "yacc")
            h2 = sc.sb([128, KC, STN], BF16, "h2")
            wts = sc.sb([32, STN], BF16, "wts_sb")
            wtsf = sc.sb([32, STN], F32, "wts_f")
            W1b = sc.ring(2, [128, KC, DE], BF16, "W1b")
            W3b = sc.ring(2, [128, KC, DE], BF16, "W3b")
            W2b = sc.ring(2, [128, 4, D], BF16, "W2b")
            wbp = sc.ring(1, [128, NT], F32, "wbp", psum=True)
            ap_ = sc.ring(2, [128, NT], F32, "aps", psum=True)
            bp_ = sc.ring(2, [128, NT], F32, "bps", psum=True)
            ypr = sc.ring(3, [128, NT], F32, "ypm", psum=True)
            wbs = sc.ring(2, [128, NT], F32, "wbs")
            sar = sc.ring(2, [128, NT], F32, "sa")
            tr = sc.ring(2, [128, NT], F32, "tmoe")
            hbr = sc.ring(2, [128, 4, NT], BF16, "hb")
            FN = 256
            xfr = sc.ring(2, [128, KC, FN], F32, "xf")
            if last:
                rings = {
                    "xsq": sc.ring(1, [128, KC, FN], BF16, "xsq"),
                    "f32a": sc.ring(1, [128, FN], F32, "f32a"),
                    "f32b": sc.ring(1, [128, FN], F32, "f32b"),
                }
            seq = [(st_, e_) for st_ in range(4) for e_ in range(NEXP)]
            loaded = {}
            staged = {}

            def issue_dma(j):
                if j >= len(seq):
                    return
                e = seq[j][1]
                srcs = [w1v[e][:, 0:4, :], w1v[e][:, 4:8, :], w3v[e][:, 0:4, :], w3v[e][:, 4:8, :],
                        w2v[e][:, :, 0:DE], w2v[e][:, :, DE:2 * DE]]
                pieces = []
                for src in srcs:
                    st_, sk = stg.next()
                    P.dma(st_[:], src, writes=[sk])
                    pieces.append((st_, sk))
                staged[j] = pieces

            def issue_cast(j):
                if j >= len(seq):
                    return
                pieces = staged.pop(j)
                w1b, w1k = W1b.next()
                w3b, w3k = W3b.next()
                w2b, w2k = W2b.next()
                dsts = [w1b[:, 0:4, :], w1b[:, 4:8, :], w3b[:, 0:4, :], w3b[:, 4:8, :],
                        w2b[:, :, 0:DE], w2b[:, :, DE:2 * DE]]
                keys = [w1k, w1k, w3k, w3k, w2k, w2k]
                for (st_, sk), dst, kk in zip(pieces, dsts, keys):
                    P.cp("scalar", dst, st_[:], [sk], [kk])
                loaded[j] = (w1b, w1k, w3b, w3k, w2b, w2k)

            def emit_ab(W, e, c0, n):
                w1b, w1k, w3b, w3k, w2b, w2k = W
                wb, wbk = wbp.next()
                P.mm(wb[:, :n], sel[:, e * 128:(e + 1) * 128], wts[:, c0:c0 + n], True, True,
                     ["sel", "wts_sb"], [wbk])
                wb_s, wbsk = wbs.next()
                P.cp("scalar", wb_s[:, :n], wb[:, :n], [wbk], [wbsk])
                hb, hbk = hbr.next()
                for hc in range(4):
                    a_, ak = ap_.next()
                    b_, bk = bp_.next()
                    for k in range(KC):
                        P.mm(a_[:, :n], w1b[:, k, hc * 128:(hc + 1) * 128], h2[:, k, c0:c0 + n], k == 0,
                             k == KC - 1, [w1k, "h2"], [ak])
                    for k in range(KC):
                        P.mm(b_[:, :n], w3b[:, k, hc * 128:(hc + 1) * 128], h2[:, k, c0:c0 + n], k == 0,
                             k == KC - 1, [w3k, "h2"], [bk])
                    sa, sak = sar.next()
                    P.act(sa[:, :n], a_[:, :n], AF.Silu, [ak], [sak])
                    t_, tk = tr.next()
                    P.tt("vector", t_[:, :n], b_[:, :n], wb_s[:, :n], ALU.mult, [bk, wbsk], [tk])
                    P.tt("gpsimd", hb[:, hc, :n], t_[:, :n], sa[:, :n], ALU.mult, [tk, sak], [hbk])
                return hb, hbk

            def emit_w2(W, hb, hbk, ti, c0, n):
                w1b, w1k, w3b, w3k, w2b, w2k = W
                for dc in range(KC):
                    y_, yk = ypr.next()
                    for hc in range(4):
                        P.mm(y_[:, :n], w2b[:, hc, dc * 128:(dc + 1) * 128], hb[:, hc, :n], hc == 0, hc == 3,
                             [w2k, hbk], [yk])
                    P.tt("vector", yacc[:, dc, c0:c0 + n], y_[:, :n], yacc[:, dc, c0:c0 + n], ALU.add,
                         [yk, ("yacc", dc, ti)], [("yacc", dc, ti)])

            def load_st(st, s0):
                stn = st_sizes[st]
                P.dma(h2[:, :, 0:stn], h2v[:, :, s0:s0 + stn], writes=["h2"])
                P.dma(wtsf[:, 0:stn], self.wts[:, s0:s0 + stn], writes=["wts_f"])
                P.cp("vector", wts[:, 0:stn], wtsf[:, 0:stn], ["wts_f"], ["wts_sb"])

            issue_dma(0)
            issue_cast(0)
            issue_dma(1)
            issue_cast(1)
            issue_dma(2)
            s0 = t0
            for st in range(4):
                tiles = stiles[st]
                stn = st_sizes[st]
                nti = len(tiles)
                if st == 0:
                    load_st(0, s0)
                P.memset("gpsimd", yacc[:], 0.0, [("yacc", dc_, ti_) for dc_ in range(KC) for ti_ in range(4)])
                prev = None

                def flush(prev):
                    W, hb, hbk, ti, c0, n, lastt, j = prev
                    emit_w2(W, hb, hbk, ti, c0, n)
                    if lastt:
                        del loaded[j]
                        issue_cast(j + 2)
                        issue_dma(j + 3)

                for e in range(NEXP):
                    j = st * NEXP + e
                    W = loaded[j]
                    for ti, (c0, n) in enumerate(tiles):
                        hb, hbk = emit_ab(W, e, c0, n)
                        if e == NEXP - 1 and ti == nti - 1 and st < 3:
                            load_st(st + 1, s0 + stn)
                        if prev is not None:
                            flush(prev)
                        prev = (W, hb, hbk, ti, c0, n, ti == nti - 1, j)
                flush(prev)
                for ti, (tc0, tn) in enumerate(tiles):
                    for cc in range(0, tn, FN):
                        n = min(FN, tn - cc)
                        c0 = tc0 + cc
                        g0 = s0 + c0
                        xf, xfk = xfr.next()
                        P.dma(xf[:, :, :n], xv[:, :, g0:g0 + n], writes=[xfk])
                        segs = []
                        if g0 < LC:
                            nb = min(LC, g0 + n) - g0
                            segs.append((0, nb, 1))
                            if nb < n:
                                segs.append((nb, n, 0))
                        else:
                            segs.append((0, n, 0))
                        for (a, b, s) in segs:
                            for k in range(KC):
                                P.stt("vector", xf[:, k, a:b], yacc[:, k, c0 + a:c0 + b], self.mvec(l, 5, k, s),
                                      xf[:, k, a:b], ALU.mult, ALU.add, [("yacc", k, ti), xfk, "mod"], [xfk])
                        if not last:
                            P.dma(xv[:, :, g0:g0 + n], xf[:, :, :n], reads=[xfk])
                        else:
                            xsq, xsqk = rings["xsq"].next()
                            P.act(xsq[:, :, :n], xf[:, :, :n], AF.Square, [xfk], [xsqk])
                            ssp, sspk = wbp.next()
                            for k in range(KC):
                                P.mm(ssp[:, :n], self.ones_all[:], xsq[:, k, :n], k == 0, k == KC - 1,
                                     [xsqk, "ones_all"], [sspk])
                            sq, sqk = rings["f32a"].next()
                            P.act(sq[:, :n], ssp[:, :n], AF.Sqrt, [sspk], [sqk], bias=self.eps_ap(), scale=1.0 / D)
                            rstd, rk = rings["f32b"].next()
                            P.recip(rstd[:, :n], sq[:, :n], [sqk], [rk])
                            for k in range(KC):
                                P.stt("vector", xf[:, k, :n], xf[:, k, :n], self.gfin_sb[:, k:k + 1], rstd[:, :n],
                                      ALU.mult, ALU.mult, [xfk, rk, "gfin"], [xfk])
                            P.dma(outv[:, :, g0 - LC:g0 - LC + n], xf[:, :, :n], reads=[xfk])
                s0 += stn


def _feat(v):
    return np.ascontiguousarray(np.asarray(v, np.float32).reshape(KC, 128).T)


def host_constants():
    cst = {}
    cst["ident"] = np.eye(128, dtype=np.float32)
    perm = np.zeros((128, 128), np.float32)
    for p in range(128):
        partner = p + 16 if (p % 32) < 16 else p - 16
        perm[partner, p] = 1.0
    cst["perm"] = perm
    t = np.arange(S)
    pos = np.stack([t // GRID, t % GRID], -1).astype(np.float32)
    inv = (10000.0 ** (-np.arange(16, dtype=np.float32) / 16)).astype(np.float32)
    C = np.ones((128, T), np.float32)
    Sn = np.zeros((128, T), np.float32)
    for p in range(128):
        pp = p % 64
        axis = pp // 32
        half = (pp % 32) // 16
        f = pp % 16
        ang = (pos[:, axis] * inv[f]).astype(np.float32)
        C[p, LC:] = np.cos(ang)
        Sn[p, LC:] = np.sin(ang) * (-1.0 if half == 0 else 1.0)
    cst["ropeC"] = C
    cst["ropeS"] = Sn
    ic = np.ones((128, 4, 2, 8), np.float32)
    for g, w in enumerate((2, 4, 8, 16)):
        for i in range(w // 2):
            ic[:, g, 0, i] = 1.0 / (i + w // 2)
        nr = w // 2 - 1
        for i in range(nr):
            ic[:, g, 1, i] = 1.0 / (nr - i + w // 2)
    cst["pool_ic"] = ic
    sel = np.zeros((32, NEXP, 128), np.float32)
    for e in range(NEXP):
        sel[e, e, :] = 1.0
    cst["sel"] = sel.reshape(32, NEXP * 128)
    return cst


def na_bias_tiles(rel_bias):
    H = rel_bias.shape[0]
    out = np.full((H, 14, 128, 256), NEG, np.float32)
    cols = np.arange(GRID)
    cstart = np.clip(cols - 8, 0, GRID - 16)

    def fill(tile, kr_abs, qr_abs_list, kh_slot):
        for qi, qr in enumerate(qr_abs_list):
            rs = min(max(qr - 4, 0), GRID - 8)
            if not (rs <= kr_abs < rs + 8):
                continue
            ro = kr_abs - qr + 7
            for qc in range(GRID):
                kcs = np.arange(cstart[qc], cstart[qc] + 16)
                out[:, tile, kh_slot * 64 + kcs, qi * 64 + qc] = rel_bias[:, ro, kcs - qc + 15]

    for j in range(6):
        for hslot in range(2):
            fill(j, 12 + 2 * j + hslot, [16, 17, 18, 19], hslot)
    for j in range(4):
        for hslot in range(2):
            fill(6 + j, 0 + 2 * j + hslot, [0, 1, 2, 3], hslot)
            fill(10 + j, 56 + 2 * j + hslot, [60, 61, 62, 63], hslot)
    return np.ascontiguousarray(out.transpose(0, 2, 1, 3))


def make_in_maps(inputs, ncores=8, layers=(0, 1), moe=True):
    f32 = lambda a: np.ascontiguousarray(np.asarray(a, np.float32))
    cst = host_constants()
    x = np.asarray(inputs["x"], np.float32)
    ctx = np.asarray(inputs["ctx"], np.float32)
    c = np.asarray(inputs["c"], np.float32)
    c_ctx = np.asarray(inputs["c_ctx"], np.float32)
    shared = {}
    shared["ada_w"] = f32(inputs["ada_w"])
    ada_b = np.asarray(inputs["ada_b"], np.float32)
    shared["adab"] = np.ascontiguousarray(ada_b.reshape(2, 48, 128).transpose(2, 0, 1))
    shared["gmix"] = np.ascontiguousarray(np.stack([_feat(inputs["norm_mix_g"][l]) for l in range(2)], 1))
    shared["gffn"] = np.ascontiguousarray(np.stack([_feat(inputs["norm_ffn_g"][l]) for l in range(2)], 1))
    shared["gfin"] = _feat(inputs["final_g"])
    shared["ident"] = cst["ident"]
    if 0 in layers:
        shared["e_win"] = f32(inputs["even_w_in"][0])
        shared["e_wout"] = f32(inputs["even_w_out"][0])
        gq = np.asarray(inputs["a_q_gain"][0], np.float32)
        gk = np.asarray(inputs["a_k_gain"][0], np.float32)
        shared["gqk"] = np.ascontiguousarray(np.stack([np.tile(gq, 2), np.tile(gk, 2)], 1))
        shared["perm"] = cst["perm"]
        shared["ropeC"] = cst["ropeC"]
        shared["ropeS"] = cst["ropeS"]
        shared["pool_w"] = f32(inputs["pool_w"][0])
        shared["pool_s"] = np.ascontiguousarray(np.asarray(inputs["pool_scale"][0], np.float32).reshape(4, 128).T)
        shared["pool_ic"] = cst["pool_ic"]
    if 1 in layers:
        shared["o_win"] = f32(inputs["odd_w_in"][0])
        shared["o_wout"] = f32(inputs["odd_w_out"][0])
        shared["nab"] = na_bias_tiles(np.asarray(inputs["na_rel_bias"][0], np.float32))
    shared["wr"] = np.ascontiguousarray(np.concatenate(
        [np.asarray(inputs["moe_w_group"], np.float32), np.asarray(inputs["moe_w_expert"], np.float32)], -1))
    brow = np.concatenate([np.asarray(inputs["moe_b_group"], np.float32),
                           np.asarray(inputs["moe_b_expert"], np.float32)], -1)
    shared["br"] = np.ascontiguousarray(np.broadcast_to(brow[None], (128, 2, 36)))
    if moe:
        shared["sel"] = cst["sel"]
        shared["moe_w1"] = f32(inputs["moe_w1"])
        shared["moe_w3"] = f32(inputs["moe_w3"])
        shared["moe_w2"] = f32(inputs["moe_w2"])
    maps = []
    for b in range(ncores):
        m = dict(shared)
        m["xin"] = np.ascontiguousarray(np.concatenate([ctx[b].T, x[b].T], 1))
        m["cc"] = np.ascontiguousarray(np.stack([_feat(c[b]), _feat(c_ctx)], -1))
        maps.append(m)
    return maps


def kernel(**inputs):
    nc = bass.Bass("TRN2", target_bir_lowering=False)
    bld = Builder(nc)
    bld.build()
    maps = make_in_maps(inputs, 8)
    maps = [{k: v for k, v in m.items() if k in bld.inputs} for m in maps]
    res = run_bass_kernel_spmd(nc, maps, core_ids=list(range(8)))
    bld.P.close()
    out = np.stack([np.ascontiguousarray(np.asarray(r["outT"]).T) for r in res.results], 0)
    return out.astype(np.float32)
```
